# Optimizing a Trainium2 kernel written in Bass

```python
import math
import jax, jax.numpy as jnp
from jax import lax
import numpy as np

D_MODEL = 1024
BATCH = 32
SEQ = 2048
DEPTH = 2

N_MEM = 256
HEAD_DIM = 64
ROPE_DIM = HEAD_DIM // 4
ROPE_THETA = 500000.0
RMS_EPS = 1e-6
N_TOK_HEADS = 12
N_MEM_HEADS = 4
MIX_WIDTH = (N_TOK_HEADS + N_MEM_HEADS) * HEAD_DIM
MEM_Q = N_MEM_HEADS * HEAD_DIM
D_FF = ((8 * D_MODEL // 3 + 255) // 256) * 256
Q_BLOCK = 128

DSA_KV_RANK = 128
DSA_NOPE = HEAD_DIM - ROPE_DIM
IDX_HEADS = 8
IDX_DIM = 64
DSA_TOPK_MAX = 256
DSA_COLS = [N_TOK_HEADS * HEAD_DIM, DSA_KV_RANK, ROPE_DIM,
            IDX_HEADS * IDX_DIM, IDX_DIM, IDX_HEADS, MEM_Q]

NSA_GROUPS = 2
NSA_HPG = N_TOK_HEADS // NSA_GROUPS
CMP_LEN = 32
CMP_STRIDE = 16
CMP_HIDDEN = 128
SLC_LEN = 64
SLC_TOP_MAX = 16
WIN = 512
NSA_Q_BLOCK = 64
FORCE_SCORE = 1e9
NSA_KV = NSA_GROUPS * HEAD_DIM
NSA_COLS = [N_TOK_HEADS * HEAD_DIM, NSA_KV, NSA_KV, NSA_KV, NSA_KV, NSA_KV, NSA_KV,
            N_TOK_HEADS * 3, MEM_Q]

N_A = (DEPTH + 1) // 2
N_B = DEPTH // 2

kernel_name = "hybrid_dsa_nsa_memory_trunk"


def split_cols(x, sizes):
    return jnp.split(x, np.cumsum(sizes)[:-1].tolist(), axis=-1)


def rmsnorm(x, g):
    xf = x.astype(jnp.float32)
    y = xf * lax.rsqrt(jnp.mean(xf * xf, axis=-1, keepdims=True) + RMS_EPS)
    return (y * g.astype(jnp.float32)).astype(x.dtype)


def rope_tables(S):
    inv = ROPE_THETA ** (-np.arange(0, ROPE_DIM, 2, dtype=np.float64) / ROPE_DIM)
    ang = np.arange(S, dtype=np.float64)[:, None] * inv[None, :]
    return jnp.asarray(np.cos(ang).astype(np.float32)), jnp.asarray(np.sin(ang).astype(np.float32))


def apply_partial_rope(x, cos, sin):
    xr, xp = x[..., :ROPE_DIM], x[..., ROPE_DIM:]
    x1, x2 = xr[..., :ROPE_DIM // 2], xr[..., ROPE_DIM // 2:]
    c, s = cos[:, None, :], sin[:, None, :]
    rot = jnp.concatenate([x1 * c - x2 * s, x1 * s + x2 * c], axis=-1).astype(x.dtype)
    return jnp.concatenate([rot, xp], axis=-1)


def masked_softmax(s, mask):
    s = jnp.where(mask, s.astype(jnp.float32), -jnp.inf)
    m = jnp.max(s, axis=-1, keepdims=True)
    m = jnp.where(jnp.isfinite(m), m, 0.0)
    p = jnp.exp(s - m)
    return p / jnp.maximum(jnp.sum(p, axis=-1, keepdims=True), 1e-30)


def memory_attention(q_mem, mem_n, w_kv_mem):
    B, S, _ = q_mem.shape
    q = q_mem.reshape(B, S, N_MEM_HEADS, HEAD_DIM)
    k, v = jnp.split(mem_n @ w_kv_mem, 2, axis=-1)
    k = k.reshape(B, -1, N_MEM_HEADS, HEAD_DIM)
    v = v.reshape(B, -1, N_MEM_HEADS, HEAD_DIM)
    s = jnp.einsum('bshd,bnhd->bhsn', q, k).astype(jnp.float32) * HEAD_DIM ** -0.5
    p = jax.nn.softmax(s, axis=-1).astype(v.dtype)
    return jnp.einsum('bhsn,bnhd->bshd', p, v).reshape(B, S, MEM_Q)


def dsa_mixer(cols, ckv_norm, w_uk, w_uv, cos, sin):
    q, ckv, k_rope, q_idx, k_idx, w_idx = cols
    B, S, _ = q.shape
    q = apply_partial_rope(q.reshape(B, S, N_TOK_HEADS, HEAD_DIM), cos, sin)
    ckv = rmsnorm(ckv, ckv_norm)
    k_nope = ckv @ w_uk
    v = ckv @ w_uv
    k_r = apply_partial_rope(k_rope[:, :, None, :], cos, sin)[:, :, 0]
    k = jnp.concatenate([k_r, k_nope], axis=-1)
    q_idx = apply_partial_rope(q_idx.reshape(B, S, IDX_HEADS, IDX_DIM), cos, sin)
    k_idx = apply_partial_rope(k_idx[:, :, None, :], cos, sin)[:, :, 0]
    w_idx = w_idx.astype(jnp.float32) * (IDX_HEADS ** -0.5 * IDX_DIM ** -0.5)
    topk = min(DSA_TOPK_MAX, S // 4)
    key_pos = jnp.arange(S)
    gather = jax.vmap(lambda kb, ib: kb[ib])

    def block(i):
        t0 = i * Q_BLOCK
        qpos = t0 + jnp.arange(Q_BLOCK)
        qi = lax.dynamic_slice_in_dim(q, t0, Q_BLOCK, axis=1)
        qii = lax.dynamic_slice_in_dim(q_idx, t0, Q_BLOCK, axis=1)
        wi = lax.dynamic_slice_in_dim(w_idx, t0, Q_BLOCK, axis=1)
        logits = jnp.einsum('bthd,bsd->bths', qii, k_idx).astype(jnp.float32)
        score = jnp.einsum('bths,bth->bts', jax.nn.relu(logits), wi)
        causal = key_pos[None, :] <= qpos[:, None]
        score = jnp.where(causal[None], score, -jnp.inf)
        _, idx = lax.top_k(score, topk)
        k_sel = gather(k, idx)
        v_sel = gather(v, idx)
        valid = idx <= qpos[None, :, None]
        s = jnp.einsum('bthd,btkd->bthk', qi, k_sel) * HEAD_DIM ** -0.5
        p = masked_softmax(s, valid[:, :, None, :]).astype(v.dtype)
        return jnp.einsum('bthk,btkd->bthd', p, v_sel).reshape(B, Q_BLOCK, N_TOK_HEADS * HEAD_DIM)

    out = lax.map(block, jnp.arange(S // Q_BLOCK))
    return out.transpose(1, 0, 2, 3).reshape(B, S, N_TOK_HEADS * HEAD_DIM)


def cmp_to_slc_overlap(n_c, n_slc):
    c0 = np.arange(n_c) * CMP_STRIDE
    s0 = np.arange(n_slc) * SLC_LEN
    ov = np.minimum(c0[:, None] + CMP_LEN, s0[None, :] + SLC_LEN) - np.maximum(c0[:, None], s0[None, :])
    return jnp.asarray((np.clip(ov, 0, None) / CMP_LEN).astype(np.float32))


def compress(kv, pos_emb, w1, w2):
    B, S, G, Dh = kv.shape
    n_c = (S - CMP_LEN) // CMP_STRIDE + 1
    blk = np.arange(n_c)[:, None] * CMP_STRIDE + np.arange(CMP_LEN)[None, :]
    blocks = kv[:, blk] + pos_emb[None, None, :, None, :]
    flat = blocks.transpose(0, 1, 3, 2, 4).reshape(B, n_c, G, CMP_LEN * Dh)
    return jax.nn.gelu(flat @ w1) @ w2


def nsa_mixer(cols, pos_k, pos_v, ck_w1, ck_w2, cv_w1, cv_w2, cos, sin):
    q, kc, vc, ks, vs, kw, vw, gates = cols
    B, S, _ = q.shape
    G, J, Dh = NSA_GROUPS, NSA_HPG, HEAD_DIM
    q = q.reshape(B, S, N_TOK_HEADS, Dh)
    q_rot = apply_partial_rope(q, cos, sin)
    kc, vc = kc.reshape(B, S, G, Dh), vc.reshape(B, S, G, Dh)
    ks = apply_partial_rope(ks.reshape(B, S, G, Dh), cos, sin)
    vs = vs.reshape(B, S, G, Dh)
    kw = apply_partial_rope(kw.reshape(B, S, G, Dh), cos, sin)
    vw = vw.reshape(B, S, G, Dh)
    gates = jax.nn.sigmoid(gates.astype(jnp.float32)).astype(q.dtype).reshape(B, S, G, J, 3)
    scale = Dh ** -0.5

    k_cmp = compress(kc, pos_k, ck_w1, ck_w2)
    v_cmp = compress(vc, pos_v, cv_w1, cv_w2)
    n_c = k_cmp.shape[1]
    cmp_end = jnp.arange(n_c) * CMP_STRIDE + CMP_LEN - 1
    n_slc = S // SLC_LEN
    n_sel = min(SLC_TOP_MAX, n_slc)
    overlap = cmp_to_slc_overlap(n_c, n_slc)
    slc_ids = jnp.arange(n_slc)
    ks_b = ks.reshape(B, n_slc, SLC_LEN, G, Dh).transpose(0, 3, 1, 2, 4)
    vs_b = vs.reshape(B, n_slc, SLC_LEN, G, Dh).transpose(0, 3, 1, 2, 4)
    gather = jax.vmap(jax.vmap(lambda kb, ib: kb[ib]))
    kw_pad = jnp.pad(kw, ((0, 0), (WIN, 0), (0, 0), (0, 0)))
    vw_pad = jnp.pad(vw, ((0, 0), (WIN, 0), (0, 0), (0, 0)))

    def block(i):
        T = NSA_Q_BLOCK
        t0 = i * T
        qpos = t0 + jnp.arange(T)
        qc = lax.dynamic_slice_in_dim(q, t0, T, axis=1).reshape(B, T, G, J, Dh)
        qr = lax.dynamic_slice_in_dim(q_rot, t0, T, axis=1).reshape(B, T, G, J, Dh)
        gi = lax.dynamic_slice_in_dim(gates, t0, T, axis=1)
        s_c = jnp.einsum('btgjd,bcgd->btgjc', qc, k_cmp) * scale
        mask_c = cmp_end[None, :] <= qpos[:, None]
        p_c = masked_softmax(s_c, mask_c[None, :, None, None, :])
        o_c = jnp.einsum('btgjc,bcgd->btgjd', p_c.astype(v_cmp.dtype), v_cmp)
        imp = jnp.einsum('btgjc,cn->btgn', p_c, overlap)
        cur = qpos // SLC_LEN
        forced = (slc_ids[None] == 0) | (slc_ids[None] == cur[:, None]) | (slc_ids[None] == cur[:, None] - 1)
        admissible = slc_ids[None] <= cur[:, None]
        imp = jnp.where(forced[None, :, None, :], FORCE_SCORE, imp)
        imp = jnp.where(admissible[None, :, None, :], imp, -jnp.inf)
        _, sel = lax.top_k(imp, n_sel)
        sel_g = sel.transpose(0, 2, 1, 3)
        k_sel = gather(ks_b, sel_g).reshape(B, G, T, n_sel * SLC_LEN, Dh)
        v_sel = gather(vs_b, sel_g).reshape(B, G, T, n_sel * SLC_LEN, Dh)
        tok_pos = (sel_g[..., None] * SLC_LEN + jnp.arange(SLC_LEN)).reshape(B, G, T, n_sel * SLC_LEN)
        mask_s = tok_pos <= qpos[None, None, :, None]
        s_s = jnp.einsum('btgjd,bgtkd->bgtjk', qr, k_sel) * scale
        p_s = masked_softmax(s_s, mask_s[:, :, :, None, :]).astype(v_sel.dtype)
        o_s = jnp.einsum('bgtjk,bgtkd->btgjd', p_s, v_sel)
        kw_i = lax.dynamic_slice_in_dim(kw_pad, t0, WIN + T, axis=1)
        vw_i = lax.dynamic_slice_in_dim(vw_pad, t0, WIN + T, axis=1)
        kpos = t0 - WIN + jnp.arange(WIN + T)
        mask_w = (kpos[None] <= qpos[:, None]) & (kpos[None] > qpos[:, None] - WIN) & (kpos[None] >= 0)
        s_w = jnp.einsum('btgjd,bkgd->btgjk', qr, kw_i) * scale
        p_w = masked_softmax(s_w, mask_w[None, :, None, None, :]).astype(vw_i.dtype)
        o_w = jnp.einsum('btgjk,bkgd->btgjd', p_w, vw_i)
        o = gi[..., 0:1] * o_c + gi[..., 1:2] * o_s + gi[..., 2:3] * o_w
        return o.reshape(B, T, N_TOK_HEADS * Dh)

    out = lax.map(block, jnp.arange(S // NSA_Q_BLOCK))
    return out.transpose(1, 0, 2, 3).reshape(B, S, N_TOK_HEADS * HEAD_DIM)


def setup_inputs(seed: int = 0) -> dict:
    key = jax.random.key(seed)
    ks = jax.random.split(key, 24)

    def w(k, shape, fan_in):
        return jax.random.normal(k, shape, jnp.float32) * fan_in ** -0.5

    def gain(k, shape):
        return 1.0 + 0.02 * jax.random.normal(k, shape, jnp.float32)

    return {
        "x": jax.random.normal(ks[0], (BATCH, SEQ, D_MODEL), jnp.float32),
        "mem": jax.random.normal(ks[1], (BATCH, N_MEM, D_MODEL), jnp.float32),
        "attn_norm": gain(ks[2], (DEPTH, D_MODEL)),
        "mem_norm": gain(ks[3], (DEPTH, D_MODEL)),
        "ffn_norm": gain(ks[4], (DEPTH, D_MODEL)),
        "final_norm": gain(ks[5], (D_MODEL,)),
        "dsa_w_in": w(ks[6], (N_A, D_MODEL, sum(DSA_COLS)), D_MODEL),
        "dsa_ckv_norm": gain(ks[7], (N_A, DSA_KV_RANK)),
        "dsa_w_uk": w(ks[8], (N_A, DSA_KV_RANK, DSA_NOPE), DSA_KV_RANK),
        "dsa_w_uv": w(ks[9], (N_A, DSA_KV_RANK, HEAD_DIM), DSA_KV_RANK),
        "nsa_w_in": w(ks[10], (N_B, D_MODEL, sum(NSA_COLS)), D_MODEL),
        "nsa_cmp_pos_k": 0.1 * jax.random.normal(ks[11], (N_B, CMP_LEN, HEAD_DIM), jnp.float32),
        "nsa_cmp_pos_v": 0.1 * jax.random.normal(ks[12], (N_B, CMP_LEN, HEAD_DIM), jnp.float32),
        "nsa_cmp_k_w1": w(ks[13], (N_B, CMP_LEN * HEAD_DIM, CMP_HIDDEN), CMP_LEN * HEAD_DIM),
        "nsa_cmp_k_w2": w(ks[14], (N_B, CMP_HIDDEN, HEAD_DIM), CMP_HIDDEN),
        "nsa_cmp_v_w1": w(ks[15], (N_B, CMP_LEN * HEAD_DIM, CMP_HIDDEN), CMP_LEN * HEAD_DIM),
        "nsa_cmp_v_w2": w(ks[16], (N_B, CMP_HIDDEN, HEAD_DIM), CMP_HIDDEN),
        "mem_w_kv": w(ks[17], (DEPTH, D_MODEL, 2 * MEM_Q), D_MODEL),
        "w_o": w(ks[18], (DEPTH, MIX_WIDTH, D_MODEL), MIX_WIDTH),
        "ffn_w_in": w(ks[19], (DEPTH, D_MODEL, 2 * D_FF), D_MODEL),
        "ffn_w_down": w(ks[20], (DEPTH, D_FF, D_MODEL), D_FF),
    }


def reference(x, mem, attn_norm, mem_norm, ffn_norm, final_norm,
              dsa_w_in, dsa_ckv_norm, dsa_w_uk, dsa_w_uv,
              nsa_w_in, nsa_cmp_pos_k, nsa_cmp_pos_v,
              nsa_cmp_k_w1, nsa_cmp_k_w2, nsa_cmp_v_w1, nsa_cmp_v_w2,
              mem_w_kv, w_o, ffn_w_in, ffn_w_down):
    S = x.shape[1]
    cos, sin = rope_tables(S)
    for i in range(DEPTH):
        h = rmsnorm(x, attn_norm[i])
        mem_n = rmsnorm(mem, mem_norm[i])
        if i % 2 == 0:
            a = i // 2
            cols = split_cols(h @ dsa_w_in[a], DSA_COLS)
            q_mem = cols[-1]
            tok = dsa_mixer(cols[:-1], dsa_ckv_norm[a], dsa_w_uk[a], dsa_w_uv[a], cos, sin)
        else:
            b = i // 2
            cols = split_cols(h @ nsa_w_in[b], NSA_COLS)
            q_mem = cols[-1]
            tok = nsa_mixer(cols[:-1], nsa_cmp_pos_k[b], nsa_cmp_pos_v[b],
                            nsa_cmp_k_w1[b], nsa_cmp_k_w2[b], nsa_cmp_v_w1[b], nsa_cmp_v_w2[b],
                            cos, sin)
        o_mem = memory_attention(q_mem, mem_n, mem_w_kv[i])
        x = x + jnp.concatenate([tok, o_mem], axis=-1) @ w_o[i]
        h = rmsnorm(x, ffn_norm[i])
        g, u = jnp.split(h @ ffn_w_in[i], 2, axis=-1)
        x = x + (jax.nn.silu(g) * u) @ ffn_w_down[i]
    return rmsnorm(x, final_norm)
```

```python
import numpy as np
import concourse.bass as bass
import concourse.mybir as mybir
from concourse.bass_utils import run_bass_kernel_spmd

F32 = mybir.dt.float32
BF16 = mybir.dt.bfloat16
AF = mybir.ActivationFunctionType
ALU = mybir.AluOpType
AX = mybir.AxisListType

T = 2048
NG = 4
GT = 512
D = 1024
DFF = 2816
NCORES = 8
SEQ_PER_CORE = 4
EPS = 1e-6
SEM_L = 8000
N_DSEM = 24
NEG = -1.0e30

DSA_W = 1752
NSA_W = 1828


class Eng:
    def __init__(self, P, name, b, pe=False):
        self.P, self.name, self.b, self.pe = P, name, b, pe
        self.cnt = 0
        self.rank = 0
        self.rankmap = {}
        self.sems = []
        self.seen = {}

    def _sem(self, r):
        e = (r - 1) // SEM_L
        while len(self.sems) <= e:
            self.sems.append(self.P.new_sem(f"{self.name}{len(self.sems)}"))
        return self.sems[e], (r - 1) % SEM_L + 1

    def sem_for(self, c):
        return self._sem(self.rankmap[c])


class DSem:
    def __init__(self, P, i):
        self.sem = P.new_sem(f"dma{i}")
        self.cnt = 0
        self.pe = False
        self.name = f"dma{i}"

    def sem_for(self, c):
        return self.sem, 16 * c


class Prog:
    def __init__(self, nc, needed=None):
        self.nc = nc
        self.nsem = 0
        self.needed = needed
        self.waited = set()
        self.pe = Eng(self, "pe", nc.tensor, pe=True)
        self.act = Eng(self, "act", nc.scalar)
        self.dve = Eng(self, "dve", nc.vector)
        self.pool = Eng(self, "pool", nc.gpsimd)
        self.sp = Eng(self, "sp", nc.sync)
        self.dsems = [DSem(self, i) for i in range(N_DSEM)]
        self.di = 0
        self.R = {}
        self.nins = 0
        self.nwait = 0
        self.ninc = 0

    def new_sem(self, name):
        self.nsem += 1
        return self.nc.semaphore(name).__enter__()

    def _wait(self, eng, obj, c):
        if eng.seen.get(obj, 0) >= c:
            return
        if isinstance(obj, Eng):
            self.waited.add((obj.name, c))
        sem, v = obj.sem_for(c)
        eng.b.wait_ge(sem, v)
        eng.seen[obj] = c
        self.nwait += 1

    def _deps(self, eng, reads, writes, nowaw=False):
        for k in reads:
            r = self.R.get(k)
            if r is None:
                continue
            for obj, c in r[0].items():
                if obj is eng and eng.pe:
                    continue
                self._wait(eng, obj, c)
        for k in writes:
            r = self.R.get(k)
            if r is None:
                continue
            if not nowaw:
                for obj, c in r[0].items():
                    if obj is eng and eng.pe:
                        continue
                    self._wait(eng, obj, c)
            for obj, c in r[1].items():
                if obj is eng and eng.pe:
                    continue
                self._wait(eng, obj, c)

    def _mark(self, obj, c, reads, writes):
        for k in reads:
            r = self.R.get(k)
            if r is None:
                r = self.R[k] = [{}, {}]
            r[1][obj] = c
        for k in writes:
            r = self.R.get(k)
            if r is None or r[1]:
                self.R[k] = [{obj: c}, {}]
            else:
                r[0][obj] = c

    def op(self, eng, fn, R=(), W=()):
        self._deps(eng, R, W)
        ins = fn()
        eng.cnt += 1
        if self.needed is None or (eng.name, eng.cnt) in self.needed:
            eng.rank += 1
            sem, _ = eng._sem(eng.rank)
            ins.then_inc(sem, 1)
            self.ninc += 1
        eng.rankmap[eng.cnt] = eng.rank
        self._mark(eng, eng.cnt, R, W)
        self.nins += 1
        return ins

    def dma(self, out, in_, R=(), W=(), q=None, nowaw=True):
        eng = q or self.sp
        d = self.dsems[self.di % N_DSEM]
        self.di += 1
        if d.cnt > 0:
            self._wait(eng, d, d.cnt)
        self._deps(eng, R, W, nowaw=nowaw)
        ins = eng.b.dma_start(out=out, in_=in_)
        d.cnt += 1
        ins.then_inc(d.sem, 16)
        self._mark(d, d.cnt, R, W)
        self.nins += 1
        return ins

    def drain(self):
        for d in self.dsems:
            if d.cnt > 0:
                self._wait(self.sp, d, d.cnt)


class Ring:
    def __init__(self, items):
        self.items = items
        self.i = 0

    def next(self):
        it = self.items[self.i % len(self.items)]
        self.i += 1
        return it


def _rope_tables():
    inv = 500000.0 ** (-np.arange(0, 16, 2, dtype=np.float64) / 16)
    ang = np.arange(T, dtype=np.float64)[:, None] * inv[None, :]
    cos, sin = np.cos(ang).astype(np.float32), np.sin(ang).astype(np.float32)
    C = np.ones((128, T), np.float32)
    S = np.zeros((128, T), np.float32)
    for r in range(128):
        d = r % 64
        if d < 8:
            C[r] = cos[:, d]
            S[r] = -sin[:, d]
        elif d < 16:
            C[r] = cos[:, d - 8]
            S[r] = sin[:, d - 8]
    return C, S


def make_consts():
    c = {}
    C, S = _rope_tables()
    c["ropeC"], c["ropeS"] = C, S
    perm = np.zeros((128, 128), np.float32)
    for m in range(128):
        d = m % 64
        base = m - d
        if d < 8:
            perm[base + d + 8, m] = 1.0
        elif d < 16:
            perm[base + d - 8, m] = 1.0
    ident = np.eye(128, dtype=np.float32)
    sl = np.arange(128)
    cb = np.where(sl[None, :] > sl[:, None], NEG, 0.0).astype(np.float32)
    c["misc128"] = np.stack([perm, ident, np.ones((128, 128), np.float32), cb], axis=1)
    tl = np.arange(512)
    cm = np.zeros((128, 4, 512), np.float32)
    for r in range(4):
        cm[:, r, :] = ((128 * r + sl)[:, None] <= tl[None, :]).astype(np.float32)
    c["cm"] = cm
    wm = np.zeros((128, 8, 512), np.float32)
    for j in range(8):
        s = 128 * (j - 4) + sl
        wm[:, j, :] = ((s[:, None] <= tl[None, :]) & (s[:, None] > tl[None, :] - 512)).astype(np.float32)
    c["wm"] = wm
    cc = np.arange(128)
    mc = ((16 * cc + 31)[:, None] <= np.arange(T)[None, :]).astype(np.float32)
    mc[127] = 0.0
    c["mcT"] = mc
    c0 = cc * 16
    s0 = np.arange(32) * 64
    ov = np.minimum(c0[:, None] + 32, s0[None, :] + 64) - np.maximum(c0[:, None], s0[None, :])
    ov = (np.clip(ov, 0, None) / 32.0).astype(np.float32)
    ov33 = np.concatenate([ov, np.ones((128, 1), np.float32)], axis=1)
    ov33[127] = 0.0
    c["ov33"] = ov33
    tt = np.arange(T)
    cur = tt // 64
    n = np.arange(32)
    forced = (n[None] == 0) | (n[None] == cur[:, None]) | (n[None] == cur[:, None] - 1)
    adm = n[None] <= cur[:, None]
    Ff = np.where(forced, 1.0e9, 0.0).astype(np.float32)
    Ab = np.where(adm, 0.0, NEG).astype(np.float32)
    pl = (np.arange(128) >= 64).astype(np.int64)
    y = np.arange(62) - 30
    c["selF"] = np.where((y[None] == pl[:, None]) | (y[None] == pl[:, None] - 1), 1.0e9, 0.0).astype(np.float32)
    c["selA"] = np.where(y[None] <= pl[:, None], 0.0, NEG).astype(np.float32)
    E = np.zeros((64, 2, T), np.float32)
    for g in range(2):
        for nn in range(32):
            E[g * 32 + nn, g, nn * 64:(nn + 1) * 64] = 1.0
    c["Eexp"] = E
    c["bis"] = np.tile((2.0 ** -(np.arange(32) + 1)).astype(np.float32)[None, :], (128, 1))
    return c


WSPEC = [
    ("dsa_w_in", 1024, DSA_W), ("nsa_w_in", 1024, NSA_W),
    ("mem_w_kv0", 1024, 512), ("mem_w_kv1", 1024, 512),
    ("w_o0", 1024, 1024), ("w_o1", 1024, 1024),
    ("ffn_w_in0", 1024, 2 * DFF), ("ffn_w_in1", 1024, 2 * DFF),
    ("ffn_w_down0", DFF, 1024), ("ffn_w_down1", DFF, 1024),
    ("cmp_k_w1", 2048, 128), ("cmp_v_w1", 2048, 128),
]
SMALLW = [("dsa_w_uk", 128, 48), ("dsa_w_uv", 128, 64), ("cmp_k_w2", 128, 64), ("cmp_v_w2", 128, 64)]
CONST_SHAPES = None


class StopBuild(Exception):
    pass


STOP = [0]


def build(n_seq, n_layers=2, needed=None):
    nc = bass.Bass("TRN2", target_bir_lowering=False)
    P = Prog(nc, needed)
    pe, act, dve, pool = P.pe, P.act, P.dve, P.pool
    consts = make_consts()

    def din(name, shape, dt=F32):
        return nc.dram_tensor(name, list(shape), dt, kind="ExternalInput").ap()

    xT_d = din("xT", [n_seq, D, T])
    memT_d = din("memT", [n_seq, D, 256])
    gam_d = din("gam", [128, 7, 8])
    ckvg_d = din("ckvg", [128, 1])
    posk_d = din("posk", [128, 16])
    posv_d = din("posv", [128, 16])
    w_d = {n: din(n, [K, N]) for n, K, N in WSPEC}
    sw_d = {n: din(n, [K, N]) for n, K, N in SMALLW}
    c_d = {k: din("c_" + k, v.shape) for k, v in consts.items()}
    outT_d = nc.dram_tensor("outT", [n_seq, D, T], F32, kind="ExternalOutput").ap()
    xs_d = nc.dram_tensor("xs", [n_seq, D, T], F32).ap()
    wb_d = {n: nc.dram_tensor("b_" + n, [K, N], BF16).ap() for n, K, N in WSPEC}

    ARENA_BYTES = 212000
    arena = nc.alloc_sbuf_tensor("arena", [128, ARENA_BYTES // 4], F32)
    a_base = nc.lookup_mloc(arena).addr
    a_ptr = [0]
    a_max = [0]
    DT_SZ = {F32: 4, BF16: 2}

    def sb(name, shape, dt=F32):
        nbytes = int(np.prod(shape[1:])) * DT_SZ[dt]
        off = (a_ptr[0] + 31) // 32 * 32
        a_ptr[0] = off + nbytes
        a_max[0] = max(a_max[0], a_ptr[0])
        assert a_ptr[0] <= ARENA_BYTES, (name, a_ptr[0])
        return nc.alloc_sbuf_tensor_at("s_" + name, list(shape), dt, offset=a_base + off)

    def barrier():
        engs = [P.pe, P.act, P.dve, P.pool, P.sp]
        for e in engs:
            for o in engs:
                if o is not e and o.cnt > 0:
                    P._wait(e, o, o.cnt)
            for d_ in P.dsems:
                if d_.cnt > 0:
                    P._wait(e, d_, d_.cnt)

    xg = [sb("xg0", [128, 8, GT])]
    wbuf = [sb(f"wbuf{i}", [128, 8, 512], BF16) for i in range(2)]
    xg_flat = xg[0][:].rearrange("p a b -> p (a b)")
    stage = [xg_flat[:, i * 2048:(i + 1) * 2048] for i in range(2)]
    stage_b = [wbuf[i][:].rearrange("p a b -> p (a b)")[:, 0:2048] for i in range(2)]
    SCH = 2048
    misc_f = sb("misc_f", [128, 4, 128])
    misc_b = sb("misc_b", [128, 3, 128], BF16)
    cm_b = sb("cm_b", [128, 4, 512], BF16)
    wm_b = sb("wm_b", [128, 8, 512], BF16)
    mc_b = sb("mc_b", [128, T], BF16)
    ov_b = sb("ov_b", [128, 33], BF16)
    selF = sb("selF", [128, 62])
    selA = sb("selA", [128, 62])
    E_b = sb("E_b", [64, 2, T], BF16)
    bis = sb("bis", [128, 32])
    gam = sb("gam", [128, 7, 8])
    ckvg = sb("ckvg", [128, 1])
    cvt_i = [0]
    epsT = sb("epsT", [128, 1])
    P.op(pool, lambda: nc.gpsimd.memset(epsT[:], EPS), W=["epsT"])

    def cvt(dst_ap, src_ap, R, W):
        engs = [dve, pool, act]
        e = engs[cvt_i[0] % 3]
        cvt_i[0] += 1
        if e is act:
            P.op(e, lambda: nc.scalar.copy(out=dst_ap, in_=src_ap), R=R, W=W)
        else:
            P.op(e, lambda: e.b.tensor_copy(out=dst_ap, in_=src_ap), R=R, W=W)

    def load_const(dst, src_d, key, bf=False, npart=128, cols=None):
        if not bf:
            P.dma(dst[:], src_d, W=[key])
            return
        shp = list(src_d.shape)
        flat = int(np.prod(shp[1:]))
        srcf = src_d if len(shp) == 2 else src_d.rearrange("p a b -> p (a b)")
        dstf = dst[:] if len(shp) == 2 else dst[:].rearrange("p a b -> p (a b)")
        for o in range(0, flat, SCH):
            n = min(SCH, flat - o)
            i = cvt_i[0] % 2
            P.dma(stage[i][0:shp[0], 0:n], srcf[:, o:o + n], W=[("stage", i)], nowaw=False)
            cvt(dstf[:, o:o + n], stage[i][0:shp[0], 0:n], R=[("stage", i)], W=[key])

    load_const(misc_f, c_d["misc128"], "misc_f")
    P.op(dve, lambda: nc.vector.tensor_copy(out=misc_b[:], in_=misc_f[:, 0:3, :]), R=["misc_f"], W=["misc_b"])
    load_const(cm_b, c_d["cm"], "cm_b", bf=True)
    load_const(wm_b, c_d["wm"], "wm_b", bf=True)
    load_const(mc_b, c_d["mcT"], "mc_b", bf=True)
    load_const(ov_b, c_d["ov33"], "ov_b", bf=True)
    load_const(selF, c_d["selF"], "selF")
    load_const(selA, c_d["selA"], "selA")
    load_const(E_b, c_d["Eexp"], "E_b", bf=True)
    load_const(bis, c_d["bis"], "bis")
    load_const(gam, gam_d, "gam")
    load_const(ckvg, ckvg_d, "ckvg")
    perm_b, ident_b, ones_b = misc_b[:, 0, :], misc_b[:, 1, :], misc_b[:, 2, :]
    E_flat = E_b[:].rearrange("p g s -> p (g s)")
    ones_f = misc_f[:, 2, :]
    cbias_f = misc_f[:, 3, :]

    for name, K, N in WSPEC:
        for kc in range(K // 128):
            for o in range(0, N, SCH):
                n = min(SCH, N - o)
                i = cvt_i[0] % 2
                P.dma(stage[i][:, 0:n], w_d[name][kc * 128:(kc + 1) * 128, o:o + n], W=[("stage", i)], nowaw=False)
                j = i
                cvt(stage_b[j][:, 0:n], stage[i][:, 0:n], R=[("stage", i)], W=[("stageb", j)])
                P.dma(wb_d[name][kc * 128:(kc + 1) * 128, o:o + n], stage_b[j][:, 0:n],
                      R=[("stageb", j)], W=[("wb", name)])
    wuk2 = sb("wuk2", [128, 128], BF16)
    wuv = sb("wuv", [128, 64], BF16)
    w2k2 = sb("w2k2", [128, 128], BF16)
    w2v = sb("w2v", [128, 64], BF16)
    smst = sb("smst", [128, 4, 64])
    P.op(pool, lambda: nc.gpsimd.memset(wuk2[:], 0.0), W=["wuk2"])
    P.dma(smst[:, 0, 0:48], sw_d["dsa_w_uk"], W=["smst"])
    P.dma(smst[:, 1, :], sw_d["dsa_w_uv"], W=["smst"])
    P.dma(smst[:, 2, :], sw_d["cmp_k_w2"], W=["smst"])
    P.dma(smst[:, 3, :], sw_d["cmp_v_w2"], W=["smst"])
    P.op(dve, lambda: nc.vector.tensor_copy(out=wuk2[:, 16:64], in_=smst[:, 0, 0:48]), R=["smst"], W=["wuk2"])
    P.op(dve, lambda: nc.vector.tensor_copy(out=wuk2[:, 80:128], in_=smst[:, 0, 0:48]), R=["smst"], W=["wuk2"])
    P.op(dve, lambda: nc.vector.tensor_copy(out=wuv[:], in_=smst[:, 1, :]), R=["smst"], W=["wuv"])
    P.op(dve, lambda: nc.vector.tensor_copy(out=w2k2[:, 0:64], in_=smst[:, 2, :]), R=["smst"], W=["w2k2"])
    P.op(dve, lambda: nc.vector.tensor_copy(out=w2k2[:, 64:128], in_=smst[:, 2, :]), R=["smst"], W=["w2k2"])
    P.op(dve, lambda: nc.vector.tensor_copy(out=w2v[:], in_=smst[:, 3, :]), R=["smst"], W=["w2v"])
    posf = sb("posf", [128, 2, 16])
    posb = sb("posb", [128, 2, 16], BF16)
    P.dma(posf[:, 0, :], posk_d, W=["posf"])
    P.dma(posf[:, 1, :], posv_d, W=["posf"])
    P.op(dve, lambda: nc.vector.tensor_copy(out=posb[:], in_=posf[:]), R=["posf"], W=["posb"])

    barrier()
    psf = nc.alloc_psum_tensor("psf", [128, 7, 512], F32)
    psb = nc.alloc_psum_tensor("psb", [128, 1024], BF16)
    bank_i = [0]

    def bank():
        b = bank_i[0] % 4
        bank_i[0] += 1
        return b

    bankL_i = [0]

    def bankL():
        b = 4 + bankL_i[0] % 3
        bankL_i[0] += 1
        return b

    def bank2():
        if not hasattr(bank2, "i"):
            bank2.i = 0
        b = (bank2.i % 2) * 2
        bank2.i += 1
        return b

    xg_i = [0]
    wbuf_i = [0]
    hT = sb("hT", [128, 8, GT], BF16)
    oT = hT
    sq = [sb(f"sq{i}", [128, GT]) for i in range(1)]
    sq_i = [0]
    sqh = [sb(f"sqh{i}", [128, 2, GT], BF16) for i in range(1)]
    sqh_i = [0]
    rstd = sb("rstd", [128, GT])
    wdn = [sb(f"wdn{i}", [128, 11, 128], BF16) for i in range(2)]
    wdn_i = [0]
    ropeCs = sb("ropeCs", [128, GT])
    ropeSs = sb("ropeSs", [128, GT])
    pre = [sb(f"pre{i}", [128, GT], BF16) for i in range(2)]
    pre_i = [0]
    rtmp = [sb(f"rtmp{i}", [128, GT]) for i in range(2)]
    rtmp_i = [0]
    qT = sb("qT", [128, 6, GT], BF16)
    qmemT = sb("qmemT", [128, 2, GT], BF16)
    mk_mark = a_ptr[0]
    maskT = sb("maskT", [128, 16, GT], BF16)
    mk_end = a_ptr[0]
    a_ptr[0] = mk_mark
    actT = sb("maskT", [128, 11, GT], BF16)
    a_ptr[0] = mk_mark
    memnT = sb("maskT", [128, 8, 256], BF16)
    a_ptr[0] = mk_end
    memf = xg[0][:, :, 0:256]
    sil = [sb(f"sil{i}", [128, GT], BF16) for i in range(2)]
    sil_i = [0]
    kmemT = sb("kmemT", [128, 2, 256], BF16)
    v1m = sb("v1m", [128, 2, 4, 128], BF16)
    PT = [sb(f"PT{i}", [128, 2, GT], BF16) for i in range(3)]
    PT_i = [0]
    rd = [sb(f"rd{i}", [128, GT]) for i in range(2)]
    rd_i = [0]
    layer_mark = a_ptr[0]
    KT2 = sb("KT2", [128, T], BF16)
    kidxT2 = sb("kidxT2", [128, T], BF16)
    V1 = sb("V1", [128, 16, 128], BF16)
    ckvn = sb("ckvn", [128, GT], BF16)
    ckvf = sb("ckvf", [128, GT])
    wkr2 = sb("wkr2", [128, 8, 128], BF16)
    wki2 = sb("wki2", [128, 8, 128], BF16)
    widx = sb("widx", [128, 4, 8])
    Dh = [sb(f"Dh{i}", [128, 8, 128], BF16) for i in range(2)]
    Dh_i = [0]
    relu_t = [sb(f"relu{i}", [128, 512], BF16) for i in range(3)]
    relu_i = [0]
    sc = [sb(f"sc{i}", [128, T]) for i in range(2)]
    sc_i = [0]
    bst = sb("bst", [128, 8])
    wtab = sb("wtab", [128, 32])
    mask_tm = [sb(f"masktm{i}", [128, T], BF16) for i in range(2)]
    mask_i = [0]
    qidxT = sb("qidxT", [128, 4, GT], BF16)
    dsa_end = a_ptr[0]
    a_ptr[0] = layer_mark
    ksT2 = sb("ksT2", [128, 2, T], BF16)
    kwT2 = sb("kwT2", [128, 2, T], BF16)
    V1s = sb("V1s", [128, 16, 2, 128], BF16)
    V1w = sb("V1w", [128, 16, 2, 128], BF16)
    ph_mark = a_ptr[0]
    kcT = sb("kcT", [128, T], BF16)
    vcT = sb("vcT", [128, T], BF16)
    ph_end = a_ptr[0]
    a_ptr[0] = ph_mark
    cmpo = sb("cmpo", [128, 6, GT], BF16)
    oacc = sb("oacc", [128, GT])
    a_ptr[0] = max(a_ptr[0], ph_end)
    cb_b = sb("cb_b", [128, 2])
    kcmpT2 = sb("kcmpT2", [128, 2, 128], BF16)
    V1c = sb("V1c", [128, 2, 128], BF16)
    gel = [sb(f"gel{i}", [128, 128]) for i in range(4)]
    gT = sb("gT", [128, 128], BF16)
    qrawT = sb("qrawT", [128, 6, GT], BF16)
    sigT = sb("sigT", [36, GT])
    sigH = sb("sigH", [36, GT], BF16)
    sigL = sb("sigL", [36, GT], BF16)
    imp = sb("imp", [128, 4, 2, 32])
    imp2 = sb("imp2", [128, 4, 2, 32])
    imp3 = sb("imp3", [128, 4, 2, 32])
    m8 = sb("m8", [128, 16])
    selm = sb("selm", [128, 4, 64], BF16)
    selT = sb("selT", [64, GT], BF16)
    rec = sb("rec", [128, 8])
    otmp = [sb(f"otmp{i}", [128, GT]) for i in range(2)]
    otmp_i = [0]
    rg = [sb(f"rg{i}", [64, GT]) for i in range(2)]
    rg_i = [0]
    nsa_end = a_ptr[0]
    a_ptr[0] = max(dsa_end, nsa_end)

    def rot(lst, ctr):
        i = ctr[0] % len(lst)
        ctr[0] += 1
        return i

    def load_w(name, col0, ncols, nk=8):
        i = rot(wbuf, wbuf_i)
        src = wb_d[name].rearrange("(c p) n -> p c n", p=128)[:, 0:nk, col0:col0 + ncols]
        P.dma(wbuf[i][:, 0:nk, 0:ncols], src, R=[("wb", name)], W=[("wbuf", i)])
        return wbuf[i], ("wbuf", i)

    def sumsq_mm(b, src_ap, src_key, ncols, first, last):
        i = rot(sq, sq_i)
        P.op(act, lambda: nc.scalar.activation(out=sq[i][:, 0:ncols], in_=src_ap, func=AF.Square), R=[src_key], W=[("sq", i)])
        j = rot(sqh, sqh_i)
        P.op(dve, lambda: nc.vector.tensor_copy(out=sqh[j][:, 0, 0:ncols], in_=sq[i][:, 0:ncols]), R=[("sq", i)], W=[("sqh", j)])
        P.op(pool, lambda: nc.gpsimd.tensor_tensor(out=sqh[j][:, 1, 0:ncols], in0=sq[i][:, 0:ncols], in1=sqh[j][:, 0, 0:ncols], op=ALU.subtract),
             R=[("sq", i), ("sqh", j)], W=[("sqh", j)])
        P.op(pe, lambda: nc.tensor.matmul(psf[:, b, 0:ncols], lhsT=ones_b, rhs=sqh[j][:, 0, 0:ncols], start=first, stop=False),
             R=[("sqh", j), "misc_b"], W=[("ps", b)])
        P.op(pe, lambda: nc.tensor.matmul(psf[:, b, 0:ncols], lhsT=ones_b, rhs=sqh[j][:, 1, 0:ncols], start=False, stop=last),
             R=[("sqh", j), "misc_b"], W=[("ps", b)])

    def norm_group(xt, xkey, gidx, ncols=GT, dst=None, dkey="hT"):
        dst = hT if dst is None else dst
        b = bank()
        for c in range(8):
            sumsq_mm(b, xt[:, c, 0:ncols], xkey, ncols, c == 0, c == 7)
        P.op(act, lambda: nc.scalar.activation(out=rstd[:, 0:ncols], in_=psf[:, b, 0:ncols], func=AF.Sqrt,
                                               scale=1.0 / D, bias=epsT[:, 0:1]),
             R=[("ps", b)], W=["rstd"])
        P.op(dve, lambda: nc.vector.reciprocal(out=rstd[:, 0:ncols], in_=rstd[:, 0:ncols]), R=["rstd"], W=["rstd"])
        for c in range(8):
            P.op(dve, lambda c=c: nc.vector.scalar_tensor_tensor(
                out=dst[:, c, 0:ncols], in0=xt[:, c, 0:ncols], scalar=gam[:, gidx, c:c + 1],
                in1=rstd[:, 0:ncols], op0=ALU.mult, op1=ALU.mult),
                 R=[xkey, "rstd", "gam"], W=[dkey])

    def proj(lhs_fn, lhs_keys, rhs_t, rhs_key, ncols, M=128):
        b = bank()
        for kc in range(8):
            P.op(pe, lambda kc=kc: nc.tensor.matmul(psf[0:M, b, 0:ncols], lhsT=lhs_fn(kc), rhs=rhs_t[:, kc, 0:ncols],
                                                    start=(kc == 0), stop=(kc == 7)),
                 R=list(lhs_keys) + [rhs_key], W=[("ps", b)])
        return b

    def rope_from_bank(b, dst_ap, dkey, ncols=GT, raw=None):
        i = rot(pre, pre_i)
        P.op(act, lambda: nc.scalar.copy(out=pre[i][:, 0:ncols], in_=psf[:, b, 0:ncols]), R=[("ps", b)], W=[("pre", i)])
        if raw is not None:
            P.op(pool, lambda: nc.gpsimd.tensor_copy(out=raw[0], in_=pre[i][:, 0:ncols]), R=[("pre", i)], W=[raw[1]])
        b2 = bank()
        P.op(pe, lambda: nc.tensor.matmul(psf[:, b2, 0:ncols], lhsT=perm_b, rhs=pre[i][:, 0:ncols], start=True, stop=True),
             R=[("pre", i), "misc_b"], W=[("ps", b2)])
        j = rot(rtmp, rtmp_i)
        P.op(dve, lambda: nc.vector.tensor_tensor(out=rtmp[j][:, 0:ncols], in0=psf[:, b2, 0:ncols], in1=ropeSs[:, 0:ncols], op=ALU.mult),
             R=[("ps", b2), "ropeS"], W=[("rtmp", j)])
        k = rot(rtmp, rtmp_i)
        P.op(pool, lambda: nc.gpsimd.tensor_tensor(out=rtmp[k][:, 0:ncols], in0=pre[i][:, 0:ncols], in1=ropeCs[:, 0:ncols], op=ALU.mult),
             R=[("pre", i), "ropeC"], W=[("rtmp", k)])
        P.op(pool, lambda: nc.gpsimd.tensor_tensor(out=dst_ap, in0=rtmp[j][:, 0:ncols], in1=rtmp[k][:, 0:ncols], op=ALU.add),
             R=[("rtmp", j), ("rtmp", k)], W=[dkey])

    def evac(b, dst_ap, dkey, ncols=GT, M=128, eng=None):
        e = eng or act
        if e is act:
            P.op(act, lambda: nc.scalar.copy(out=dst_ap, in_=psf[0:M, b, 0:ncols]), R=[("ps", b)], W=[dkey])
        else:
            P.op(e, lambda: e.b.tensor_copy(out=dst_ap, in_=psf[0:M, b, 0:ncols]), R=[("ps", b)], W=[dkey])

    def load_rope(g):
        P.dma(ropeCs[:], c_d["ropeC"][:, g * GT:(g + 1) * GT], W=["ropeC"])
        P.dma(ropeSs[:], c_d["ropeS"][:, g * GT:(g + 1) * GT], W=["ropeS"])

    def load_x(src_d, s, g):
        i = 0
        src = src_d[s].rearrange("(c p) t -> p c t", p=128)
        for c in range(0, 8, 2):
            P.dma(xg[i][:, c:c + 2, :], src[:, c:c + 2, g * GT:(g + 1) * GT], W=[("xg", i)])
        return xg[i], ("xg", i)

    def attn_head(q_ap, q_key, blocks, dst_fn, scale=0.125, guard=False):
        bo = bankL()
        nb = len(blocks)
        pend = []
        for p0 in range(0, nb, 2):
            pair = blocks[p0:p0 + 2]
            b2 = bank2()
            for u, blk in enumerate(pair):
                P.op(pe, lambda blk=blk, u=u: nc.tensor.matmul(psf[:, b2 + u, :], lhsT=blk["kT"], rhs=q_ap, start=True, stop=True),
                     R=[blk["kkey"], q_key], W=[("ps", b2 + u)])
            pi = rot(PT, PT_i)
            n = len(pair)
            P.op(act, lambda b2=b2, pi=pi, n=n: nc.scalar.activation(out=PT[pi][:, 0:n, :], in_=psf[:, b2:b2 + n, :], func=AF.Exp, scale=scale),
                 R=[("ps", b2 + u) for u in range(n)], W=[("PT", pi)])
            for u, blk in enumerate(pair):
                if blk.get("mask") is not None:
                    m_ap, m_key = blk["mask"]
                    P.op(dve, lambda pi=pi, u=u, m_ap=m_ap: nc.vector.tensor_tensor(out=PT[pi][:, u, :], in0=PT[pi][:, u, :], in1=m_ap, op=ALU.mult),
                         R=[("PT", pi), m_key], W=[("PT", pi)])
            pend.append((pair, pi, p0))
            if len(pend) > 1:
                _pv(pend.pop(0), bo, nb)
        while pend:
            _pv(pend.pop(0), bo, nb)
        ri = rot(rd, rd_i)
        if guard:
            P.op(dve, lambda: nc.vector.tensor_scalar_max(out=rd[ri][0:64, :], in0=psf[0:64, bo, :], scalar1=1e-30),
                 R=[("ps", bo)], W=[("rd", ri)])
            P.op(dve, lambda: nc.vector.reciprocal(out=rd[ri][0:64, :], in_=rd[ri][0:64, :]), R=[("rd", ri)], W=[("rd", ri)])
        else:
            P.op(dve, lambda: nc.vector.reciprocal(out=rd[ri][0:64, :], in_=psf[0:64, bo, :]), R=[("ps", bo)], W=[("rd", ri)])
        dst_fn(bo, rd[ri][0:64, :], ("rd", ri))

    def _pv(item, bo, nb):
        pair, pi, p0 = item
        for u, blk in enumerate(pair):
            idx = p0 + u
            P.op(pe, lambda blk=blk, u=u, idx=idx: nc.tensor.matmul(psf[:, bo, :], lhsT=blk["v1"], rhs=PT[pi][:, u, :],
                                                                    start=(idx == 0), stop=(idx == nb - 1)),
                 R=[blk["vkey"], ("PT", pi)], W=[("ps", bo)])

    def plain_dst(chunk, base):
        def f(bo, rden, rkey):
            P.op(dve, lambda: nc.vector.tensor_tensor(out=oT[base:base + 64, chunk, :], in0=psf[64:128, bo, :], in1=rden, op=ALU.mult),
                 R=[("ps", bo), rkey], W=["hT"])
        return f

    def mem_kv(s, li):
        for c in range(0, 8, 4):
            P.dma(memf[:, c:c + 4, :], memT_d[s].rearrange("(c p) t -> p c t", p=128)[:, c:c + 4, :], W=[("xg", 0)])
        norm_group(memf, ("xg", 0), 2 + li, ncols=256, dst=memnT, dkey="maskT")
        wname = f"mem_w_kv{li}"
        wt, wk = load_w(wname, 0, 512)
        for c in range(2):
            b = proj(lambda kc, c=c: wt[:, kc, c * 128:(c + 1) * 128], [wk], memnT, "maskT", 256)
            evac(b, kmemT[:, c, :], "kmemT", ncols=256)
        P.op(pool, lambda: nc.gpsimd.memset(v1m[:, :, :, 0:64], 1.0), W=["v1m"])
        for nb_ in range(2):
            b = bank()
            for kc in range(8):
                P.op(pe, lambda kc=kc: nc.tensor.matmul(psf[:, b, 0:256], lhsT=memnT[:, kc, nb_ * 128:(nb_ + 1) * 128],
                                                        rhs=wt[:, kc, 256:512], start=(kc == 0), stop=(kc == 7)),
                     R=["maskT", wk], W=[("ps", b)])
            P.op(act, lambda: nc.scalar.copy(out=v1m[:, nb_, :, 64:128], in_=psf[:, b, 0:256].rearrange("p (h d) -> p h d", h=4)),
                 R=[("ps", b)], W=["v1m"])

    def mem_attn():
        for h in range(4):
            c, base = h // 2, (h % 2) * 64
            blocks = [dict(kT=kmemT[base:base + 64, c, nb_ * 128:(nb_ + 1) * 128], kkey="kmemT",
                           v1=v1m[:, nb_, h, :], vkey="v1m", mask=None) for nb_ in range(2)]
            attn_head(qmemT[base:base + 64, c, :], "qmemT", blocks, plain_dst(6 + c, base))

    def wo_ffn(s, li, g, xt, xkey, last):
        for half in range(2):
            wt, wk = load_w(f"w_o{li}", half * 512, 512)
            for oc4 in range(4):
                oc = half * 4 + oc4
                b = proj(lambda kc, oc4=oc4, wt=wt: wt[:, kc, oc4 * 128:(oc4 + 1) * 128], [wk], oT, "hT", GT)
                P.op(dve, lambda oc=oc, b=b: nc.vector.tensor_tensor(out=xt[:, oc, :], in0=xt[:, oc, :], in1=psf[:, b, :], op=ALU.add),
                     R=[xkey, ("ps", b)], W=[xkey])
        norm_group(xt, xkey, 4 + li)
        wsrc = wb_d[f"ffn_w_in{li}"].rearrange("(c p) n -> p c n", p=128)
        dsrc = wb_d[f"ffn_w_down{li}"].rearrange("(c p) n -> p c n", p=128)
        for fh in range(2):
            for cl in range(11):
                ch = fh * 11 + cl
                i = rot(wbuf, wbuf_i)
                P.dma(wbuf[i][:, :, 0:128], wsrc[:, :, ch * 128:(ch + 1) * 128], R=[("wb", f"ffn_w_in{li}")], W=[("wbuf", i)])
                P.dma(wbuf[i][:, :, 128:256], wsrc[:, :, DFF + ch * 128:DFF + (ch + 1) * 128], R=[("wb", f"ffn_w_in{li}")], W=[("wbuf", i)])
                wgk = ("wbuf", i)
                bg = proj(lambda kc, i=i: wbuf[i][:, kc, 0:128], [wgk], hT, "hT", GT)
                bu = proj(lambda kc, i=i: wbuf[i][:, kc, 128:256], [wgk], hT, "hT", GT)
                si = rot(sil, sil_i)
                P.op(act, lambda bg=bg, si=si: nc.scalar.activation(out=sil[si][:], in_=psf[:, bg, :], func=AF.Silu),
                     R=[("ps", bg)], W=[("sil", si)])
                P.op(dve, lambda cl=cl, bu=bu, si=si: nc.vector.tensor_tensor(out=actT[:, cl, :], in0=psf[:, bu, :], in1=sil[si][:], op=ALU.mult),
                     R=[("ps", bu), ("sil", si)], W=["maskT"])
            for oc in range(8):
                i = rot(wdn, wdn_i)
                P.dma(wdn[i][:], dsrc[:, fh * 11:(fh + 1) * 11, oc * 128:(oc + 1) * 128], R=[("wb", f"ffn_w_down{li}")], W=[("wdn", i)])
                b = bankL()
                for kc in range(11):
                    P.op(pe, lambda kc=kc, i=i, b=b: nc.tensor.matmul(psf[:, b, :], lhsT=wdn[i][:, kc, :], rhs=actT[:, kc, :],
                                                                     start=(kc == 0), stop=(kc == 10)),
                         R=[("wdn", i), "maskT"], W=[("ps", b)])
                P.op(dve, lambda oc=oc, b=b: nc.vector.tensor_tensor(out=xt[:, oc, :], in0=xt[:, oc, :], in1=psf[:, b, :], op=ALU.add),
                     R=[xkey, ("ps", b)], W=[xkey])

    def final_store(s, g, xt, xkey, last):
        if last:
            b = bank()
            for c in range(8):
                sumsq_mm(b, xt[:, c, :], xkey, GT, c == 0, c == 7)
            P.op(act, lambda: nc.scalar.activation(out=rstd[:], in_=psf[:, b, :], func=AF.Sqrt, scale=1.0 / D, bias=epsT[:, 0:1]),
                 R=[("ps", b)], W=["rstd"])
            P.op(dve, lambda: nc.vector.reciprocal(out=rstd[:], in_=rstd[:]), R=["rstd"], W=["rstd"])
            for c in range(8):
                P.op(dve, lambda c=c: nc.vector.scalar_tensor_tensor(out=xt[:, c, :], in0=xt[:, c, :], scalar=gam[:, 6, c:c + 1],
                                                                      in1=rstd[:], op0=ALU.mult, op1=ALU.mult),
                     R=[xkey, "rstd", "gam"], W=[xkey])
            dst = outT_d[s].rearrange("(c p) t -> p c t", p=128)
            P.dma(dst[:, :, g * GT:(g + 1) * GT], xt[:], R=[xkey], W=["out"])
        else:
            dst = xs_d[s].rearrange("(c p) t -> p c t", p=128)
            P.dma(dst[:, :, g * GT:(g + 1) * GT], xt[:], R=[xkey], W=[("xs", s)])

    def dsa_layer(s, li, src_d, last):
        barrier()
        mem_kv(s, li)
        wt, wk = load_w("dsa_w_in", 896, 16 + 0)
        P.op(pool, lambda: nc.gpsimd.memset(wkr2[:], 0.0), W=["wkr2"])
        P.op(pool, lambda: nc.gpsimd.tensor_copy(out=wkr2[:, :, 0:16], in_=wt[:, :, 0:16]), R=[wk], W=["wkr2"])
        P.op(pool, lambda: nc.gpsimd.tensor_copy(out=wkr2[:, :, 64:80], in_=wt[:, :, 0:16]), R=[wk], W=["wkr2"])
        wt2, wk2 = load_w("dsa_w_in", 1424, 64)
        P.op(pool, lambda: nc.gpsimd.tensor_copy(out=wki2[:, :, 0:64], in_=wt2[:, :, 0:64]), R=[wk2], W=["wki2"])
        P.op(pool, lambda: nc.gpsimd.tensor_copy(out=wki2[:, :, 64:128], in_=wt2[:, :, 0:64]), R=[wk2], W=["wki2"])
        P.op(pool, lambda: nc.gpsimd.memset(V1[:, :, 0:64], 1.0), W=["V1"])
        for g in range(NG):
            xt, xkey = load_x(src_d, s, g)
            load_rope(g)
            norm_group(xt, xkey, 0 + li)
            wc, wck = load_w("dsa_w_in", 768, 128)
            b = proj(lambda kc: wc[:, kc, 0:128], [wck], hT, "hT", GT)
            evac(b, ckvf[:], "ckvf", eng=dve)
            b2 = bank()
            sumsq_mm(b2, ckvf[:], "ckvf", GT, True, True)
            P.op(act, lambda: nc.scalar.activation(out=rstd[:], in_=psf[:, b2, :], func=AF.Sqrt, scale=1.0 / 128, bias=epsT[:, 0:1]),
                 R=[("ps", b2)], W=["rstd"])
            P.op(dve, lambda: nc.vector.reciprocal(out=rstd[:], in_=rstd[:]), R=["rstd"], W=["rstd"])
            P.op(dve, lambda: nc.vector.scalar_tensor_tensor(out=ckvn[:], in0=ckvf[:], scalar=ckvg[:, 0:1], in1=rstd[:],
                                                             op0=ALU.mult, op1=ALU.mult),
                 R=["ckvf", "rstd", "ckvg"], W=["ckvn"])
            bk = bank()
            for kc in range(8):
                P.op(pe, lambda kc=kc: nc.tensor.matmul(psf[:, bk, :], lhsT=wkr2[:, kc, :], rhs=hT[:, kc, :], start=(kc == 0), stop=False),
                     R=["wkr2", "hT"], W=[("ps", bk)])
            P.op(pe, lambda: nc.tensor.matmul(psf[:, bk, :], lhsT=wuk2[:], rhs=ckvn[:], start=False, stop=True),
                 R=["wuk2", "ckvn"], W=[("ps", bk)])
            rope_from_bank(bk, KT2[:, g * GT:(g + 1) * GT], "KT2")
            bi = proj(lambda kc: wki2[:, kc, :], ["wki2"], hT, "hT", GT)
            rope_from_bank(bi, kidxT2[:, g * GT:(g + 1) * GT], "kidxT2")
            for tt in range(4):
                bv = bank()
                P.op(pe, lambda tt=tt: nc.tensor.matmul(psf[:, bv, 0:64], lhsT=ckvn[:, tt * 128:(tt + 1) * 128], rhs=wuv[:], start=True, stop=True),
                     R=["ckvn", "wuv"], W=[("ps", bv)])
                P.op(act, lambda tt=tt, bv=bv: nc.scalar.copy(out=V1[:, g * 4 + tt, 64:128], in_=psf[:, bv, 0:64]), R=[("ps", bv)], W=["V1"])
        for g in range(NG):
            xt, xkey = load_x(src_d, s, g)
            load_rope(g)
            norm_group(xt, xkey, 0 + li)
            wq, wqk = load_w("dsa_w_in", 0, 512)
            for c in range(4):
                b = proj(lambda kc, c=c: wq[:, kc, c * 128:(c + 1) * 128], [wqk], hT, "hT", GT)
                rope_from_bank(b, qT[:, c, :], "qT")
            wq2, wq2k = load_w("dsa_w_in", 512, 256)
            for c in range(2):
                b = proj(lambda kc, c=c: wq2[:, kc, c * 128:(c + 1) * 128], [wq2k], hT, "hT", GT)
                rope_from_bank(b, qT[:, 4 + c, :], "qT")
            wi, wik = load_w("dsa_w_in", 912, 512)
            for c in range(4):
                b = proj(lambda kc, c=c: wi[:, kc, c * 128:(c + 1) * 128], [wik], hT, "hT", GT)
                rope_from_bank(b, qidxT[:, c, :], "qidxT")
            wm_, wmk = load_w("dsa_w_in", 1488, 264)
            for c in range(2):
                b = proj(lambda kc, c=c: wm_[:, kc, 8 + c * 128:8 + (c + 1) * 128], [wmk], hT, "hT", GT)
                evac(b, qmemT[:, c, :], "qmemT")
            for tt in range(4):
                b = bank()
                for kc in range(8):
                    P.op(pe, lambda kc=kc, tt=tt: nc.tensor.matmul(psf[:, b, 0:8], lhsT=hT[:, kc, tt * 128:(tt + 1) * 128], rhs=wm_[:, kc, 0:8],
                                                                   start=(kc == 0), stop=(kc == 7)),
                         R=["hT", wmk], W=[("ps", b)])
                P.op(act, lambda tt=tt, b=b: nc.scalar.mul(widx[:, tt, :], psf[:, b, 0:8], float(8 ** -0.5 * 64 ** -0.5)),
                     R=[("ps", b)], W=["widx"])
            P.op(pool, lambda: nc.gpsimd.memset(maskT[:], 0.0), W=["maskT"])
            for tt in range(4):
                qi = g * 4 + tt
                Sc = 128 * (qi + 1)
                di = rot(Dh, Dh_i)
                for h in range(8):
                    P.op(dve, lambda h=h, di=di, tt=tt: nc.vector.tensor_scalar(out=Dh[di][:, h, :], in0=ident_b, scalar1=widx[:, tt, h:h + 1],
                                                                                scalar2=None, op0=ALU.mult),
                         R=["misc_b", "widx"], W=[("Dh", di)])
                si = rot(sc, sc_i)
                for k0 in range(0, Sc, 512):
                    kn = min(512, Sc - k0)
                    ba = bankL()
                    for h in range(8):
                        c, base = h // 2, (h % 2) * 64
                        bl = bank()
                        P.op(pe, lambda c=c, base=base, bl=bl, k0=k0, kn=kn: nc.tensor.matmul(
                            psf[:, bl, 0:kn], lhsT=qidxT[base:base + 64, c, tt * 128:(tt + 1) * 128],
                            rhs=kidxT2[base:base + 64, k0:k0 + kn], start=True, stop=True),
                             R=["qidxT", "kidxT2"], W=[("ps", bl)])
                        ri = rot(relu_t, relu_i)
                        P.op(act, lambda bl=bl, ri=ri, kn=kn: nc.scalar.activation(out=relu_t[ri][:, 0:kn], in_=psf[:, bl, 0:kn], func=AF.Relu),
                             R=[("ps", bl)], W=[("relu", ri)])
                        P.op(pe, lambda h=h, ri=ri, ba=ba, kn=kn, di=di: nc.tensor.matmul(
                            psf[:, ba, 0:kn], lhsT=Dh[di][:, h, :], rhs=relu_t[ri][:, 0:kn], start=(h == 0), stop=(h == 7)),
                             R=[("Dh", di), ("relu", ri)], W=[("ps", ba)])
                    P.op(act, lambda ba=ba, si=si, k0=k0, kn=kn: nc.scalar.copy(out=sc[si][:, k0:k0 + kn], in_=psf[:, ba, 0:kn]),
                         R=[("ps", ba)], W=[("sc", si)])
                scv = sc[si]
                skey = ("sc", si)
                if qi >= 2:
                    P.op(dve, lambda: nc.vector.tensor_reduce(out=bst[:, 0:1], in_=scv[:, 0:Sc], axis=AX.X, op=ALU.min), R=[skey], W=["bst0"])
                    P.op(dve, lambda: nc.vector.tensor_reduce(out=bst[:, 1:2], in_=scv[:, 0:Sc], axis=AX.X, op=ALU.max), R=[skey], W=["bst1"])
                P.op(dve, lambda: nc.vector.tensor_tensor(out=scv[:, Sc - 128:Sc], in0=scv[:, Sc - 128:Sc], in1=cbias_f, op=ALU.add),
                     R=[skey, "misc_f", "bst0", "bst1"], W=[skey])
                mi = rot(mask_tm, mask_i)
                if qi >= 2:
                    NIT = 20
                    P.op(dve, lambda: nc.vector.tensor_tensor(out=bst[:, 2:3], in0=bst[:, 1:2], in1=bst[:, 0:1], op=ALU.subtract),
                         R=["bst0", "bst1"], W=["bst2"])
                    P.op(dve, lambda: nc.vector.tensor_scalar(out=wtab[:], in0=bis[:], scalar1=bst[:, 2:3], scalar2=None, op0=ALU.mult),
                         R=["bis", "bst2"], W=["wtab"])
                    P.op(dve, lambda: nc.vector.tensor_tensor(out=bst[:, 3:4], in0=bst[:, 0:1], in1=wtab[:, 0:1], op=ALU.add),
                         R=["bst0", "wtab"], W=["bst3"])
                    for it in range(NIT):
                        P.op(dve, lambda: nc.vector.tensor_scalar(out=mask_tm[mi][:, 0:Sc], in0=scv[:, 0:Sc], scalar1=bst[:, 3:4], scalar2=0.0,
                                                                  op0=ALU.is_ge, op1=ALU.add, accum_out=bst[:, 4:5]),
                             R=[skey, "bst3"], W=[("masktm", mi), "bst4"])
                        if it < NIT - 1:
                            wn = wtab[:, it + 1:it + 2]
                            P.op(dve, lambda wn=wn: nc.vector.tensor_scalar(out=bst[:, 5:6], in0=bst[:, 4:5], scalar1=255.5, scalar2=wn,
                                                                            op0=ALU.is_ge, op1=ALU.mult),
                                 R=["bst4", "wtab"], W=["bst5"])
                            P.op(dve, lambda wn=wn: nc.vector.tensor_scalar(out=bst[:, 5:6], in0=bst[:, 5:6], scalar1=2.0, scalar2=wn,
                                                                            op0=ALU.mult, op1=ALU.subtract),
                                 R=["bst5", "wtab"], W=["bst5"])
                            P.op(dve, lambda: nc.vector.tensor_tensor(out=bst[:, 3:4], in0=bst[:, 3:4], in1=bst[:, 5:6], op=ALU.add),
                                 R=["bst3", "bst5"], W=["bst3"])
                        else:
                            wl = wtab[:, it:it + 1]
                            P.op(dve, lambda wl=wl: nc.vector.tensor_scalar(out=bst[:, 5:6], in0=bst[:, 4:5], scalar1=255.5, scalar2=1.0,
                                                                            op0=ALU.is_ge, op1=ALU.subtract),
                                 R=["bst4"], W=["bst5"])
                            P.op(dve, lambda wl=wl: nc.vector.scalar_tensor_tensor(out=bst[:, 6:7], in0=bst[:, 5:6], scalar=wl, in1=bst[:, 3:4],
                                                                                   op0=ALU.mult, op1=ALU.add),
                                 R=["bst5", "bst3", "wtab"], W=["bst6"])
                    P.op(dve, lambda: nc.vector.tensor_scalar(out=mask_tm[mi][:, 0:Sc], in0=scv[:, 0:Sc], scalar1=bst[:, 6:7], scalar2=None, op0=ALU.is_ge),
                         R=[skey, "bst6"], W=[("masktm", mi)])
                else:
                    P.op(dve, lambda: nc.vector.tensor_scalar(out=mask_tm[mi][:, 0:Sc], in0=scv[:, 0:Sc], scalar1=-1.0e29, scalar2=None, op0=ALU.is_ge),
                         R=[skey], W=[("masktm", mi)])
                nblk = qi + 1
                for j0 in range(0, nblk, 8):
                    jn = min(8, nblk - j0)
                    for j in range(j0, j0 + jn):
                        P.op(pe, lambda j=j, j0=j0, mi=mi: nc.tensor.transpose(psb[:, (j - j0) * 128:(j - j0 + 1) * 128],
                                                                              mask_tm[mi][:, j * 128:(j + 1) * 128], ident_b),
                             R=[("masktm", mi), "misc_b"], W=["psb"])
                    P.op(act, lambda j0=j0, jn=jn, tt=tt: nc.scalar.copy(out=maskT[:, j0:j0 + jn, tt * 128:(tt + 1) * 128],
                                                                          in_=psb[:, 0:jn * 128].rearrange("p (j t) -> p j t", j=jn)),
                         R=["psb"], W=["maskT"])
            nkb = 4 * g + 4
            for h in range(12):
                c, base = h // 2, (h % 2) * 64
                blocks = [dict(kT=KT2[base:base + 64, j * 128:(j + 1) * 128], kkey="KT2", v1=V1[:, j, :], vkey="V1",
                               mask=(maskT[:, j, :], "maskT")) for j in range(nkb)]
                attn_head(qT[base:base + 64, c, :], "qT", blocks, plain_dst(c, base))
            mem_attn()
            wo_ffn(s, li, g, xt, xkey, last)
            final_store(s, g, xt, xkey, last)

    def nsa_layer(s, li, src_d, last):
        barrier()
        mem_kv(s, li)
        for kv, nm in enumerate(["cmp_k_w1", "cmp_v_w1"]):
            i = rot(wbuf, wbuf_i)
            w1c = wbuf[i][:].rearrange("p a b -> p (a b)")[:, 0:2048].rearrange("p (c m) -> p c m", m=128)
            src2 = wb_d[nm].rearrange("(c p) m -> p c m", p=128)
            P.dma(w1c, src2, R=[("wb", nm)], W=[("wbuf", i)])
            b = bank()
            for c in range(16):
                P.op(pe, lambda c=c, kv=kv, w1c=w1c, b=b: nc.tensor.matmul(psf[:, b, 0:1], lhsT=w1c[:, c, :], rhs=posb[:, kv, c:c + 1],
                                                                          start=(c == 0), stop=(c == 15)),
                     R=[("wbuf", i), "posb"], W=[("ps", b)])
            P.op(act, lambda kv=kv, b=b: nc.scalar.copy(out=cb_b[:, kv:kv + 1], in_=psf[:, b, 0:1]), R=[("ps", b)], W=["cb_b"])
        if STOP[0] == 1:
            raise StopBuild()
        P.op(pool, lambda: nc.gpsimd.memset(V1s[:, :, :, 0:64], 1.0), W=["V1s"])
        P.op(pool, lambda: nc.gpsimd.memset(V1w[:, :, :, 0:64], 1.0), W=["V1w"])
        for g in range(NG):
            xt, xkey = load_x(src_d, s, g)
            load_rope(g)
            norm_group(xt, xkey, 0 + li)
            wk_, wkk = load_w("nsa_w_in", 768, 512)
            wk2_, wk2k = load_w("nsa_w_in", 1280, 256)
            b = proj(lambda kc: wk_[:, kc, 0:128], [wkk], hT, "hT", GT)
            evac(b, kcT[:, g * GT:(g + 1) * GT], "kcT")
            b = proj(lambda kc: wk_[:, kc, 128:256], [wkk], hT, "hT", GT)
            evac(b, vcT[:, g * GT:(g + 1) * GT], "vcT")
            for src_t, src_k, off, dstT, dkey in ((wk_, wkk, 256, ksT2, "ksT2"), (wk2_, wk2k, 0, kwT2, "kwT2")):
                for grp in range(2):
                    bb = bank()
                    for half in range(2):
                        for kc in range(8):
                            P.op(pe, lambda kc=kc, half=half, grp=grp, src_t=src_t, off=off, bb=bb: nc.tensor.matmul(
                                psf[half * 64:half * 64 + 64, bb, :], lhsT=src_t[:, kc, off + grp * 64:off + grp * 64 + 64], rhs=hT[:, kc, :],
                                start=(kc == 0), stop=(kc == 7)),
                                 R=[src_k, "hT"], W=[("ps", bb)])
                    rope_from_bank(bb, dstT[:, grp, g * GT:(g + 1) * GT], dkey)
            for src_t, src_k, off, dstV, dkey in ((wk_, wkk, 384, V1s, "V1s"), (wk2_, wk2k, 128, V1w, "V1w")):
                for tt in range(4):
                    bv = bank()
                    for kc in range(8):
                        P.op(pe, lambda kc=kc, tt=tt, src_t=src_t, off=off, bv=bv: nc.tensor.matmul(
                            psf[:, bv, 0:128], lhsT=hT[:, kc, tt * 128:(tt + 1) * 128], rhs=src_t[:, kc, off:off + 128],
                            start=(kc == 0), stop=(kc == 7)),
                             R=["hT", src_k], W=[("ps", bv)])
                    P.op(act, lambda tt=tt, bv=bv, dstV=dstV: nc.scalar.copy(out=dstV[:, g * 4 + tt, :, 64:128],
                                                                               in_=psf[:, bv, 0:128].rearrange("p (g d) -> p g d", g=2)),
                         R=[("ps", bv)], W=[dkey])
        if STOP[0] == 2:
            raise StopBuild()
        P.op(pool, lambda: nc.gpsimd.memset(V1c[:, :, 0:64], 1.0), W=["V1c"])
        for kv, (srcT, skey) in enumerate(((kcT, "kcT"), (vcT, "vcT"))):
            nm = ["cmp_k_w1", "cmp_v_w1"][kv]
            wi_ = rot(wbuf, wbuf_i)
            w1dup = wbuf[wi_][:].rearrange("p a b -> p (a b)").rearrange("p (l m) -> p l m", m=128)
            srcw = wb_d[nm].rearrange("(l d) m -> d l m", d=64)
            P.dma(w1dup[0:64, :, :], srcw, R=[("wb", nm)], W=[("wbuf", wi_)])
            P.dma(w1dup[64:128, :, :], srcw, R=[("wb", nm)], W=[("wbuf", wi_)])
            for grp in range(2):
                base = grp * 64
                b = bank()
                for l in range(32):
                    P.op(pe, lambda l=l, kv=kv, base=base, srcT=srcT, b=b, w1dup=w1dup: nc.tensor.matmul(
                        psf[:, b, 0:127], lhsT=w1dup[base:base + 64, l, :], rhs=srcT[base:base + 64, l:l + 16 * 126 + 1:16],
                        start=(l == 0), stop=(l == 31)),
                         R=[("wbuf", wi_), skey], W=[("ps", b)])
                P.op(act, lambda kv=kv, b=b: nc.scalar.activation(out=gel[0][:, 0:127], in_=psf[:, b, 0:127], func=AF.Identity,
                                                                  bias=cb_b[:, kv:kv + 1], scale=1.0),
                     R=[("ps", b), "cb_b"], W=["gel0"])
                P.op(act, lambda: nc.scalar.activation(out=gel[1][:, 0:127], in_=gel[0][:, 0:127], func=AF.Square), R=["gel0"], W=["gel1"])
                P.op(dve, lambda: nc.vector.tensor_scalar(out=gel[1][:, 0:127], in0=gel[1][:, 0:127], scalar1=0.044715, scalar2=1.0,
                                                          op0=ALU.mult, op1=ALU.add), R=["gel1"], W=["gel1"])
                P.op(dve, lambda: nc.vector.tensor_tensor(out=gel[2][:, 0:127], in0=gel[1][:, 0:127], in1=gel[0][:, 0:127], op=ALU.mult),
                     R=["gel1", "gel0"], W=["gel2"])
                P.op(act, lambda: nc.scalar.activation(out=gel[3][:, 0:127], in_=gel[2][:, 0:127], func=AF.Sigmoid, scale=1.5957691216),
                     R=["gel2"], W=["gel3"])
                P.op(pool, lambda: nc.gpsimd.memset(gT[:], 0.0), W=["gT"])
                P.op(dve, lambda: nc.vector.tensor_tensor(out=gT[:, 0:127], in0=gel[3][:, 0:127], in1=gel[0][:, 0:127], op=ALU.mult),
                     R=["gel3", "gel0"], W=["gT"])
                b2 = bank()
                if kv == 0:
                    P.op(pe, lambda: nc.tensor.matmul(psf[:, b2, 0:128], lhsT=w2k2[:], rhs=gT[:], start=True, stop=True),
                         R=["w2k2", "gT"], W=[("ps", b2)])
                    evac(b2, kcmpT2[:, grp, :], "kcmpT2", ncols=128)
                else:
                    P.op(pe, lambda: nc.tensor.matmul(psf[:, b2, 0:64], lhsT=gT[:], rhs=w2v[:], start=True, stop=True),
                         R=["w2v", "gT"], W=[("ps", b2)])
                    evac(b2, V1c[:, grp, 64:128], "V1c", ncols=64)
        if STOP[0] == 3:
            raise StopBuild()
        barrier()
        if STOP[0] == 40:
            raise StopBuild()
        for g in range(NG):
            xt, xkey = load_x(src_d, s, g)
            if STOP[0] == 401:
                raise StopBuild()
            load_rope(g)
            if STOP[0] == 402:
                raise StopBuild()
            norm_group(xt, xkey, 0 + li)
            if STOP[0] == 41:
                raise StopBuild()
            wq, wqk = load_w("nsa_w_in", 0, 512)
            wq2, wq2k = load_w("nsa_w_in", 512, 256)
            for c in range(6):
                w_, wk_ = (wq, wqk) if c < 4 else (wq2, wq2k)
                cc = c if c < 4 else c - 4
                b = proj(lambda kc, cc=cc, w_=w_: w_[:, kc, cc * 128:(cc + 1) * 128], [wk_], hT, "hT", GT)
                rope_from_bank(b, qT[:, c, :], "qT", raw=(qrawT[:, c, :], "qrawT"))
            if STOP[0] == 42:
                raise StopBuild()
            wm_, wmk = load_w("nsa_w_in", 1536, 292)
            for c in range(2):
                b = proj(lambda kc, c=c: wm_[:, kc, 36 + c * 128:36 + (c + 1) * 128], [wmk], hT, "hT", GT)
                evac(b, qmemT[:, c, :], "qmemT")
            if STOP[0] == 43:
                raise StopBuild()
            b = proj(lambda kc: wm_[:, kc, 0:64], [wmk], hT, "hT", GT, M=64)
            P.op(act, lambda b=b: nc.scalar.activation(out=sigT[:], in_=psf[0:36, b, :], func=AF.Sigmoid), R=[("ps", b)], W=["sigT"])
            P.op(dve, lambda: nc.vector.tensor_copy(out=sigH[:], in_=sigT[:]), R=["sigT"], W=["sigH"])
            P.op(dve, lambda: nc.vector.tensor_tensor(out=sigL[:], in0=sigT[:], in1=sigH[:], op=ALU.subtract), R=["sigT", "sigH"], W=["sigL"])
            if STOP[0] == 4:
                raise StopBuild()
            mcg = mc_b[:, g * GT:(g + 1) * GT]
            P.op(pool, lambda: nc.gpsimd.memset(imp[:], 0.0), W=["imp"])

            def gated_dst(h, br, add_ap, add_key, out_ap, out_key):
                def f(bo, rden, rkey):
                    bgate = bank()
                    kk = h * 3 + br
                    P.op(pe, lambda: nc.tensor.matmul(psf[0:64, bgate, :], lhsT=E_flat[0:36, kk * 64:(kk + 1) * 64], rhs=sigH[:], start=True, stop=False),
                         R=["E_b", "sigH"], W=[("ps", bgate)])
                    P.op(pe, lambda: nc.tensor.matmul(psf[0:64, bgate, :], lhsT=E_flat[0:36, kk * 64:(kk + 1) * 64], rhs=sigL[:], start=False, stop=True),
                         R=["E_b", "sigL"], W=[("ps", bgate)])
                    gi = rot(rg, rg_i)
                    P.op(dve, lambda: nc.vector.tensor_tensor(out=rg[gi][:], in0=psf[0:64, bgate, :], in1=rden, op=ALU.mult),
                         R=[("ps", bgate), rkey], W=[("rg", gi)])
                    base = (h % 2) * 64
                    if add_ap is None:
                        P.op(dve, lambda: nc.vector.tensor_tensor(out=out_ap, in0=psf[64:128, bo, :], in1=rg[gi][:], op=ALU.mult),
                             R=[("ps", bo), ("rg", gi)], W=[out_key])
                    else:
                        oi = rot(otmp, otmp_i)
                        P.op(dve, lambda: nc.vector.tensor_tensor(out=otmp[oi][base:base + 64, :], in0=psf[64:128, bo, :], in1=rg[gi][:], op=ALU.mult),
                             R=[("ps", bo), ("rg", gi)], W=[("otmp", oi)])
                        P.op(pool, lambda: nc.gpsimd.tensor_tensor(out=out_ap, in0=otmp[oi][base:base + 64, :], in1=add_ap, op=ALU.add),
                             R=[("otmp", oi), add_key], W=[out_key])
                return f

            for grp in range(2):
                heads = list(range(grp * 6, grp * 6 + 6))
                for h in heads:
                    c, base = h // 2, (h % 2) * 64
                    bs = bank()
                    P.op(pe, lambda c=c, base=base, bs=bs: nc.tensor.matmul(psf[:, bs, :], lhsT=kcmpT2[base:base + 64, grp, :],
                                                                           rhs=qrawT[base:base + 64, c, :], start=True, stop=True),
                         R=["kcmpT2", "qrawT"], W=[("ps", bs)])
                    pi = rot(PT, PT_i)
                    P.op(act, lambda bs=bs, pi=pi: nc.scalar.activation(out=PT[pi][:, 0, :], in_=psf[:, bs, :], func=AF.Exp, scale=0.125),
                         R=[("ps", bs)], W=[("PT", pi)])
                    P.op(dve, lambda pi=pi: nc.vector.tensor_tensor(out=PT[pi][:, 0, :], in0=PT[pi][:, 0, :], in1=mcg, op=ALU.mult),
                         R=[("PT", pi), "mc_b"], W=[("PT", pi)])
                    for tt in range(4):
                        bi_ = bank()
                        P.op(pe, lambda tt=tt, pi=pi, bi_=bi_: nc.tensor.matmul(psf[:, bi_, 0:33], lhsT=PT[pi][:, 0, tt * 128:(tt + 1) * 128],
                                                                               rhs=ov_b[:], start=True, stop=True),
                             R=[("PT", pi), "ov_b"], W=[("ps", bi_)])
                        P.op(dve, lambda bi_=bi_, tt=tt: nc.vector.tensor_scalar_max(out=rec[:, tt:tt + 1], in0=psf[:, bi_, 32:33], scalar1=1e-30),
                             R=[("ps", bi_)], W=[("rec", tt)])
                        P.op(dve, lambda tt=tt: nc.vector.reciprocal(out=rec[:, tt:tt + 1], in_=rec[:, tt:tt + 1]), R=[("rec", tt)], W=[("rec", tt)])
                        P.op(dve, lambda bi_=bi_, tt=tt: nc.vector.scalar_tensor_tensor(out=imp[:, tt, grp, :], in0=psf[:, bi_, 0:32],
                                                                                        scalar=rec[:, tt:tt + 1], in1=imp[:, tt, grp, :],
                                                                                        op0=ALU.mult, op1=ALU.add),
                             R=[("ps", bi_), ("rec", tt), "imp"], W=["imp"])
                    bo = bankL()
                    P.op(pe, lambda pi=pi, bo=bo: nc.tensor.matmul(psf[:, bo, :], lhsT=V1c[:, grp, :], rhs=PT[pi][:, 0, :], start=True, stop=True),
                         R=["V1c", ("PT", pi)], W=[("ps", bo)])
                    ri = rot(rd, rd_i)
                    P.op(dve, lambda bo=bo, ri=ri: nc.vector.tensor_scalar_max(out=rd[ri][0:64, :], in0=psf[0:64, bo, :], scalar1=1e-30),
                         R=[("ps", bo)], W=[("rd", ri)])
                    P.op(dve, lambda ri=ri: nc.vector.reciprocal(out=rd[ri][0:64, :], in_=rd[ri][0:64, :]), R=[("rd", ri)], W=[("rd", ri)])
                    hl = h - grp * 6
                    gated_dst(h, 0, None, None, cmpo[base:base + 64, hl, :], "cmpo")(bo, rd[ri][0:64, :], ("rd", ri))
                for tt in range(4):
                    qi = g * 4 + tt
                    P.op(dve, lambda tt=tt, qi=qi: nc.vector.tensor_tensor(out=imp2[:, tt, grp, :], in0=imp[:, tt, grp, :], in1=selF[:, 30 - 2 * qi:62 - 2 * qi], op=ALU.max),
                         R=["imp", "selF"], W=["imp2"])
                    P.op(dve, lambda tt=tt, qi=qi: nc.vector.tensor_tensor(out=imp2[:, tt, grp, :], in0=imp2[:, tt, grp, :], in1=selA[:, 30 - 2 * qi:62 - 2 * qi], op=ALU.add),
                         R=["imp2", "selA"], W=["imp2"])
                    P.op(dve, lambda tt=tt: nc.vector.memset(imp2[:, tt, grp, 0:1], 1.0e9), R=["imp2"], W=["imp2"])
                    P.op(dve, lambda tt=tt: nc.vector.max(out=m8[:, 0:8], in_=imp2[:, tt, grp, :]), R=["imp2"], W=["m8a"])
                    P.op(dve, lambda tt=tt: nc.vector.match_replace(out=imp3[:, tt, grp, :], in_to_replace=m8[:, 0:8], in_values=imp2[:, tt, grp, :],
                                                                    imm_value=-3.0e38),
                         R=["imp2", "m8a"], W=["imp3"])
                    P.op(dve, lambda tt=tt: nc.vector.max(out=m8[:, 8:16], in_=imp3[:, tt, grp, :]), R=["imp3"], W=["m8b"])
                    P.op(dve, lambda tt=tt: nc.vector.tensor_scalar(out=selm[:, tt, grp * 32:(grp + 1) * 32], in0=imp2[:, tt, grp, :],
                                                                    scalar1=m8[:, 15:16], scalar2=None, op0=ALU.is_ge),
                         R=["imp2", "m8b"], W=["selm"])
                if STOP[0] == 5:
                    raise StopBuild()
                if grp == 0:
                    P.op(pool, lambda: nc.gpsimd.memset(selm[:, :, 32:64], 0.0), W=["selm"])
                for tt in range(4):
                    P.op(pe, lambda tt=tt: nc.tensor.transpose(psb[0:64, tt * 128:(tt + 1) * 128], selm[:, tt, :], ident_b),
                         R=["selm", "misc_b"], W=["psb"])
                P.op(act, lambda: nc.scalar.copy(out=selT[:], in_=psb[0:64, 0:512]), R=["psb"], W=["selT"])
                nkb = 4 * g + 4
                for j in range(nkb):
                    bm = bank()
                    P.op(pe, lambda j=j, bm=bm: nc.tensor.matmul(psf[:, bm, :], lhsT=E_b[:, grp, j * 128:(j + 1) * 128], rhs=selT[:], start=True, stop=True),
                         R=["E_b", "selT"], W=[("ps", bm)])
                    if j >= 4 * g:
                        P.op(dve, lambda j=j, bm=bm: nc.vector.tensor_tensor(out=maskT[:, j, :], in0=psf[:, bm, :], in1=cm_b[:, j - 4 * g, :], op=ALU.mult),
                             R=[("ps", bm), "cm_b"], W=["maskT"])
                    else:
                        evac(bm, maskT[:, j, :], "maskT")
                if STOP[0] == 6:
                    raise StopBuild()
                for h in heads:
                    c, base = h // 2, (h % 2) * 64
                    hl = h - grp * 6
                    blocks = [dict(kT=ksT2[base:base + 64, grp, j * 128:(j + 1) * 128], kkey="ksT2", v1=V1s[:, j, grp, :], vkey="V1s",
                                   mask=(maskT[:, j, :], "maskT")) for j in range(nkb)]
                    attn_head(qT[base:base + 64, c, :], "qT", blocks, gated_dst(h, 1, cmpo[base:base + 64, hl, :], "cmpo", oacc[base:base + 64, :], "oacc"))
                    j0 = max(0, 4 * g - 4)
                    order = [4 * g] + [j for j in range(j0, nkb) if j != 4 * g]
                    blocks = [dict(kT=kwT2[base:base + 64, grp, j * 128:(j + 1) * 128], kkey="kwT2", v1=V1w[:, j, grp, :], vkey="V1w",
                                   mask=(wm_b[:, j - 4 * g + 4, :], "wm_b")) for j in order]
                    attn_head(qT[base:base + 64, c, :], "qT", blocks, gated_dst(h, 2, oacc[base:base + 64, :], "oacc", oT[base:base + 64, c, :], "hT"))
            mem_attn()
            wo_ffn(s, li, g, xt, xkey, last)
            final_store(s, g, xt, xkey, last)

    try:
        for s in range(n_seq):
            for li in range(n_layers):
                src_d = xT_d if li == 0 else xs_d
                last = (li == n_layers - 1)
                if li % 2 == 0:
                    dsa_layer(s, li, src_d, last)
                else:
                    nsa_layer(s, li, src_d, last)
    except StopBuild:
        pass
    barrier()
    P.drain()
    return nc, P


def host_inputs(inputs, core, n_seq=SEQ_PER_CORE):
    x = inputs["x"][core * n_seq:(core + 1) * n_seq]
    mem = inputs["mem"][core * n_seq:(core + 1) * n_seq]
    m = {}
    m["xT"] = np.ascontiguousarray(np.transpose(x, (0, 2, 1)))
    m["memT"] = np.ascontiguousarray(np.transpose(mem, (0, 2, 1)))
    g = np.concatenate([inputs["attn_norm"], inputs["mem_norm"], inputs["ffn_norm"], inputs["final_norm"][None]], axis=0)
    m["gam"] = np.ascontiguousarray(g.reshape(7, 8, 128).transpose(2, 0, 1))
    m["ckvg"] = np.ascontiguousarray(inputs["dsa_ckv_norm"][0].reshape(128, 1))
    m["posk"] = np.ascontiguousarray(inputs["nsa_cmp_pos_k"][0].reshape(16, 128).T)
    m["posv"] = np.ascontiguousarray(inputs["nsa_cmp_pos_v"][0].reshape(16, 128).T)
    m["dsa_w_in"] = inputs["dsa_w_in"][0]
    m["nsa_w_in"] = inputs["nsa_w_in"][0]
    for i in range(2):
        m[f"mem_w_kv{i}"] = inputs["mem_w_kv"][i]
        m[f"w_o{i}"] = inputs["w_o"][i]
        m[f"ffn_w_in{i}"] = inputs["ffn_w_in"][i]
        m[f"ffn_w_down{i}"] = inputs["ffn_w_down"][i]
    m["cmp_k_w1"] = inputs["nsa_cmp_k_w1"][0]
    m["cmp_v_w1"] = inputs["nsa_cmp_v_w1"][0]
    m["dsa_w_uk"] = inputs["dsa_w_uk"][0]
    m["dsa_w_uv"] = inputs["dsa_w_uv"][0]
    m["cmp_k_w2"] = inputs["nsa_cmp_k_w2"][0]
    m["cmp_v_w2"] = inputs["nsa_cmp_v_w2"][0]
    for k, v in make_consts().items():
        m["c_" + k] = v
    return {k: np.ascontiguousarray(v, dtype=np.float32) for k, v in m.items()}


def build2(n_seq, n_layers=2):
    _, P1 = build(n_seq, n_layers)
    return build(n_seq, n_layers, needed=P1.waited)


def kernel(**inputs):
    inputs = {k: np.asarray(v) for k, v in inputs.items()}
    nc, _ = build2(SEQ_PER_CORE)
    in_maps = [host_inputs(inputs, c) for c in range(NCORES)]
    res = run_bass_kernel_spmd(nc, in_maps, core_ids=list(range(NCORES)))
    outs = [np.transpose(r["outT"], (0, 2, 1)) for r in res.results]
    return np.ascontiguousarray(np.concatenate(outs, axis=0)).astype(np.float32)
```

```python
import numpy as np
import concourse.bass as bass
import concourse.mybir as mybir
from concourse.bass_utils import run_bass_kernel_spmd

F32 = mybir.dt.float32
BF16 = mybir.dt.bfloat16
AF = mybir.ActivationFunctionType
ALU = mybir.AluOpType
AX = mybir.AxisListType

T = 2048
NG = 4
GT = 512
D = 1024
DFF = 2816
NCORES = 8
SEQ_PER_CORE = 4
EPS = 1e-6
SEM_L = 8000
N_DSEM = 24
NEG = -1.0e30

DSA_W = 1752
NSA_W = 1828


class Eng:
    def __init__(self, P, name, b, pe=False):
        self.P, self.name, self.b, self.pe = P, name, b, pe
        self.cnt = 0
        self.rank = 0
        self.rankmap = {}
        self.sems = []
        self.seen = {}

    def _sem(self, r):
        e = (r - 1) // SEM_L
        while len(self.sems) <= e:
            self.sems.append(self.P.new_sem(f"{self.name}{len(self.sems)}"))
        return self.sems[e], (r - 1) % SEM_L + 1

    def sem_for(self, c):
        return self._sem(self.rankmap[c])


class DSem:
    def __init__(self, P, i):
        self.sem = P.new_sem(f"dma{i}")
        self.cnt = 0
        self.pe = False
        self.name = f"dma{i}"

    def sem_for(self, c):
        return self.sem, 16 * c


class Prog:
    def __init__(self, nc, needed=None):
        self.nc = nc
        self.nsem = 0
        self.needed = needed
        self.waited = set()
        self.pe = Eng(self, "pe", nc.tensor, pe=True)
        self.act = Eng(self, "act", nc.scalar)
        self.dve = Eng(self, "dve", nc.vector)
        self.pool = Eng(self, "pool", nc.gpsimd)
        self.sp = Eng(self, "sp", nc.sync)
        self.dsems = [DSem(self, i) for i in range(N_DSEM)]
        self.di = 0
        self.R = {}
        self.nins = 0
        self.nwait = 0
        self.ninc = 0

    def new_sem(self, name):
        self.nsem += 1
        return self.nc.semaphore(name).__enter__()

    def _wait(self, eng, obj, c):
        if eng.seen.get(obj, 0) >= c:
            return
        if isinstance(obj, Eng):
            self.waited.add((obj.name, c))
        sem, v = obj.sem_for(c)
        eng.b.wait_ge(sem, v)
        eng.seen[obj] = c
        self.nwait += 1

    def _deps(self, eng, reads, writes, nowaw=False):
        for k in reads:
            r = self.R.get(k)
            if r is None:
                continue
            for obj, c in r[0].items():
                if obj is eng and eng.pe:
                    continue
                self._wait(eng, obj, c)
        for k in writes:
            r = self.R.get(k)
            if r is None:
                continue
            if not nowaw:
                for obj, c in r[0].items():
                    if obj is eng and eng.pe:
                        continue
                    self._wait(eng, obj, c)
            for obj, c in r[1].items():
                if obj is eng and eng.pe:
                    continue
                self._wait(eng, obj, c)

    def _mark(self, obj, c, reads, writes):
        for k in reads:
            r = self.R.get(k)
            if r is None:
                r = self.R[k] = [{}, {}]
            r[1][obj] = c
        for k in writes:
            r = self.R.get(k)
            if r is None or r[1]:
                self.R[k] = [{obj: c}, {}]
            else:
                r[0][obj] = c

    def op(self, eng, fn, R=(), W=()):
        self._deps(eng, R, W)
        ins = fn()
        eng.cnt += 1
        if self.needed is None or (eng.name, eng.cnt) in self.needed:
            eng.rank += 1
            sem, _ = eng._sem(eng.rank)
            ins.then_inc(sem, 1)
            self.ninc += 1
        eng.rankmap[eng.cnt] = eng.rank
        self._mark(eng, eng.cnt, R, W)
        self.nins += 1
        return ins

    def dma(self, out, in_, R=(), W=(), q=None, nowaw=True):
        eng = q or self.sp
        d = self.dsems[self.di % N_DSEM]
        self.di += 1
        if d.cnt > 0:
            self._wait(eng, d, d.cnt)
        self._deps(eng, R, W, nowaw=nowaw)
        ins = eng.b.dma_start(out=out, in_=in_)
        d.cnt += 1
        ins.then_inc(d.sem, 16)
        self._mark(d, d.cnt, R, W)
        self.nins += 1
        return ins

    def drain(self):
        for d in self.dsems:
            if d.cnt > 0:
                self._wait(self.sp, d, d.cnt)


class Ring:
    def __init__(self, items):
        self.items = items
        self.i = 0

    def next(self):
        it = self.items[self.i % len(self.items)]
        self.i += 1
        return it


def _rope_tables():
    inv = 500000.0 ** (-np.arange(0, 16, 2, dtype=np.float64) / 16)
    ang = np.arange(T, dtype=np.float64)[:, None] * inv[None, :]
    cos, sin = np.cos(ang).astype(np.float32), np.sin(ang).astype(np.float32)
    C = np.ones((128, T), np.float32)
    S = np.zeros((128, T), np.float32)
    for r in range(128):
        d = r % 64
        if d < 8:
            C[r] = cos[:, d]
            S[r] = -sin[:, d]
        elif d < 16:
            C[r] = cos[:, d - 8]
            S[r] = sin[:, d - 8]
    return C, S


def make_consts():
    c = {}
    C, S = _rope_tables()
    c["ropeC"], c["ropeS"] = C, S
    perm = np.zeros((128, 128), np.float32)
    for m in range(128):
        d = m % 64
        base = m - d
        if d < 8:
            perm[base + d + 8, m] = 1.0
        elif d < 16:
            perm[base + d - 8, m] = 1.0
    ident = np.eye(128, dtype=np.float32)
    sl = np.arange(128)
    cb = np.where(sl[None, :] > sl[:, None], NEG, 0.0).astype(np.float32)
    c["misc128"] = np.stack([perm, ident, np.ones((128, 128), np.float32), cb], axis=1)
    tl = np.arange(512)
    cm = np.zeros((128, 4, 512), np.float32)
    for r in range(4):
        cm[:, r, :] = ((128 * r + sl)[:, None] <= tl[None, :]).astype(np.float32)
    c["cm"] = cm
    wm = np.zeros((128, 8, 512), np.float32)
    for j in range(8):
        s = 128 * (j - 4) + sl
        wm[:, j, :] = ((s[:, None] <= tl[None, :]) & (s[:, None] > tl[None, :] - 512)).astype(np.float32)
    c["wm"] = wm
    cc = np.arange(128)
    mc = ((16 * cc + 31)[:, None] <= np.arange(T)[None, :]).astype(np.float32)
    mc[127] = 0.0
    c["mcT"] = mc
    c0 = cc * 16
    s0 = np.arange(32) * 64
    ov = np.minimum(c0[:, None] + 32, s0[None, :] + 64) - np.maximum(c0[:, None], s0[None, :])
    ov = (np.clip(ov, 0, None) / 32.0).astype(np.float32)
    ov33 = np.concatenate([ov, np.ones((128, 1), np.float32)], axis=1)
    ov33[127] = 0.0
    c["ov33"] = ov33
    tt = np.arange(T)
    cur = tt // 64
    n = np.arange(32)
    forced = (n[None] == 0) | (n[None] == cur[:, None]) | (n[None] == cur[:, None] - 1)
    adm = n[None] <= cur[:, None]
    Ff = np.where(forced, 1.0e9, 0.0).astype(np.float32)
    Ab = np.where(adm, 0.0, NEG).astype(np.float32)
    pl = (np.arange(128) >= 64).astype(np.int64)
    y = np.arange(62) - 30
    c["selF"] = np.where((y[None] == pl[:, None]) | (y[None] == pl[:, None] - 1), 1.0e9, 0.0).astype(np.float32)
    c["selA"] = np.where(y[None] <= pl[:, None], 0.0, NEG).astype(np.float32)
    E = np.zeros((64, 2, T), np.float32)
    for g in range(2):
        for nn in range(32):
            E[g * 32 + nn, g, nn * 64:(nn + 1) * 64] = 1.0
    c["Eexp"] = E
    c["bis"] = np.tile((2.0 ** -(np.arange(32) + 1)).astype(np.float32)[None, :], (128, 1))
    return c


WSPEC = [
    ("dsa_w_in", 1024, DSA_W), ("nsa_w_in", 1024, NSA_W),
    ("mem_w_kv0", 1024, 512), ("mem_w_kv1", 1024, 512),
    ("w_o0", 1024, 1024), ("w_o1", 1024, 1024),
    ("ffn_w_in0", 1024, 2 * DFF), ("ffn_w_in1", 1024, 2 * DFF),
    ("ffn_w_down0", DFF, 1024), ("ffn_w_down1", DFF, 1024),
    ("cmp_k_w1", 2048, 128), ("cmp_v_w1", 2048, 128),
]
SMALLW = [("dsa_w_uk", 128, 48), ("dsa_w_uv", 128, 64), ("cmp_k_w2", 128, 64), ("cmp_v_w2", 128, 64)]
CONST_SHAPES = None


class StopBuild(Exception):
    pass


STOP = [0]


def build(n_seq, n_layers=2, needed=None):
    nc = bass.Bass("TRN2", target_bir_lowering=False)
    P = Prog(nc, needed)
    pe, act, dve, pool = P.pe, P.act, P.dve, P.pool
    consts = make_consts()

    def din(name, shape, dt=F32):
        return nc.dram_tensor(name, list(shape), dt, kind="ExternalInput").ap()

    xT_d = din("xT", [n_seq, D, T])
    memT_d = din("memT", [n_seq, D, 256])
    gam_d = din("gam", [128, 7, 8])
    ckvg_d = din("ckvg", [128, 1])
    posk_d = din("posk", [128, 16])
    posv_d = din("posv", [128, 16])
    w_d = {n: din(n, [K, N]) for n, K, N in WSPEC}
    sw_d = {n: din(n, [K, N]) for n, K, N in SMALLW}
    c_d = {k: din("c_" + k, v.shape) for k, v in consts.items()}
    outT_d = nc.dram_tensor("outT", [n_seq, D, T], F32, kind="ExternalOutput").ap()
    xs_d = nc.dram_tensor("xs", [n_seq, D, T], F32).ap()
    wb_d = {n: nc.dram_tensor("b_" + n, [K, N], BF16).ap() for n, K, N in WSPEC}

    ARENA_BYTES = 212000
    arena = nc.alloc_sbuf_tensor("arena", [128, ARENA_BYTES // 4], F32)
    a_base = nc.lookup_mloc(arena).addr
    a_ptr = [0]
    a_max = [0]
    DT_SZ = {F32: 4, BF16: 2}

    def sb(name, shape, dt=F32):
        nbytes = int(np.prod(shape[1:])) * DT_SZ[dt]
        off = (a_ptr[0] + 31) // 32 * 32
        a_ptr[0] = off + nbytes
        a_max[0] = max(a_max[0], a_ptr[0])
        assert a_ptr[0] <= ARENA_BYTES, (name, a_ptr[0])
        return nc.alloc_sbuf_tensor_at("s_" + name, list(shape), dt, offset=a_base + off)

    def barrier():
        engs = [P.pe, P.act, P.dve, P.pool, P.sp]
        for e in engs:
            for o in engs:
                if o is not e and o.cnt > 0:
                    P._wait(e, o, o.cnt)
            for d_ in P.dsems:
                if d_.cnt > 0:
                    P._wait(e, d_, d_.cnt)

    xg = [sb("xg0", [128, 8, GT])]
    wbuf = [sb(f"wbuf{i}", [128, 8, 512], BF16) for i in range(2)]
    xg_flat = xg[0][:].rearrange("p a b -> p (a b)")
    stage = [xg_flat[:, i * 2048:(i + 1) * 2048] for i in range(2)]
    stage_b = [wbuf[i][:].rearrange("p a b -> p (a b)")[:, 0:2048] for i in range(2)]
    SCH = 2048
    misc_f = sb("misc_f", [128, 4, 128])
    misc_b = sb("misc_b", [128, 3, 128], BF16)
    cm_b = sb("cm_b", [128, 4, 512], BF16)
    wm_b = sb("wm_b", [128, 8, 512], BF16)
    mc_b = sb("mc_b", [128, T], BF16)
    ov_b = sb("ov_b", [128, 33], BF16)
    selF = sb("selF", [128, 62])
    selA = sb("selA", [128, 62])
    E_b = sb("E_b", [64, 2, T], BF16)
    bis = sb("bis", [128, 32])
    gam = sb("gam", [128, 7, 8])
    ckvg = sb("ckvg", [128, 1])
    cvt_i = [0]
    epsT = sb("epsT", [128, 1])
    P.op(pool, lambda: nc.gpsimd.memset(epsT[:], EPS), W=["epsT"])

    def cvt(dst_ap, src_ap, R, W):
        engs = [dve, pool, act]
        e = engs[cvt_i[0] % 3]
        cvt_i[0] += 1
        if e is act:
            P.op(e, lambda: nc.scalar.copy(out=dst_ap, in_=src_ap), R=R, W=W)
        else:
            P.op(e, lambda: e.b.tensor_copy(out=dst_ap, in_=src_ap), R=R, W=W)

    def load_const(dst, src_d, key, bf=False, npart=128, cols=None):
        if not bf:
            P.dma(dst[:], src_d, W=[key])
            return
        shp = list(src_d.shape)
        flat = int(np.prod(shp[1:]))
        srcf = src_d if len(shp) == 2 else src_d.rearrange("p a b -> p (a b)")
        dstf = dst[:] if len(shp) == 2 else dst[:].rearrange("p a b -> p (a b)")
        for o in range(0, flat, SCH):
            n = min(SCH, flat - o)
            i = cvt_i[0] % 2
            P.dma(stage[i][0:shp[0], 0:n], srcf[:, o:o + n], W=[("stage", i)], nowaw=False)
            cvt(dstf[:, o:o + n], stage[i][0:shp[0], 0:n], R=[("stage", i)], W=[key])

    load_const(misc_f, c_d["misc128"], "misc_f")
    P.op(dve, lambda: nc.vector.tensor_copy(out=misc_b[:], in_=misc_f[:, 0:3, :]), R=["misc_f"], W=["misc_b"])
    load_const(cm_b, c_d["cm"], "cm_b", bf=True)
    load_const(wm_b, c_d["wm"], "wm_b", bf=True)
    load_const(mc_b, c_d["mcT"], "mc_b", bf=True)
    load_const(ov_b, c_d["ov33"], "ov_b", bf=True)
    load_const(selF, c_d["selF"], "selF")
    load_const(selA, c_d["selA"], "selA")
    load_const(E_b, c_d["Eexp"], "E_b", bf=True)
    load_const(bis, c_d["bis"], "bis")
    load_const(gam, gam_d, "gam")
    load_const(ckvg, ckvg_d, "ckvg")
    perm_b, ident_b, ones_b = misc_b[:, 0, :], misc_b[:, 1, :], misc_b[:, 2, :]
    E_flat = E_b[:].rearrange("p g s -> p (g s)")
    ones_f = misc_f[:, 2, :]
    cbias_f = misc_f[:, 3, :]

    for name, K, N in WSPEC:
        for kc in range(K // 128):
            for o in range(0, N, SCH):
                n = min(SCH, N - o)
                i = cvt_i[0] % 2
                P.dma(stage[i][:, 0:n], w_d[name][kc * 128:(kc + 1) * 128, o:o + n], W=[("stage", i)], nowaw=False)
                j = i
                cvt(stage_b[j][:, 0:n], stage[i][:, 0:n], R=[("stage", i)], W=[("stageb", j)])
                P.dma(wb_d[name][kc * 128:(kc + 1) * 128, o:o + n], stage_b[j][:, 0:n],
                      R=[("stageb", j)], W=[("wb", name)])
    wuk2 = sb("wuk2", [128, 128], BF16)
    wuv = sb("wuv", [128, 64], BF16)
    w2k2 = sb("w2k2", [128, 128], BF16)
    w2v = sb("w2v", [128, 64], BF16)
    smst = sb("smst", [128, 4, 64])
    P.op(pool, lambda: nc.gpsimd.memset(wuk2[:], 0.0), W=["wuk2"])
    P.dma(smst[:, 0, 0:48], sw_d["dsa_w_uk"], W=["smst"])
    P.dma(smst[:, 1, :], sw_d["dsa_w_uv"], W=["smst"])
    P.dma(smst[:, 2, :], sw_d["cmp_k_w2"], W=["smst"])
    P.dma(smst[:, 3, :], sw_d["cmp_v_w2"], W=["smst"])
    P.op(dve, lambda: nc.vector.tensor_copy(out=wuk2[:, 16:64], in_=smst[:, 0, 0:48]), R=["smst"], W=["wuk2"])
    P.op(dve, lambda: nc.vector.tensor_copy(out=wuk2[:, 80:128], in_=smst[:, 0, 0:48]), R=["smst"], W=["wuk2"])
    P.op(dve, lambda: nc.vector.tensor_copy(out=wuv[:], in_=smst[:, 1, :]), R=["smst"], W=["wuv"])
    P.op(dve, lambda: nc.vector.tensor_copy(out=w2k2[:, 0:64], in_=smst[:, 2, :]), R=["smst"], W=["w2k2"])
    P.op(dve, lambda: nc.vector.tensor_copy(out=w2k2[:, 64:128], in_=smst[:, 2, :]), R=["smst"], W=["w2k2"])
    P.op(dve, lambda: nc.vector.tensor_copy(out=w2v[:], in_=smst[:, 3, :]), R=["smst"], W=["w2v"])
    posf = sb("posf", [128, 2, 16])
    posb = sb("posb", [128, 2, 16], BF16)
    P.dma(posf[:, 0, :], posk_d, W=["posf"])
    P.dma(posf[:, 1, :], posv_d, W=["posf"])
    P.op(dve, lambda: nc.vector.tensor_copy(out=posb[:], in_=posf[:]), R=["posf"], W=["posb"])

    barrier()
    psf = nc.alloc_psum_tensor("psf", [128, 7, 512], F32)
    psb = nc.alloc_psum_tensor("psb", [128, 1024], BF16)
    bank_i = [0]

    def bank():
        b = bank_i[0] % 4
        bank_i[0] += 1
        return b

    bankL_i = [0]

    def bankL():
        b = 4 + bankL_i[0] % 3
        bankL_i[0] += 1
        return b

    def bank2():
        if not hasattr(bank2, "i"):
            bank2.i = 0
        b = (bank2.i % 2) * 2
        bank2.i += 1
        return b

    xg_i = [0]
    wbuf_i = [0]
    hT = sb("hT", [128, 8, GT], BF16)
    oT = hT
    sq = [sb(f"sq{i}", [128, GT]) for i in range(1)]
    sq_i = [0]
    sqh = [sb(f"sqh{i}", [128, 2, GT], BF16) for i in range(1)]
    sqh_i = [0]
    rstd = sb("rstd", [128, GT])
    wdn = [sb(f"wdn{i}", [128, 11, 128], BF16) for i in range(2)]
    wdn_i = [0]
    ropeCs = sb("ropeCs", [128, GT])
    ropeSs = sb("ropeSs", [128, GT])
    pre = [sb(f"pre{i}", [128, GT], BF16) for i in range(2)]
    pre_i = [0]
    rtmp = [sb(f"rtmp{i}", [128, GT]) for i in range(2)]
    rtmp_i = [0]
    qT = sb("qT", [128, 6, GT], BF16)
    qmemT = sb("qmemT", [128, 2, GT], BF16)
    mk_mark = a_ptr[0]
    maskT = sb("maskT", [128, 16, GT], BF16)
    mk_end = a_ptr[0]
    a_ptr[0] = mk_mark
    actT = sb("maskT", [128, 11, GT], BF16)
    a_ptr[0] = mk_mark
    memnT = sb("maskT", [128, 8, 256], BF16)
    a_ptr[0] = mk_end
    memf = xg[0][:, :, 0:256]
    sil = [sb(f"sil{i}", [128, GT], BF16) for i in range(2)]
    sil_i = [0]
    kmemT = sb("kmemT", [128, 2, 256], BF16)
    v1m = sb("v1m", [128, 2, 4, 128], BF16)
    PT = [sb(f"PT{i}", [128, GT], BF16) for i in range(6)]
    PT_i = [0]
    rd = [sb(f"rd{i}", [128, GT]) for i in range(2)]
    rd_i = [0]
    layer_mark = a_ptr[0]
    KT2 = sb("KT2", [128, T], BF16)
    kidxT2 = sb("kidxT2", [128, T], BF16)
    V1 = sb("V1", [128, 16, 128], BF16)
    ckvn = sb("ckvn", [128, GT], BF16)
    ckvf = sb("ckvf", [128, GT])
    wkr2 = sb("wkr2", [128, 8, 128], BF16)
    wki2 = sb("wki2", [128, 8, 128], BF16)
    widx = sb("widx", [128, 4, 8])
    Dh = [sb(f"Dh{i}", [128, 8, 128], BF16) for i in range(2)]
    Dh_i = [0]
    relu_t = [sb(f"relu{i}", [128, 512], BF16) for i in range(3)]
    relu_i = [0]
    sc = [sb(f"sc{i}", [128, T]) for i in range(2)]
    sc_i = [0]
    bst = sb("bst", [128, 8])
    wtab = sb("wtab", [128, 32])
    wtab2 = sb("wtab2", [128, 32])
    mask_tm = [sb(f"masktm{i}", [128, T], BF16) for i in range(2)]
    mask_i = [0]
    qidxT = sb("qidxT", [128, 4, GT], BF16)
    dsa_end = a_ptr[0]
    a_ptr[0] = layer_mark
    ksT2 = sb("ksT2", [128, 2, T], BF16)
    kwT2 = sb("kwT2", [128, 2, T], BF16)
    V1s = sb("V1s", [128, 16, 2, 128], BF16)
    V1w = sb("V1w", [128, 16, 2, 128], BF16)
    ph_mark = a_ptr[0]
    kcT = sb("kcT", [128, T], BF16)
    vcT = sb("vcT", [128, T], BF16)
    ph_end = a_ptr[0]
    a_ptr[0] = ph_mark
    cmpo = sb("cmpo", [128, 6, GT], BF16)
    oacc = sb("oacc", [128, GT])
    a_ptr[0] = max(a_ptr[0], ph_end)
    cb_b = sb("cb_b", [128, 2])
    kcmpT2 = sb("kcmpT2", [128, 2, 128], BF16)
    V1c = sb("V1c", [128, 2, 128], BF16)
    gel = [sb(f"gel{i}", [128, 128]) for i in range(4)]
    gT = sb("gT", [128, 128], BF16)
    qrawT = sb("qrawT", [128, 6, GT], BF16)
    sigT = sb("sigT", [36, GT])
    sigH = sb("sigH", [36, GT], BF16)
    sigL = sb("sigL", [36, GT], BF16)
    imp = sb("imp", [128, 4, 2, 32])
    imp2 = sb("imp2", [128, 4, 2, 32])
    imp3 = sb("imp3", [128, 4, 2, 32])
    m8 = sb("m8", [128, 16])
    selm = sb("selm", [128, 4, 64], BF16)
    selT = sb("selT", [64, GT], BF16)
    rec = sb("rec", [128, 8])
    otmp = [sb(f"otmp{i}", [128, GT]) for i in range(2)]
    otmp_i = [0]
    rg = [sb(f"rg{i}", [64, GT]) for i in range(2)]
    rg_i = [0]
    nsa_end = a_ptr[0]
    a_ptr[0] = max(dsa_end, nsa_end)

    def rot(lst, ctr):
        i = ctr[0] % len(lst)
        ctr[0] += 1
        return i

    def load_w(name, col0, ncols, nk=8):
        i = rot(wbuf, wbuf_i)
        src = wb_d[name].rearrange("(c p) n -> p c n", p=128)[:, 0:nk, col0:col0 + ncols]
        P.dma(wbuf[i][:, 0:nk, 0:ncols], src, R=[("wb", name)], W=[("wbuf", i)])
        return wbuf[i], ("wbuf", i)

    def sumsq_mm(b, src_ap, src_key, ncols, first, last):
        i = rot(sq, sq_i)
        P.op(act, lambda: nc.scalar.activation(out=sq[i][:, 0:ncols], in_=src_ap, func=AF.Square), R=[src_key], W=[("sq", i)])
        j = rot(sqh, sqh_i)
        P.op(dve, lambda: nc.vector.tensor_copy(out=sqh[j][:, 0, 0:ncols], in_=sq[i][:, 0:ncols]), R=[("sq", i)], W=[("sqh", j)])
        P.op(pool, lambda: nc.gpsimd.tensor_tensor(out=sqh[j][:, 1, 0:ncols], in0=sq[i][:, 0:ncols], in1=sqh[j][:, 0, 0:ncols], op=ALU.subtract),
             R=[("sq", i), ("sqh", j)], W=[("sqh", j)])
        P.op(pe, lambda: nc.tensor.matmul(psf[:, b, 0:ncols], lhsT=ones_b, rhs=sqh[j][:, 0, 0:ncols], start=first, stop=False),
             R=[("sqh", j), "misc_b"], W=[("ps", b)])
        P.op(pe, lambda: nc.tensor.matmul(psf[:, b, 0:ncols], lhsT=ones_b, rhs=sqh[j][:, 1, 0:ncols], start=False, stop=last),
             R=[("sqh", j), "misc_b"], W=[("ps", b)])

    def norm_group(xt, xkey, gidx, ncols=GT, dst=None, dkey="hT"):
        dst = hT if dst is None else dst
        b = bank()
        for c in range(8):
            sumsq_mm(b, xt[:, c, 0:ncols], xkey, ncols, c == 0, c == 7)
        P.op(act, lambda: nc.scalar.activation(out=rstd[:, 0:ncols], in_=psf[:, b, 0:ncols], func=AF.Sqrt,
                                               scale=1.0 / D, bias=epsT[:, 0:1]),
             R=[("ps", b)], W=["rstd"])
        P.op(dve, lambda: nc.vector.reciprocal(out=rstd[:, 0:ncols], in_=rstd[:, 0:ncols]), R=["rstd"], W=["rstd"])
        for c in range(8):
            P.op(dve, lambda c=c: nc.vector.scalar_tensor_tensor(
                out=dst[:, c, 0:ncols], in0=xt[:, c, 0:ncols], scalar=gam[:, gidx, c:c + 1],
                in1=rstd[:, 0:ncols], op0=ALU.mult, op1=ALU.mult),
                 R=[xkey, "rstd", "gam"], W=[dkey])

    def proj(lhs_fn, lhs_keys, rhs_t, rhs_key, ncols, M=128):
        b = bank()
        for kc in range(8):
            P.op(pe, lambda kc=kc: nc.tensor.matmul(psf[0:M, b, 0:ncols], lhsT=lhs_fn(kc), rhs=rhs_t[:, kc, 0:ncols],
                                                    start=(kc == 0), stop=(kc == 7)),
                 R=list(lhs_keys) + [rhs_key], W=[("ps", b)])
        return b

    def rope_from_bank(b, dst_ap, dkey, ncols=GT, raw=None):
        i = rot(pre, pre_i)
        P.op(act, lambda: nc.scalar.copy(out=pre[i][:, 0:ncols], in_=psf[:, b, 0:ncols]), R=[("ps", b)], W=[("pre", i)])
        if raw is not None:
            P.op(pool, lambda: nc.gpsimd.tensor_copy(out=raw[0], in_=pre[i][:, 0:ncols]), R=[("pre", i)], W=[raw[1]])
        b2 = bank()
        P.op(pe, lambda: nc.tensor.matmul(psf[:, b2, 0:ncols], lhsT=perm_b, rhs=pre[i][:, 0:ncols], start=True, stop=True),
             R=[("pre", i), "misc_b"], W=[("ps", b2)])
        j = rot(rtmp, rtmp_i)
        P.op(dve, lambda: nc.vector.tensor_tensor(out=rtmp[j][:, 0:ncols], in0=psf[:, b2, 0:ncols], in1=ropeSs[:, 0:ncols], op=ALU.mult),
             R=[("ps", b2), "ropeS"], W=[("rtmp", j)])
        k = rot(rtmp, rtmp_i)
        P.op(pool, lambda: nc.gpsimd.tensor_tensor(out=rtmp[k][:, 0:ncols], in0=pre[i][:, 0:ncols], in1=ropeCs[:, 0:ncols], op=ALU.mult),
             R=[("pre", i), "ropeC"], W=[("rtmp", k)])
        P.op(pool, lambda: nc.gpsimd.tensor_tensor(out=dst_ap, in0=rtmp[j][:, 0:ncols], in1=rtmp[k][:, 0:ncols], op=ALU.add),
             R=[("rtmp", j), ("rtmp", k)], W=[dkey])

    def evac(b, dst_ap, dkey, ncols=GT, M=128, eng=None):
        e = eng or act
        if e is act:
            P.op(act, lambda: nc.scalar.copy(out=dst_ap, in_=psf[0:M, b, 0:ncols]), R=[("ps", b)], W=[dkey])
        else:
            P.op(e, lambda: e.b.tensor_copy(out=dst_ap, in_=psf[0:M, b, 0:ncols]), R=[("ps", b)], W=[dkey])

    def load_rope(g):
        P.dma(ropeCs[:], c_d["ropeC"][:, g * GT:(g + 1) * GT], W=["ropeC"])
        P.dma(ropeSs[:], c_d["ropeS"][:, g * GT:(g + 1) * GT], W=["ropeS"])

    def load_x(src_d, s, g):
        i = 0
        src = src_d[s].rearrange("(c p) t -> p c t", p=128)
        for c in range(0, 8, 2):
            P.dma(xg[i][:, c:c + 2, :], src[:, c:c + 2, g * GT:(g + 1) * GT], W=[("xg", i)])
        return xg[i], ("xg", i)

    def attn_head(q_ap, q_key, blocks, dst_fn, scale=0.125, guard=False):
        bo = bankL()
        nb = len(blocks)
        pend = []
        for idx, blk in enumerate(blocks):
            bs = bank()
            P.op(pe, lambda: nc.tensor.matmul(psf[:, bs, :], lhsT=blk["kT"], rhs=q_ap, start=True, stop=True),
                 R=[blk["kkey"], q_key], W=[("ps", bs)])
            pi = rot(PT, PT_i)
            P.op(act, lambda: nc.scalar.activation(out=PT[pi][:], in_=psf[:, bs, :], func=AF.Exp, scale=scale),
                 R=[("ps", bs)], W=[("PT", pi)])
            if blk.get("mask") is not None:
                m_ap, m_key = blk["mask"]
                P.op(dve, lambda: nc.vector.tensor_tensor(out=PT[pi][:], in0=PT[pi][:], in1=m_ap, op=ALU.mult),
                     R=[("PT", pi), m_key], W=[("PT", pi)])
            pend.append((blk, pi, idx))
            if len(pend) > 2:
                _pv(pend.pop(0), bo, nb)
        while pend:
            _pv(pend.pop(0), bo, nb)
        ri = rot(rd, rd_i)
        if guard:
            P.op(dve, lambda: nc.vector.tensor_scalar_max(out=rd[ri][0:64, :], in0=psf[0:64, bo, :], scalar1=1e-18),
                 R=[("ps", bo)], W=[("rd", ri)])
            P.op(act, lambda: nc.scalar.activation(out=rd[ri][0:64, :], in_=rd[ri][0:64, :], func=AF.Ln), R=[("rd", ri)], W=[("rd", ri)])
        else:
            P.op(act, lambda: nc.scalar.activation(out=rd[ri][0:64, :], in_=psf[0:64, bo, :], func=AF.Ln), R=[("ps", bo)], W=[("rd", ri)])
        P.op(act, lambda: nc.scalar.activation(out=rd[ri][0:64, :], in_=rd[ri][0:64, :], func=AF.Exp, scale=-1.0), R=[("rd", ri)], W=[("rd", ri)])
        dst_fn(bo, rd[ri][0:64, :], ("rd", ri))

    def _pv(item, bo, nb):
        blk, pi, idx = item
        P.op(pe, lambda: nc.tensor.matmul(psf[:, bo, :], lhsT=blk["v1"], rhs=PT[pi][:], start=(idx == 0), stop=(idx == nb - 1)),
             R=[blk["vkey"], ("PT", pi)], W=[("ps", bo)])

    def plain_dst(chunk, base):
        def f(bo, rden, rkey):
            P.op(dve, lambda: nc.vector.tensor_tensor(out=oT[base:base + 64, chunk, :], in0=psf[64:128, bo, :], in1=rden, op=ALU.mult),
                 R=[("ps", bo), rkey], W=["hT"])
        return f

    def mem_kv(s, li):
        for c in range(0, 8, 4):
            P.dma(memf[:, c:c + 4, :], memT_d[s].rearrange("(c p) t -> p c t", p=128)[:, c:c + 4, :], W=[("xg", 0)])
        norm_group(memf, ("xg", 0), 2 + li, ncols=256, dst=memnT, dkey="maskT")
        wname = f"mem_w_kv{li}"
        wt, wk = load_w(wname, 0, 512)
        for c in range(2):
            b = proj(lambda kc, c=c: wt[:, kc, c * 128:(c + 1) * 128], [wk], memnT, "maskT", 256)
            evac(b, kmemT[:, c, :], "kmemT", ncols=256)
        P.op(pool, lambda: nc.gpsimd.memset(v1m[:, :, :, 0:64], 1.0), W=["v1m"])
        for nb_ in range(2):
            b = bank()
            for kc in range(8):
                P.op(pe, lambda kc=kc: nc.tensor.matmul(psf[:, b, 0:256], lhsT=memnT[:, kc, nb_ * 128:(nb_ + 1) * 128],
                                                        rhs=wt[:, kc, 256:512], start=(kc == 0), stop=(kc == 7)),
                     R=["maskT", wk], W=[("ps", b)])
            P.op(act, lambda: nc.scalar.copy(out=v1m[:, nb_, :, 64:128], in_=psf[:, b, 0:256].rearrange("p (h d) -> p h d", h=4)),
                 R=[("ps", b)], W=["v1m"])

    def mem_attn():
        for h in range(4):
            c, base = h // 2, (h % 2) * 64
            blocks = [dict(kT=kmemT[base:base + 64, c, nb_ * 128:(nb_ + 1) * 128], kkey="kmemT",
                           v1=v1m[:, nb_, h, :], vkey="v1m", mask=None) for nb_ in range(2)]
            attn_head(qmemT[base:base + 64, c, :], "qmemT", blocks, plain_dst(6 + c, base))

    def wo_ffn(s, li, g, xt, xkey, last):
        for half in range(2):
            wt, wk = load_w(f"w_o{li}", half * 512, 512)
            for oc4 in range(4):
                oc = half * 4 + oc4
                b = proj(lambda kc, oc4=oc4, wt=wt: wt[:, kc, oc4 * 128:(oc4 + 1) * 128], [wk], oT, "hT", GT)
                P.op(dve, lambda oc=oc, b=b: nc.vector.tensor_tensor(out=xt[:, oc, :], in0=xt[:, oc, :], in1=psf[:, b, :], op=ALU.add),
                     R=[xkey, ("ps", b)], W=[xkey])
        norm_group(xt, xkey, 4 + li)
        wsrc = wb_d[f"ffn_w_in{li}"].rearrange("(c p) n -> p c n", p=128)
        dsrc = wb_d[f"ffn_w_down{li}"].rearrange("(c p) n -> p c n", p=128)
        for fh in range(2):
            for cl in range(11):
                ch = fh * 11 + cl
                i = rot(wbuf, wbuf_i)
                P.dma(wbuf[i][:, :, 0:128], wsrc[:, :, ch * 128:(ch + 1) * 128], R=[("wb", f"ffn_w_in{li}")], W=[("wbuf", i)])
                P.dma(wbuf[i][:, :, 128:256], wsrc[:, :, DFF + ch * 128:DFF + (ch + 1) * 128], R=[("wb", f"ffn_w_in{li}")], W=[("wbuf", i)])
                wgk = ("wbuf", i)
                bg = proj(lambda kc, i=i: wbuf[i][:, kc, 0:128], [wgk], hT, "hT", GT)
                bu = proj(lambda kc, i=i: wbuf[i][:, kc, 128:256], [wgk], hT, "hT", GT)
                si = rot(sil, sil_i)
                P.op(act, lambda bg=bg, si=si: nc.scalar.activation(out=sil[si][:], in_=psf[:, bg, :], func=AF.Silu),
                     R=[("ps", bg)], W=[("sil", si)])
                P.op(dve, lambda cl=cl, bu=bu, si=si: nc.vector.tensor_tensor(out=actT[:, cl, :], in0=psf[:, bu, :], in1=sil[si][:], op=ALU.mult),
                     R=[("ps", bu), ("sil", si)], W=["maskT"])
            for oc in range(8):
                i = rot(wdn, wdn_i)
                P.dma(wdn[i][:], dsrc[:, fh * 11:(fh + 1) * 11, oc * 128:(oc + 1) * 128], R=[("wb", f"ffn_w_down{li}")], W=[("wdn", i)])
                b = bankL()
                for kc in range(11):
                    P.op(pe, lambda kc=kc, i=i, b=b: nc.tensor.matmul(psf[:, b, :], lhsT=wdn[i][:, kc, :], rhs=actT[:, kc, :],
                                                                     start=(kc == 0), stop=(kc == 10)),
                         R=[("wdn", i), "maskT"], W=[("ps", b)])
                P.op(dve, lambda oc=oc, b=b: nc.vector.tensor_tensor(out=xt[:, oc, :], in0=xt[:, oc, :], in1=psf[:, b, :], op=ALU.add),
                     R=[xkey, ("ps", b)], W=[xkey])

    def final_store(s, g, xt, xkey, last):
        if last:
            b = bank()
            for c in range(8):
                sumsq_mm(b, xt[:, c, :], xkey, GT, c == 0, c == 7)
            P.op(act, lambda: nc.scalar.activation(out=rstd[:], in_=psf[:, b, :], func=AF.Sqrt, scale=1.0 / D, bias=epsT[:, 0:1]),
                 R=[("ps", b)], W=["rstd"])
            P.op(dve, lambda: nc.vector.reciprocal(out=rstd[:], in_=rstd[:]), R=["rstd"], W=["rstd"])
            for c in range(8):
                P.op(dve, lambda c=c: nc.vector.scalar_tensor_tensor(out=xt[:, c, :], in0=xt[:, c, :], scalar=gam[:, 6, c:c + 1],
                                                                      in1=rstd[:], op0=ALU.mult, op1=ALU.mult),
                     R=[xkey, "rstd", "gam"], W=[xkey])
            dst = outT_d[s].rearrange("(c p) t -> p c t", p=128)
            P.dma(dst[:, :, g * GT:(g + 1) * GT], xt[:], R=[xkey], W=["out"])
        else:
            dst = xs_d[s].rearrange("(c p) t -> p c t", p=128)
            P.dma(dst[:, :, g * GT:(g + 1) * GT], xt[:], R=[xkey], W=[("xs", s)])

    def dsa_layer(s, li, src_d, last):
        barrier()
        mem_kv(s, li)
        wt, wk = load_w("dsa_w_in", 896, 16 + 0)
        P.op(pool, lambda: nc.gpsimd.memset(wkr2[:], 0.0), W=["wkr2"])
        P.op(pool, lambda: nc.gpsimd.tensor_copy(out=wkr2[:, :, 0:16], in_=wt[:, :, 0:16]), R=[wk], W=["wkr2"])
        P.op(pool, lambda: nc.gpsimd.tensor_copy(out=wkr2[:, :, 64:80], in_=wt[:, :, 0:16]), R=[wk], W=["wkr2"])
        wt2, wk2 = load_w("dsa_w_in", 1424, 64)
        P.op(pool, lambda: nc.gpsimd.tensor_copy(out=wki2[:, :, 0:64], in_=wt2[:, :, 0:64]), R=[wk2], W=["wki2"])
        P.op(pool, lambda: nc.gpsimd.tensor_copy(out=wki2[:, :, 64:128], in_=wt2[:, :, 0:64]), R=[wk2], W=["wki2"])
        P.op(pool, lambda: nc.gpsimd.memset(V1[:, :, 0:64], 1.0), W=["V1"])
        for g in range(NG):
            xt, xkey = load_x(src_d, s, g)
            load_rope(g)
            norm_group(xt, xkey, 0 + li)
            wc, wck = load_w("dsa_w_in", 768, 128)
            b = proj(lambda kc: wc[:, kc, 0:128], [wck], hT, "hT", GT)
            evac(b, ckvf[:], "ckvf", eng=dve)
            b2 = bank()
            sumsq_mm(b2, ckvf[:], "ckvf", GT, True, True)
            P.op(act, lambda: nc.scalar.activation(out=rstd[:], in_=psf[:, b2, :], func=AF.Sqrt, scale=1.0 / 128, bias=epsT[:, 0:1]),
                 R=[("ps", b2)], W=["rstd"])
            P.op(dve, lambda: nc.vector.reciprocal(out=rstd[:], in_=rstd[:]), R=["rstd"], W=["rstd"])
            P.op(dve, lambda: nc.vector.scalar_tensor_tensor(out=ckvn[:], in0=ckvf[:], scalar=ckvg[:, 0:1], in1=rstd[:],
                                                             op0=ALU.mult, op1=ALU.mult),
                 R=["ckvf", "rstd", "ckvg"], W=["ckvn"])
            bk = bank()
            for kc in range(8):
                P.op(pe, lambda kc=kc: nc.tensor.matmul(psf[:, bk, :], lhsT=wkr2[:, kc, :], rhs=hT[:, kc, :], start=(kc == 0), stop=False),
                     R=["wkr2", "hT"], W=[("ps", bk)])
            P.op(pe, lambda: nc.tensor.matmul(psf[:, bk, :], lhsT=wuk2[:], rhs=ckvn[:], start=False, stop=True),
                 R=["wuk2", "ckvn"], W=[("ps", bk)])
            rope_from_bank(bk, KT2[:, g * GT:(g + 1) * GT], "KT2")
            bi = proj(lambda kc: wki2[:, kc, :], ["wki2"], hT, "hT", GT)
            rope_from_bank(bi, kidxT2[:, g * GT:(g + 1) * GT], "kidxT2")
            for tt in range(4):
                bv = bank()
                P.op(pe, lambda tt=tt: nc.tensor.matmul(psf[:, bv, 0:64], lhsT=ckvn[:, tt * 128:(tt + 1) * 128], rhs=wuv[:], start=True, stop=True),
                     R=["ckvn", "wuv"], W=[("ps", bv)])
                P.op(act, lambda tt=tt, bv=bv: nc.scalar.copy(out=V1[:, g * 4 + tt, 64:128], in_=psf[:, bv, 0:64]), R=[("ps", bv)], W=["V1"])
        for g in range(NG):
            xt, xkey = load_x(src_d, s, g)
            load_rope(g)
            norm_group(xt, xkey, 0 + li)
            wq, wqk = load_w("dsa_w_in", 0, 512)
            for c in range(4):
                b = proj(lambda kc, c=c: wq[:, kc, c * 128:(c + 1) * 128], [wqk], hT, "hT", GT)
                rope_from_bank(b, qT[:, c, :], "qT")
            wq2, wq2k = load_w("dsa_w_in", 512, 256)
            for c in range(2):
                b = proj(lambda kc, c=c: wq2[:, kc, c * 128:(c + 1) * 128], [wq2k], hT, "hT", GT)
                rope_from_bank(b, qT[:, 4 + c, :], "qT")
            wi, wik = load_w("dsa_w_in", 912, 512)
            for c in range(4):
                b = proj(lambda kc, c=c: wi[:, kc, c * 128:(c + 1) * 128], [wik], hT, "hT", GT)
                rope_from_bank(b, qidxT[:, c, :], "qidxT")
            wm_, wmk = load_w("dsa_w_in", 1488, 264)
            for c in range(2):
                b = proj(lambda kc, c=c: wm_[:, kc, 8 + c * 128:8 + (c + 1) * 128], [wmk], hT, "hT", GT)
                evac(b, qmemT[:, c, :], "qmemT")
            for tt in range(4):
                b = bank()
                for kc in range(8):
                    P.op(pe, lambda kc=kc, tt=tt: nc.tensor.matmul(psf[:, b, 0:8], lhsT=hT[:, kc, tt * 128:(tt + 1) * 128], rhs=wm_[:, kc, 0:8],
                                                                   start=(kc == 0), stop=(kc == 7)),
                         R=["hT", wmk], W=[("ps", b)])
                P.op(act, lambda tt=tt, b=b: nc.scalar.mul(widx[:, tt, :], psf[:, b, 0:8], float(8 ** -0.5 * 64 ** -0.5)),
                     R=[("ps", b)], W=["widx"])
            P.op(pool, lambda: nc.gpsimd.memset(maskT[:], 0.0), W=["maskT"])
            for tt in range(4):
                qi = g * 4 + tt
                Sc = 128 * (qi + 1)
                di = rot(Dh, Dh_i)
                for h in range(8):
                    P.op(dve, lambda h=h, di=di, tt=tt: nc.vector.tensor_scalar(out=Dh[di][:, h, :], in0=ident_b, scalar1=widx[:, tt, h:h + 1],
                                                                                scalar2=None, op0=ALU.mult),
                         R=["misc_b", "widx"], W=[("Dh", di)])
                si = rot(sc, sc_i)
                for k0 in range(0, Sc, 512):
                    kn = min(512, Sc - k0)
                    ba = bankL()
                    for h in range(8):
                        c, base = h // 2, (h % 2) * 64
                        bl = bank()
                        P.op(pe, lambda c=c, base=base, bl=bl, k0=k0, kn=kn: nc.tensor.matmul(
                            psf[:, bl, 0:kn], lhsT=qidxT[base:base + 64, c, tt * 128:(tt + 1) * 128],
                            rhs=kidxT2[base:base + 64, k0:k0 + kn], start=True, stop=True),
                             R=["qidxT", "kidxT2"], W=[("ps", bl)])
                        ri = rot(relu_t, relu_i)
                        P.op(act, lambda bl=bl, ri=ri, kn=kn: nc.scalar.activation(out=relu_t[ri][:, 0:kn], in_=psf[:, bl, 0:kn], func=AF.Relu),
                             R=[("ps", bl)], W=[("relu", ri)])
                        P.op(pe, lambda h=h, ri=ri, ba=ba, kn=kn, di=di: nc.tensor.matmul(
                            psf[:, ba, 0:kn], lhsT=Dh[di][:, h, :], rhs=relu_t[ri][:, 0:kn], start=(h == 0), stop=(h == 7)),
                             R=[("Dh", di), ("relu", ri)], W=[("ps", ba)])
                    P.op(act, lambda ba=ba, si=si, k0=k0, kn=kn: nc.scalar.copy(out=sc[si][:, k0:k0 + kn], in_=psf[:, ba, 0:kn]),
                         R=[("ps", ba)], W=[("sc", si)])
                scv = sc[si]
                skey = ("sc", si)
                if qi >= 2:
                    P.op(dve, lambda: nc.vector.tensor_reduce(out=bst[:, 0:1], in_=scv[:, 0:Sc], axis=AX.X, op=ALU.min), R=[skey], W=["bst0"])
                    P.op(dve, lambda: nc.vector.tensor_reduce(out=bst[:, 1:2], in_=scv[:, 0:Sc], axis=AX.X, op=ALU.max), R=[skey], W=["bst1"])
                P.op(dve, lambda: nc.vector.tensor_tensor(out=scv[:, Sc - 128:Sc], in0=scv[:, Sc - 128:Sc], in1=cbias_f, op=ALU.add),
                     R=[skey, "misc_f", "bst0", "bst1"], W=[skey])
                mi = rot(mask_tm, mask_i)
                if qi >= 2:
                    NIT = 16
                    P.op(dve, lambda: nc.vector.tensor_tensor(out=bst[:, 2:3], in0=bst[:, 1:2], in1=bst[:, 0:1], op=ALU.subtract),
                         R=["bst0", "bst1"], W=["bst2"])
                    P.op(dve, lambda: nc.vector.tensor_scalar(out=wtab[:], in0=bis[:], scalar1=bst[:, 2:3], scalar2=None, op0=ALU.mult),
                         R=["bis", "bst2"], W=["wtab"])
                    P.op(dve, lambda: nc.vector.tensor_scalar(out=wtab2[:], in0=bis[:], scalar1=bst[:, 2:3], scalar2=2.0, op0=ALU.mult, op1=ALU.mult),
                         R=["bis", "bst2"], W=["wtab2"])
                    P.op(dve, lambda: nc.vector.tensor_tensor(out=bst[:, 3:4], in0=bst[:, 0:1], in1=wtab[:, 0:1], op=ALU.add),
                         R=["bst0", "wtab"], W=["bst3"])
                    for it in range(NIT):
                        P.op(dve, lambda: nc.vector.tensor_scalar(out=mask_tm[mi][:, 0:Sc], in0=scv[:, 0:Sc], scalar1=bst[:, 3:4], scalar2=0.0,
                                                                  op0=ALU.is_ge, op1=ALU.add, accum_out=bst[:, 4:5]),
                             R=[skey, "bst3"], W=[("masktm", mi), "bst4"])
                        if it < NIT - 1:
                            wn = wtab[:, it + 1:it + 2]
                            wn2 = wtab2[:, it + 1:it + 2]
                            P.op(dve, lambda wn2=wn2: nc.vector.tensor_scalar(out=bst[:, 5:6], in0=bst[:, 4:5], scalar1=255.5, scalar2=wn2,
                                                                              op0=ALU.is_ge, op1=ALU.mult),
                                 R=["bst4", "wtab2"], W=["bst5"])
                            P.op(dve, lambda wn=wn: nc.vector.scalar_tensor_tensor(out=bst[:, 3:4], in0=bst[:, 5:6], scalar=wn, in1=bst[:, 3:4],
                                                                                   op0=ALU.subtract, op1=ALU.add),
                                 R=["bst5", "bst3", "wtab"], W=["bst3"])
                        else:
                            wl = wtab[:, it:it + 1]
                            P.op(dve, lambda wl=wl: nc.vector.tensor_scalar(out=bst[:, 5:6], in0=bst[:, 4:5], scalar1=255.5, scalar2=1.0,
                                                                            op0=ALU.is_ge, op1=ALU.subtract),
                                 R=["bst4"], W=["bst5"])
                            P.op(dve, lambda wl=wl: nc.vector.scalar_tensor_tensor(out=bst[:, 6:7], in0=bst[:, 5:6], scalar=wl, in1=bst[:, 3:4],
                                                                                   op0=ALU.mult, op1=ALU.add),
                                 R=["bst5", "bst3", "wtab"], W=["bst6"])
                    P.op(dve, lambda: nc.vector.tensor_scalar(out=mask_tm[mi][:, 0:Sc], in0=scv[:, 0:Sc], scalar1=bst[:, 6:7], scalar2=None, op0=ALU.is_ge),
                         R=[skey, "bst6"], W=[("masktm", mi)])
                else:
                    P.op(dve, lambda: nc.vector.tensor_scalar(out=mask_tm[mi][:, 0:Sc], in0=scv[:, 0:Sc], scalar1=-1.0e29, scalar2=None, op0=ALU.is_ge),
                         R=[skey], W=[("masktm", mi)])
                nblk = qi + 1
                for j0 in range(0, nblk, 8):
                    jn = min(8, nblk - j0)
                    for j in range(j0, j0 + jn):
                        P.op(pe, lambda j=j, j0=j0, mi=mi: nc.tensor.transpose(psb[:, (j - j0) * 128:(j - j0 + 1) * 128],
                                                                              mask_tm[mi][:, j * 128:(j + 1) * 128], ident_b),
                             R=[("masktm", mi), "misc_b"], W=["psb"])
                    P.op(act, lambda j0=j0, jn=jn, tt=tt: nc.scalar.copy(out=maskT[:, j0:j0 + jn, tt * 128:(tt + 1) * 128],
                                                                          in_=psb[:, 0:jn * 128].rearrange("p (j t) -> p j t", j=jn)),
                         R=["psb"], W=["maskT"])
            nkb = 4 * g + 4
            for h in range(12):
                c, base = h // 2, (h % 2) * 64
                blocks = [dict(kT=KT2[base:base + 64, j * 128:(j + 1) * 128], kkey="KT2", v1=V1[:, j, :], vkey="V1",
                               mask=(maskT[:, j, :], "maskT")) for j in range(nkb)]
                attn_head(qT[base:base + 64, c, :], "qT", blocks, plain_dst(c, base))
            mem_attn()
            wo_ffn(s, li, g, xt, xkey, last)
            final_store(s, g, xt, xkey, last)

    def nsa_layer(s, li, src_d, last):
        barrier()
        mem_kv(s, li)
        for kv, nm in enumerate(["cmp_k_w1", "cmp_v_w1"]):
            i = rot(wbuf, wbuf_i)
            w1c = wbuf[i][:].rearrange("p a b -> p (a b)")[:, 0:2048].rearrange("p (c m) -> p c m", m=128)
            src2 = wb_d[nm].rearrange("(c p) m -> p c m", p=128)
            P.dma(w1c, src2, R=[("wb", nm)], W=[("wbuf", i)])
            b = bank()
            for c in range(16):
                P.op(pe, lambda c=c, kv=kv, w1c=w1c, b=b: nc.tensor.matmul(psf[:, b, 0:1], lhsT=w1c[:, c, :], rhs=posb[:, kv, c:c + 1],
                                                                          start=(c == 0), stop=(c == 15)),
                     R=[("wbuf", i), "posb"], W=[("ps", b)])
            P.op(act, lambda kv=kv, b=b: nc.scalar.copy(out=cb_b[:, kv:kv + 1], in_=psf[:, b, 0:1]), R=[("ps", b)], W=["cb_b"])
        if STOP[0] == 1:
            raise StopBuild()
        P.op(pool, lambda: nc.gpsimd.memset(V1s[:, :, :, 0:64], 1.0), W=["V1s"])
        P.op(pool, lambda: nc.gpsimd.memset(V1w[:, :, :, 0:64], 1.0), W=["V1w"])
        for g in range(NG):
            xt, xkey = load_x(src_d, s, g)
            load_rope(g)
            norm_group(xt, xkey, 0 + li)
            wk_, wkk = load_w("nsa_w_in", 768, 512)
            wk2_, wk2k = load_w("nsa_w_in", 1280, 256)
            b = proj(lambda kc: wk_[:, kc, 0:128], [wkk], hT, "hT", GT)
            evac(b, kcT[:, g * GT:(g + 1) * GT], "kcT")
            b = proj(lambda kc: wk_[:, kc, 128:256], [wkk], hT, "hT", GT)
            evac(b, vcT[:, g * GT:(g + 1) * GT], "vcT")
            for src_t, src_k, off, dstT, dkey in ((wk_, wkk, 256, ksT2, "ksT2"), (wk2_, wk2k, 0, kwT2, "kwT2")):
                for grp in range(2):
                    bb = bank()
                    for half in range(2):
                        for kc in range(8):
                            P.op(pe, lambda kc=kc, half=half, grp=grp, src_t=src_t, off=off, bb=bb: nc.tensor.matmul(
                                psf[half * 64:half * 64 + 64, bb, :], lhsT=src_t[:, kc, off + grp * 64:off + grp * 64 + 64], rhs=hT[:, kc, :],
                                start=(kc == 0), stop=(kc == 7)),
                                 R=[src_k, "hT"], W=[("ps", bb)])
                    rope_from_bank(bb, dstT[:, grp, g * GT:(g + 1) * GT], dkey)
            for src_t, src_k, off, dstV, dkey in ((wk_, wkk, 384, V1s, "V1s"), (wk2_, wk2k, 128, V1w, "V1w")):
                for tt in range(4):
                    bv = bank()
                    for kc in range(8):
                        P.op(pe, lambda kc=kc, tt=tt, src_t=src_t, off=off, bv=bv: nc.tensor.matmul(
                            psf[:, bv, 0:128], lhsT=hT[:, kc, tt * 128:(tt + 1) * 128], rhs=src_t[:, kc, off:off + 128],
                            start=(kc == 0), stop=(kc == 7)),
                             R=["hT", src_k], W=[("ps", bv)])
                    P.op(act, lambda tt=tt, bv=bv, dstV=dstV: nc.scalar.copy(out=dstV[:, g * 4 + tt, :, 64:128],
                                                                               in_=psf[:, bv, 0:128].rearrange("p (g d) -> p g d", g=2)),
                         R=[("ps", bv)], W=[dkey])
        if STOP[0] == 2:
            raise StopBuild()
        P.op(pool, lambda: nc.gpsimd.memset(V1c[:, :, 0:64], 1.0), W=["V1c"])
        for kv, (srcT, skey) in enumerate(((kcT, "kcT"), (vcT, "vcT"))):
            nm = ["cmp_k_w1", "cmp_v_w1"][kv]
            wi_ = rot(wbuf, wbuf_i)
            w1dup = wbuf[wi_][:].rearrange("p a b -> p (a b)").rearrange("p (l m) -> p l m", m=128)
            srcw = wb_d[nm].rearrange("(l d) m -> d l m", d=64)
            P.dma(w1dup[0:64, :, :], srcw, R=[("wb", nm)], W=[("wbuf", wi_)])
            P.dma(w1dup[64:128, :, :], srcw, R=[("wb", nm)], W=[("wbuf", wi_)])
            for grp in range(2):
                base = grp * 64
                b = bank()
                for l in range(32):
                    P.op(pe, lambda l=l, kv=kv, base=base, srcT=srcT, b=b, w1dup=w1dup: nc.tensor.matmul(
                        psf[:, b, 0:127], lhsT=w1dup[base:base + 64, l, :], rhs=srcT[base:base + 64, l:l + 16 * 126 + 1:16],
                        start=(l == 0), stop=(l == 31)),
                         R=[("wbuf", wi_), skey], W=[("ps", b)])
                P.op(act, lambda kv=kv, b=b: nc.scalar.activation(out=gel[0][:, 0:127], in_=psf[:, b, 0:127], func=AF.Identity,
                                                                  bias=cb_b[:, kv:kv + 1], scale=1.0),
                     R=[("ps", b), "cb_b"], W=["gel0"])
                P.op(act, lambda: nc.scalar.activation(out=gel[1][:, 0:127], in_=gel[0][:, 0:127], func=AF.Square), R=["gel0"], W=["gel1"])
                P.op(dve, lambda: nc.vector.tensor_scalar(out=gel[1][:, 0:127], in0=gel[1][:, 0:127], scalar1=0.044715, scalar2=1.0,
                                                          op0=ALU.mult, op1=ALU.add), R=["gel1"], W=["gel1"])
                P.op(dve, lambda: nc.vector.tensor_tensor(out=gel[2][:, 0:127], in0=gel[1][:, 0:127], in1=gel[0][:, 0:127], op=ALU.mult),
                     R=["gel1", "gel0"], W=["gel2"])
                P.op(act, lambda: nc.scalar.activation(out=gel[3][:, 0:127], in_=gel[2][:, 0:127], func=AF.Sigmoid, scale=1.5957691216),
                     R=["gel2"], W=["gel3"])
                P.op(pool, lambda: nc.gpsimd.memset(gT[:], 0.0), W=["gT"])
                P.op(dve, lambda: nc.vector.tensor_tensor(out=gT[:, 0:127], in0=gel[3][:, 0:127], in1=gel[0][:, 0:127], op=ALU.mult),
                     R=["gel3", "gel0"], W=["gT"])
                b2 = bank()
                if kv == 0:
                    P.op(pe, lambda: nc.tensor.matmul(psf[:, b2, 0:128], lhsT=w2k2[:], rhs=gT[:], start=True, stop=True),
                         R=["w2k2", "gT"], W=[("ps", b2)])
                    evac(b2, kcmpT2[:, grp, :], "kcmpT2", ncols=128)
                else:
                    P.op(pe, lambda: nc.tensor.matmul(psf[:, b2, 0:64], lhsT=gT[:], rhs=w2v[:], start=True, stop=True),
                         R=["w2v", "gT"], W=[("ps", b2)])
                    evac(b2, V1c[:, grp, 64:128], "V1c", ncols=64)
        if STOP[0] == 3:
            raise StopBuild()
        barrier()
        if STOP[0] == 40:
            raise StopBuild()
        for g in range(NG):
            xt, xkey = load_x(src_d, s, g)
            if STOP[0] == 401:
                raise StopBuild()
            load_rope(g)
            if STOP[0] == 402:
                raise StopBuild()
            norm_group(xt, xkey, 0 + li)
            if STOP[0] == 41:
                raise StopBuild()
            wq, wqk = load_w("nsa_w_in", 0, 512)
            wq2, wq2k = load_w("nsa_w_in", 512, 256)
            for c in range(6):
                w_, wk_ = (wq, wqk) if c < 4 else (wq2, wq2k)
                cc = c if c < 4 else c - 4
                b = proj(lambda kc, cc=cc, w_=w_: w_[:, kc, cc * 128:(cc + 1) * 128], [wk_], hT, "hT", GT)
                rope_from_bank(b, qT[:, c, :], "qT", raw=(qrawT[:, c, :], "qrawT"))
            if STOP[0] == 42:
                raise StopBuild()
            wm_, wmk = load_w("nsa_w_in", 1536, 292)
            for c in range(2):
                b = proj(lambda kc, c=c: wm_[:, kc, 36 + c * 128:36 + (c + 1) * 128], [wmk], hT, "hT", GT)
                evac(b, qmemT[:, c, :], "qmemT")
            if STOP[0] == 43:
                raise StopBuild()
            b = proj(lambda kc: wm_[:, kc, 0:64], [wmk], hT, "hT", GT, M=64)
            P.op(act, lambda b=b: nc.scalar.activation(out=sigT[:], in_=psf[0:36, b, :], func=AF.Sigmoid), R=[("ps", b)], W=["sigT"])
            P.op(dve, lambda: nc.vector.tensor_copy(out=sigH[:], in_=sigT[:]), R=["sigT"], W=["sigH"])
            P.op(dve, lambda: nc.vector.tensor_tensor(out=sigL[:], in0=sigT[:], in1=sigH[:], op=ALU.subtract), R=["sigT", "sigH"], W=["sigL"])
            if STOP[0] == 4:
                raise StopBuild()
            mcg = mc_b[:, g * GT:(g + 1) * GT]
            P.op(pool, lambda: nc.gpsimd.memset(imp[:], 0.0), W=["imp"])

            def gated_dst(h, br, add_ap, add_key, out_ap, out_key):
                def f(bo, rden, rkey):
                    bgate = bank()
                    kk = h * 3 + br
                    P.op(pe, lambda: nc.tensor.matmul(psf[0:64, bgate, :], lhsT=E_flat[0:36, kk * 64:(kk + 1) * 64], rhs=sigH[:], start=True, stop=False),
                         R=["E_b", "sigH"], W=[("ps", bgate)])
                    P.op(pe, lambda: nc.tensor.matmul(psf[0:64, bgate, :], lhsT=E_flat[0:36, kk * 64:(kk + 1) * 64], rhs=sigL[:], start=False, stop=True),
                         R=["E_b", "sigL"], W=[("ps", bgate)])
                    gi = rot(rg, rg_i)
                    P.op(dve, lambda: nc.vector.tensor_tensor(out=rg[gi][:], in0=psf[0:64, bgate, :], in1=rden, op=ALU.mult),
                         R=[("ps", bgate), rkey], W=[("rg", gi)])
                    base = (h % 2) * 64
                    if add_ap is None:
                        P.op(dve, lambda: nc.vector.tensor_tensor(out=out_ap, in0=psf[64:128, bo, :], in1=rg[gi][:], op=ALU.mult),
                             R=[("ps", bo), ("rg", gi)], W=[out_key])
                    else:
                        oi = rot(otmp, otmp_i)
                        P.op(dve, lambda: nc.vector.tensor_tensor(out=otmp[oi][base:base + 64, :], in0=psf[64:128, bo, :], in1=rg[gi][:], op=ALU.mult),
                             R=[("ps", bo), ("rg", gi)], W=[("otmp", oi)])
                        P.op(pool, lambda: nc.gpsimd.tensor_tensor(out=out_ap, in0=otmp[oi][base:base + 64, :], in1=add_ap, op=ALU.add),
                             R=[("otmp", oi), add_key], W=[out_key])
                return f

            for grp in range(2):
                heads = list(range(grp * 6, grp * 6 + 6))
                for h in heads:
                    c, base = h // 2, (h % 2) * 64
                    bs = bank()
                    P.op(pe, lambda c=c, base=base, bs=bs: nc.tensor.matmul(psf[:, bs, :], lhsT=kcmpT2[base:base + 64, grp, :],
                                                                           rhs=qrawT[base:base + 64, c, :], start=True, stop=True),
                         R=["kcmpT2", "qrawT"], W=[("ps", bs)])
                    pi = rot(PT, PT_i)
                    P.op(act, lambda bs=bs, pi=pi: nc.scalar.activation(out=PT[pi][:], in_=psf[:, bs, :], func=AF.Exp, scale=0.125),
                         R=[("ps", bs)], W=[("PT", pi)])
                    P.op(dve, lambda pi=pi: nc.vector.tensor_tensor(out=PT[pi][:], in0=PT[pi][:], in1=mcg, op=ALU.mult),
                         R=[("PT", pi), "mc_b"], W=[("PT", pi)])
                    for tt in range(4):
                        bi_ = bank()
                        P.op(pe, lambda tt=tt, pi=pi, bi_=bi_: nc.tensor.matmul(psf[:, bi_, 0:33], lhsT=PT[pi][:, tt * 128:(tt + 1) * 128],
                                                                               rhs=ov_b[:], start=True, stop=True),
                             R=[("PT", pi), "ov_b"], W=[("ps", bi_)])
                        P.op(dve, lambda bi_=bi_, tt=tt: nc.vector.tensor_scalar_max(out=rec[:, tt:tt + 1], in0=psf[:, bi_, 32:33], scalar1=1e-30),
                             R=[("ps", bi_)], W=[("rec", tt)])
                        P.op(dve, lambda tt=tt: nc.vector.reciprocal(out=rec[:, tt:tt + 1], in_=rec[:, tt:tt + 1]), R=[("rec", tt)], W=[("rec", tt)])
                        P.op(dve, lambda bi_=bi_, tt=tt: nc.vector.scalar_tensor_tensor(out=imp[:, tt, grp, :], in0=psf[:, bi_, 0:32],
                                                                                        scalar=rec[:, tt:tt + 1], in1=imp[:, tt, grp, :],
                                                                                        op0=ALU.mult, op1=ALU.add),
                             R=[("ps", bi_), ("rec", tt), "imp"], W=["imp"])
                    bo = bankL()
                    P.op(pe, lambda pi=pi, bo=bo: nc.tensor.matmul(psf[:, bo, :], lhsT=V1c[:, grp, :], rhs=PT[pi][:], start=True, stop=True),
                         R=["V1c", ("PT", pi)], W=[("ps", bo)])
                    ri = rot(rd, rd_i)
                    P.op(dve, lambda bo=bo, ri=ri: nc.vector.tensor_scalar_max(out=rd[ri][0:64, :], in0=psf[0:64, bo, :], scalar1=1e-18),
                         R=[("ps", bo)], W=[("rd", ri)])
                    P.op(act, lambda ri=ri: nc.scalar.activation(out=rd[ri][0:64, :], in_=rd[ri][0:64, :], func=AF.Ln), R=[("rd", ri)], W=[("rd", ri)])
                    P.op(act, lambda ri=ri: nc.scalar.activation(out=rd[ri][0:64, :], in_=rd[ri][0:64, :], func=AF.Exp, scale=-1.0), R=[("rd", ri)], W=[("rd", ri)])
                    hl = h - grp * 6
                    gated_dst(h, 0, None, None, cmpo[base:base + 64, hl, :], "cmpo")(bo, rd[ri][0:64, :], ("rd", ri))
                for tt in range(4):
                    qi = g * 4 + tt
                    P.op(dve, lambda tt=tt, qi=qi: nc.vector.tensor_tensor(out=imp2[:, tt, grp, :], in0=imp[:, tt, grp, :], in1=selF[:, 30 - 2 * qi:62 - 2 * qi], op=ALU.max),
                         R=["imp", "selF"], W=["imp2"])
                    P.op(dve, lambda tt=tt, qi=qi: nc.vector.tensor_tensor(out=imp2[:, tt, grp, :], in0=imp2[:, tt, grp, :], in1=selA[:, 30 - 2 * qi:62 - 2 * qi], op=ALU.add),
                         R=["imp2", "selA"], W=["imp2"])
                    P.op(dve, lambda tt=tt: nc.vector.memset(imp2[:, tt, grp, 0:1], 1.0e9), R=["imp2"], W=["imp2"])
                    P.op(dve, lambda tt=tt: nc.vector.max(out=m8[:, 0:8], in_=imp2[:, tt, grp, :]), R=["imp2"], W=["m8a"])
                    P.op(dve, lambda tt=tt: nc.vector.match_replace(out=imp3[:, tt, grp, :], in_to_replace=m8[:, 0:8], in_values=imp2[:, tt, grp, :],
                                                                    imm_value=-3.0e38),
                         R=["imp2", "m8a"], W=["imp3"])
                    P.op(dve, lambda tt=tt: nc.vector.max(out=m8[:, 8:16], in_=imp3[:, tt, grp, :]), R=["imp3"], W=["m8b"])
                    P.op(dve, lambda tt=tt: nc.vector.tensor_scalar(out=selm[:, tt, grp * 32:(grp + 1) * 32], in0=imp2[:, tt, grp, :],
                                                                    scalar1=m8[:, 15:16], scalar2=None, op0=ALU.is_ge),
                         R=["imp2", "m8b"], W=["selm"])
                if STOP[0] == 5:
                    raise StopBuild()
                if grp == 0:
                    P.op(pool, lambda: nc.gpsimd.memset(selm[:, :, 32:64], 0.0), W=["selm"])
                for tt in range(4):
                    P.op(pe, lambda tt=tt: nc.tensor.transpose(psb[0:64, tt * 128:(tt + 1) * 128], selm[:, tt, :], ident_b),
                         R=["selm", "misc_b"], W=["psb"])
                P.op(act, lambda: nc.scalar.copy(out=selT[:], in_=psb[0:64, 0:512]), R=["psb"], W=["selT"])
                nkb = 4 * g + 4
                for j in range(nkb):
                    bm = bank()
                    P.op(pe, lambda j=j, bm=bm: nc.tensor.matmul(psf[:, bm, :], lhsT=E_b[:, grp, j * 128:(j + 1) * 128], rhs=selT[:], start=True, stop=True),
                         R=["E_b", "selT"], W=[("ps", bm)])
                    if j >= 4 * g:
                        P.op(dve, lambda j=j, bm=bm: nc.vector.tensor_tensor(out=maskT[:, j, :], in0=psf[:, bm, :], in1=cm_b[:, j - 4 * g, :], op=ALU.mult),
                             R=[("ps", bm), "cm_b"], W=["maskT"])
                    else:
                        evac(bm, maskT[:, j, :], "maskT")
                if STOP[0] == 6:
                    raise StopBuild()
                for h in heads:
                    c, base = h // 2, (h % 2) * 64
                    hl = h - grp * 6
                    blocks = [dict(kT=ksT2[base:base + 64, grp, j * 128:(j + 1) * 128], kkey="ksT2", v1=V1s[:, j, grp, :], vkey="V1s",
                                   mask=(maskT[:, j, :], "maskT")) for j in range(nkb)]
                    attn_head(qT[base:base + 64, c, :], "qT", blocks, gated_dst(h, 1, cmpo[base:base + 64, hl, :], "cmpo", oacc[base:base + 64, :], "oacc"))
                    j0 = max(0, 4 * g - 4)
                    order = [4 * g] + [j for j in range(j0, nkb) if j != 4 * g]
                    blocks = [dict(kT=kwT2[base:base + 64, grp, j * 128:(j + 1) * 128], kkey="kwT2", v1=V1w[:, j, grp, :], vkey="V1w",
                                   mask=(wm_b[:, j - 4 * g + 4, :], "wm_b")) for j in order]
                    attn_head(qT[base:base + 64, c, :], "qT", blocks, gated_dst(h, 2, oacc[base:base + 64, :], "oacc", oT[base:base + 64, c, :], "hT"))
            mem_attn()
            wo_ffn(s, li, g, xt, xkey, last)
            final_store(s, g, xt, xkey, last)

    try:
        for s in range(n_seq):
            for li in range(n_layers):
                src_d = xT_d if li == 0 else xs_d
                last = (li == n_layers - 1)
                if li % 2 == 0:
                    dsa_layer(s, li, src_d, last)
                else:
                    nsa_layer(s, li, src_d, last)
    except StopBuild:
        pass
    barrier()
    P.drain()
    return nc, P


def host_inputs(inputs, core, n_seq=SEQ_PER_CORE):
    x = inputs["x"][core * n_seq:(core + 1) * n_seq]
    mem = inputs["mem"][core * n_seq:(core + 1) * n_seq]
    m = {}
    m["xT"] = np.ascontiguousarray(np.transpose(x, (0, 2, 1)))
    m["memT"] = np.ascontiguousarray(np.transpose(mem, (0, 2, 1)))
    g = np.concatenate([inputs["attn_norm"], inputs["mem_norm"], inputs["ffn_norm"], inputs["final_norm"][None]], axis=0)
    m["gam"] = np.ascontiguousarray(g.reshape(7, 8, 128).transpose(2, 0, 1))
    m["ckvg"] = np.ascontiguousarray(inputs["dsa_ckv_norm"][0].reshape(128, 1))
    m["posk"] = np.ascontiguousarray(inputs["nsa_cmp_pos_k"][0].reshape(16, 128).T)
    m["posv"] = np.ascontiguousarray(inputs["nsa_cmp_pos_v"][0].reshape(16, 128).T)
    m["dsa_w_in"] = inputs["dsa_w_in"][0]
    m["nsa_w_in"] = inputs["nsa_w_in"][0]
    for i in range(2):
        m[f"mem_w_kv{i}"] = inputs["mem_w_kv"][i]
        m[f"w_o{i}"] = inputs["w_o"][i]
        m[f"ffn_w_in{i}"] = inputs["ffn_w_in"][i]
        m[f"ffn_w_down{i}"] = inputs["ffn_w_down"][i]
    m["cmp_k_w1"] = inputs["nsa_cmp_k_w1"][0]
    m["cmp_v_w1"] = inputs["nsa_cmp_v_w1"][0]
    m["dsa_w_uk"] = inputs["dsa_w_uk"][0]
    m["dsa_w_uv"] = inputs["dsa_w_uv"][0]
    m["cmp_k_w2"] = inputs["nsa_cmp_k_w2"][0]
    m["cmp_v_w2"] = inputs["nsa_cmp_v_w2"][0]
    for k, v in make_consts().items():
        m["c_" + k] = v
    return {k: np.ascontiguousarray(v, dtype=np.float32) for k, v in m.items()}


def build2(n_seq, n_layers=2):
    _, P1 = build(n_seq, n_layers)
    return build(n_seq, n_layers, needed=P1.waited)


def kernel(**inputs):
    inputs = {k: np.asarray(v) for k, v in inputs.items()}
    nc, _ = build2(SEQ_PER_CORE)
    in_maps = [host_inputs(inputs, c) for c in range(NCORES)]
    res = run_bass_kernel_spmd(nc, in_maps, core_ids=list(range(NCORES)))
    outs = [np.transpose(r["outT"], (0, 2, 1)) for r in res.results]
    return np.ascontiguousarray(np.concatenate(outs, axis=0)).astype(np.float32)
```

```python
import numpy as np
import concourse.bass as bass
import concourse.mybir as mybir
from concourse.bass_utils import run_bass_kernel_spmd

F32 = mybir.dt.float32
BF16 = mybir.dt.bfloat16
AF = mybir.ActivationFunctionType
ALU = mybir.AluOpType
AX = mybir.AxisListType

T = 2048
NG = 4
GT = 512
D = 1024
DFF = 2816
NCORES = 8
SEQ_PER_CORE = 4
EPS = 1e-6
SEM_L = 8000
N_DSEM = 24
NEG = -1.0e30

DSA_W = 1752
NSA_W = 1828


class Eng:
    def __init__(self, P, name, b, pe=False):
        self.P, self.name, self.b, self.pe = P, name, b, pe
        self.cnt = 0
        self.rank = 0
        self.rankmap = {}
        self.sems = []
        self.seen = {}

    def _sem(self, r):
        e = (r - 1) // SEM_L
        while len(self.sems) <= e:
            self.sems.append(self.P.new_sem(f"{self.name}{len(self.sems)}"))
        return self.sems[e], (r - 1) % SEM_L + 1

    def sem_for(self, c):
        return self._sem(self.rankmap[c])


class DSem:
    def __init__(self, P, i):
        self.sem = P.new_sem(f"dma{i}")
        self.cnt = 0
        self.pe = False
        self.name = f"dma{i}"

    def sem_for(self, c):
        return self.sem, 16 * c


class Prog:
    def __init__(self, nc, needed=None):
        self.nc = nc
        self.nsem = 0
        self.needed = needed
        self.waited = set()
        self.pe = Eng(self, "pe", nc.tensor, pe=True)
        self.act = Eng(self, "act", nc.scalar)
        self.dve = Eng(self, "dve", nc.vector)
        self.pool = Eng(self, "pool", nc.gpsimd)
        self.sp = Eng(self, "sp", nc.sync)
        self.dsems = [DSem(self, i) for i in range(N_DSEM)]
        self.di = 0
        self.R = {}
        self.nins = 0
        self.nwait = 0
        self.ninc = 0

    def new_sem(self, name):
        self.nsem += 1
        return self.nc.semaphore(name).__enter__()

    def _wait(self, eng, obj, c):
        if eng.seen.get(obj, 0) >= c:
            return
        if isinstance(obj, Eng):
            self.waited.add((obj.name, c))
        sem, v = obj.sem_for(c)
        eng.b.wait_ge(sem, v)
        eng.seen[obj] = c
        self.nwait += 1

    def _deps(self, eng, reads, writes, nowaw=False):
        for k in reads:
            r = self.R.get(k)
            if r is None:
                continue
            for obj, c in r[0].items():
                if obj is eng and eng.pe:
                    continue
                self._wait(eng, obj, c)
        for k in writes:
            r = self.R.get(k)
            if r is None:
                continue
            if not nowaw:
                for obj, c in r[0].items():
                    if obj is eng and eng.pe:
                        continue
                    self._wait(eng, obj, c)
            for obj, c in r[1].items():
                if obj is eng and eng.pe:
                    continue
                self._wait(eng, obj, c)

    def _mark(self, obj, c, reads, writes):
        for k in reads:
            r = self.R.get(k)
            if r is None:
                r = self.R[k] = [{}, {}]
            r[1][obj] = c
        for k in writes:
            r = self.R.get(k)
            if r is None or r[1]:
                self.R[k] = [{obj: c}, {}]
            else:
                r[0][obj] = c

    def op(self, eng, fn, R=(), W=()):
        self._deps(eng, R, W)
        ins = fn()
        eng.cnt += 1
        if self.needed is None or (eng.name, eng.cnt) in self.needed:
            eng.rank += 1
            sem, _ = eng._sem(eng.rank)
            ins.then_inc(sem, 1)
            self.ninc += 1
        eng.rankmap[eng.cnt] = eng.rank
        self._mark(eng, eng.cnt, R, W)
        self.nins += 1
        return ins

    def dma(self, out, in_, R=(), W=(), q=None, nowaw=True):
        eng = q or self.sp
        d = self.dsems[self.di % N_DSEM]
        self.di += 1
        if d.cnt > 0:
            self._wait(eng, d, d.cnt)
        self._deps(eng, R, W, nowaw=nowaw)
        ins = eng.b.dma_start(out=out, in_=in_)
        d.cnt += 1
        ins.then_inc(d.sem, 16)
        self._mark(d, d.cnt, R, W)
        self.nins += 1
        return ins

    def drain(self):
        for d in self.dsems:
            if d.cnt > 0:
                self._wait(self.sp, d, d.cnt)


class Ring:
    def __init__(self, items):
        self.items = items
        self.i = 0

    def next(self):
        it = self.items[self.i % len(self.items)]
        self.i += 1
        return it


def _rope_tables():
    inv = 500000.0 ** (-np.arange(0, 16, 2, dtype=np.float64) / 16)
    ang = np.arange(T, dtype=np.float64)[:, None] * inv[None, :]
    cos, sin = np.cos(ang).astype(np.float32), np.sin(ang).astype(np.float32)
    C = np.ones((128, T), np.float32)
    S = np.zeros((128, T), np.float32)
    for r in range(128):
        d = r % 64
        if d < 8:
            C[r] = cos[:, d]
            S[r] = -sin[:, d]
        elif d < 16:
            C[r] = cos[:, d - 8]
            S[r] = sin[:, d - 8]
    return C, S


def make_consts():
    c = {}
    C, S = _rope_tables()
    c["ropeC"], c["ropeS"] = C, S
    perm = np.zeros((128, 128), np.float32)
    for m in range(128):
        d = m % 64
        base = m - d
        if d < 8:
            perm[base + d + 8, m] = 1.0
        elif d < 16:
            perm[base + d - 8, m] = 1.0
    ident = np.eye(128, dtype=np.float32)
    sl = np.arange(128)
    cb = np.where(sl[None, :] > sl[:, None], NEG, 0.0).astype(np.float32)
    c["misc128"] = np.stack([perm, ident, np.ones((128, 128), np.float32), cb], axis=1)
    tl = np.arange(512)
    cm = np.zeros((128, 4, 512), np.float32)
    for r in range(4):
        cm[:, r, :] = ((128 * r + sl)[:, None] <= tl[None, :]).astype(np.float32)
    c["cm"] = cm
    wm = np.zeros((128, 8, 512), np.float32)
    for j in range(8):
        s = 128 * (j - 4) + sl
        wm[:, j, :] = ((s[:, None] <= tl[None, :]) & (s[:, None] > tl[None, :] - 512)).astype(np.float32)
    c["wm"] = wm
    cc = np.arange(128)
    mc = ((16 * cc + 31)[:, None] <= np.arange(T)[None, :]).astype(np.float32)
    mc[127] = 0.0
    c["mcT"] = mc
    c0 = cc * 16
    s0 = np.arange(32) * 64
    ov = np.minimum(c0[:, None] + 32, s0[None, :] + 64) - np.maximum(c0[:, None], s0[None, :])
    ov = (np.clip(ov, 0, None) / 32.0).astype(np.float32)
    ov33 = np.concatenate([ov, np.ones((128, 1), np.float32)], axis=1)
    ov33[127] = 0.0
    c["ov33"] = ov33
    tt = np.arange(T)
    cur = tt // 64
    n = np.arange(32)
    forced = (n[None] == 0) | (n[None] == cur[:, None]) | (n[None] == cur[:, None] - 1)
    adm = n[None] <= cur[:, None]
    Ff = np.where(forced, 1.0e9, 0.0).astype(np.float32)
    Ab = np.where(adm, 0.0, NEG).astype(np.float32)
    pl = (np.arange(128) >= 64).astype(np.int64)
    y = np.arange(62) - 30
    c["selF"] = np.where((y[None] == pl[:, None]) | (y[None] == pl[:, None] - 1), 1.0e9, 0.0).astype(np.float32)
    c["selA"] = np.where(y[None] <= pl[:, None], 0.0, NEG).astype(np.float32)
    E = np.zeros((64, 2, T), np.float32)
    for g in range(2):
        for nn in range(32):
            E[g * 32 + nn, g, nn * 64:(nn + 1) * 64] = 1.0
    c["Eexp"] = E
    c["bis"] = np.tile((2.0 ** -(np.arange(32) + 1)).astype(np.float32)[None, :], (128, 1))
    return c


WSPEC = [
    ("dsa_w_in", 1024, DSA_W), ("nsa_w_in", 1024, NSA_W),
    ("mem_w_kv0", 1024, 512), ("mem_w_kv1", 1024, 512),
    ("w_o0", 1024, 1024), ("w_o1", 1024, 1024),
    ("ffn_w_in0", 1024, 2 * DFF), ("ffn_w_in1", 1024, 2 * DFF),
    ("ffn_w_down0", DFF, 1024), ("ffn_w_down1", DFF, 1024),
    ("cmp_k_w1", 2048, 128), ("cmp_v_w1", 2048, 128),
]
SMALLW = [("dsa_w_uk", 128, 48), ("dsa_w_uv", 128, 64), ("cmp_k_w2", 128, 64), ("cmp_v_w2", 128, 64)]
CONST_SHAPES = None


class StopBuild(Exception):
    pass


STOP = [0]


def build(n_seq, n_layers=2, needed=None):
    nc = bass.Bass("TRN2", target_bir_lowering=False)
    P = Prog(nc, needed)
    pe, act, dve, pool = P.pe, P.act, P.dve, P.pool
    consts = make_consts()

    def din(name, shape, dt=F32):
        return nc.dram_tensor(name, list(shape), dt, kind="ExternalInput").ap()

    xT_d = din("xT", [n_seq, D, T])
    memT_d = din("memT", [n_seq, D, 256])
    gam_d = din("gam", [128, 7, 8])
    ckvg_d = din("ckvg", [128, 1])
    posk_d = din("posk", [128, 16])
    posv_d = din("posv", [128, 16])
    w_d = {n: din(n, [K, N]) for n, K, N in WSPEC}
    sw_d = {n: din(n, [K, N]) for n, K, N in SMALLW}
    c_d = {k: din("c_" + k, v.shape) for k, v in consts.items()}
    outT_d = nc.dram_tensor("outT", [n_seq, D, T], F32, kind="ExternalOutput").ap()
    xs_d = nc.dram_tensor("xs", [n_seq, D, T], F32).ap()
    wb_d = {n: nc.dram_tensor("b_" + n, [K, N], BF16).ap() for n, K, N in WSPEC}

    ARENA_BYTES = 212000
    arena = nc.alloc_sbuf_tensor("arena", [128, ARENA_BYTES // 4], F32)
    a_base = nc.lookup_mloc(arena).addr
    a_ptr = [0]
    a_max = [0]
    DT_SZ = {F32: 4, BF16: 2}

    def sb(name, shape, dt=F32):
        nbytes = int(np.prod(shape[1:])) * DT_SZ[dt]
        off = (a_ptr[0] + 31) // 32 * 32
        a_ptr[0] = off + nbytes
        a_max[0] = max(a_max[0], a_ptr[0])
        assert a_ptr[0] <= ARENA_BYTES, (name, a_ptr[0])
        return nc.alloc_sbuf_tensor_at("s_" + name, list(shape), dt, offset=a_base + off)

    def barrier():
        engs = [P.pe, P.act, P.dve, P.pool, P.sp]
        for e in engs:
            for o in engs:
                if o is not e and o.cnt > 0:
                    P._wait(e, o, o.cnt)
            for d_ in P.dsems:
                if d_.cnt > 0:
                    P._wait(e, d_, d_.cnt)

    xg = [sb("xg0", [128, 8, GT])]
    wbuf = [sb(f"wbuf{i}", [128, 8, 512], BF16) for i in range(2)]
    xg_flat = xg[0][:].rearrange("p a b -> p (a b)")
    stage = [xg_flat[:, i * 2048:(i + 1) * 2048] for i in range(2)]
    stage_b = [wbuf[i][:].rearrange("p a b -> p (a b)")[:, 0:2048] for i in range(2)]
    SCH = 2048
    misc_f = sb("misc_f", [128, 4, 128])
    misc_b = sb("misc_b", [128, 3, 128], BF16)
    cm_b = sb("cm_b", [128, 4, 512], BF16)
    wm_b = sb("wm_b", [128, 8, 512], BF16)
    mc_b = sb("mc_b", [128, T], BF16)
    ov_b = sb("ov_b", [128, 33], BF16)
    selF = sb("selF", [128, 62])
    selA = sb("selA", [128, 62])
    E_b = sb("E_b", [64, 2, T], BF16)
    bis = sb("bis", [128, 32])
    gam = sb("gam", [128, 7, 8])
    ckvg = sb("ckvg", [128, 1])
    cvt_i = [0]
    epsT = sb("epsT", [128, 1])
    P.op(pool, lambda: nc.gpsimd.memset(epsT[:], EPS), W=["epsT"])

    def cvt(dst_ap, src_ap, R, W):
        engs = [dve, pool, act]
        e = engs[cvt_i[0] % 3]
        cvt_i[0] += 1
        if e is act:
            P.op(e, lambda: nc.scalar.copy(out=dst_ap, in_=src_ap), R=R, W=W)
        else:
            P.op(e, lambda: e.b.tensor_copy(out=dst_ap, in_=src_ap), R=R, W=W)

    def load_const(dst, src_d, key, bf=False, npart=128, cols=None):
        if not bf:
            P.dma(dst[:], src_d, W=[key])
            return
        shp = list(src_d.shape)
        flat = int(np.prod(shp[1:]))
        srcf = src_d if len(shp) == 2 else src_d.rearrange("p a b -> p (a b)")
        dstf = dst[:] if len(shp) == 2 else dst[:].rearrange("p a b -> p (a b)")
        for o in range(0, flat, SCH):
            n = min(SCH, flat - o)
            i = cvt_i[0] % 2
            P.dma(stage[i][0:shp[0], 0:n], srcf[:, o:o + n], W=[("stage", i)], nowaw=False)
            cvt(dstf[:, o:o + n], stage[i][0:shp[0], 0:n], R=[("stage", i)], W=[key])

    load_const(misc_f, c_d["misc128"], "misc_f")
    P.op(dve, lambda: nc.vector.tensor_copy(out=misc_b[:], in_=misc_f[:, 0:3, :]), R=["misc_f"], W=["misc_b"])
    load_const(cm_b, c_d["cm"], "cm_b", bf=True)
    load_const(wm_b, c_d["wm"], "wm_b", bf=True)
    load_const(mc_b, c_d["mcT"], "mc_b", bf=True)
    load_const(ov_b, c_d["ov33"], "ov_b", bf=True)
    load_const(selF, c_d["selF"], "selF")
    load_const(selA, c_d["selA"], "selA")
    load_const(E_b, c_d["Eexp"], "E_b", bf=True)
    load_const(bis, c_d["bis"], "bis")
    load_const(gam, gam_d, "gam")
    load_const(ckvg, ckvg_d, "ckvg")
    perm_b, ident_b, ones_b = misc_b[:, 0, :], misc_b[:, 1, :], misc_b[:, 2, :]
    E_flat = E_b[:].rearrange("p g s -> p (g s)")
    ones_f = misc_f[:, 2, :]
    cbias_f = misc_f[:, 3, :]

    for name, K, N in WSPEC:
        for kc in range(K // 128):
            for o in range(0, N, SCH):
                n = min(SCH, N - o)
                i = cvt_i[0] % 2
                P.dma(stage[i][:, 0:n], w_d[name][kc * 128:(kc + 1) * 128, o:o + n], W=[("stage", i)], nowaw=False)
                j = i
                cvt(stage_b[j][:, 0:n], stage[i][:, 0:n], R=[("stage", i)], W=[("stageb", j)])
                P.dma(wb_d[name][kc * 128:(kc + 1) * 128, o:o + n], stage_b[j][:, 0:n],
                      R=[("stageb", j)], W=[("wb", name)])
    wuk2 = sb("wuk2", [128, 128], BF16)
    wuv = sb("wuv", [128, 64], BF16)
    w2k2 = sb("w2k2", [128, 128], BF16)
    w2v = sb("w2v", [128, 64], BF16)
    smst = sb("smst", [128, 4, 64])
    P.op(pool, lambda: nc.gpsimd.memset(wuk2[:], 0.0), W=["wuk2"])
    P.dma(smst[:, 0, 0:48], sw_d["dsa_w_uk"], W=["smst"])
    P.dma(smst[:, 1, :], sw_d["dsa_w_uv"], W=["smst"])
    P.dma(smst[:, 2, :], sw_d["cmp_k_w2"], W=["smst"])
    P.dma(smst[:, 3, :], sw_d["cmp_v_w2"], W=["smst"])
    P.op(dve, lambda: nc.vector.tensor_copy(out=wuk2[:, 16:64], in_=smst[:, 0, 0:48]), R=["smst"], W=["wuk2"])
    P.op(dve, lambda: nc.vector.tensor_copy(out=wuk2[:, 80:128], in_=smst[:, 0, 0:48]), R=["smst"], W=["wuk2"])
    P.op(dve, lambda: nc.vector.tensor_copy(out=wuv[:], in_=smst[:, 1, :]), R=["smst"], W=["wuv"])
    P.op(dve, lambda: nc.vector.tensor_copy(out=w2k2[:, 0:64], in_=smst[:, 2, :]), R=["smst"], W=["w2k2"])
    P.op(dve, lambda: nc.vector.tensor_copy(out=w2k2[:, 64:128], in_=smst[:, 2, :]), R=["smst"], W=["w2k2"])
    P.op(dve, lambda: nc.vector.tensor_copy(out=w2v[:], in_=smst[:, 3, :]), R=["smst"], W=["w2v"])
    posf = sb("posf", [128, 2, 16])
    posb = sb("posb", [128, 2, 16], BF16)
    P.dma(posf[:, 0, :], posk_d, W=["posf"])
    P.dma(posf[:, 1, :], posv_d, W=["posf"])
    P.op(dve, lambda: nc.vector.tensor_copy(out=posb[:], in_=posf[:]), R=["posf"], W=["posb"])

    barrier()
    psf = nc.alloc_psum_tensor("psf", [128, 7, 512], F32)
    psb = nc.alloc_psum_tensor("psb", [128, 1024], BF16)
    bank_i = [0]

    def bank():
        b = bank_i[0] % 4
        bank_i[0] += 1
        return b

    bankL_i = [0]

    def bankL():
        b = 4 + bankL_i[0] % 3
        bankL_i[0] += 1
        return b

    def bank2():
        if not hasattr(bank2, "i"):
            bank2.i = 0
        b = (bank2.i % 2) * 2
        bank2.i += 1
        return b

    xg.append(sb("xg1", [128, 8, GT]))
    xg_i = [0]
    wbuf_i = [0]
    hT = sb("hT", [128, 8, GT], BF16)
    oT = hT
    sq = [sb(f"sq{i}", [128, GT]) for i in range(1)]
    sq_i = [0]
    sqh = [sb(f"sqh{i}", [128, 2, GT], BF16) for i in range(1)]
    sqh_i = [0]
    rstd = sb("rstd", [128, GT])
    wdn = [sb(f"wdn{i}", [128, 11, 128], BF16) for i in range(2)]
    wdn_i = [0]
    ropeCs = sb("ropeCs", [128, GT])
    ropeSs = sb("ropeSs", [128, GT])
    pre = [sb(f"pre{i}", [128, GT], BF16) for i in range(1)]
    pre_i = [0]
    rtmp = [sb(f"rtmp{i}", [128, GT]) for i in range(2)]
    rtmp_i = [0]
    qT = sb("qT", [128, 6, GT], BF16)
    qmemT = sb("qmemT", [128, 2, GT], BF16)
    mk_mark = a_ptr[0]
    maskT = sb("maskT", [128, 16, GT], BF16)
    mk_end = a_ptr[0]
    a_ptr[0] = mk_mark
    actT = sb("maskT", [128, 11, GT], BF16)
    a_ptr[0] = mk_mark
    memnT = sb("maskT", [128, 8, 256], BF16)
    a_ptr[0] = mk_end
    memf = xg[0][:, :, 0:256]
    sil = [sb(f"sil{i}", [128, GT], BF16) for i in range(1)]
    sil_i = [0]
    kmemT = sb("kmemT", [128, 2, 256], BF16)
    v1m = sb("v1m", [128, 2, 4, 128], BF16)
    PT = [sb(f"PT{i}", [128, GT], BF16) for i in range(4)]
    PT_i = [0]
    rd = [sb(f"rd{i}", [128, GT]) for i in range(2)]
    rd_i = [0]
    layer_mark = a_ptr[0]
    KT2 = sb("KT2", [128, T], BF16)
    kidxT2 = sb("kidxT2", [128, T], BF16)
    V1 = sb("V1", [128, 16, 128], BF16)
    ckvn = sb("ckvn", [128, GT], BF16)
    ckvf = sb("ckvf", [128, GT])
    wkr2 = sb("wkr2", [128, 8, 128], BF16)
    wki2 = sb("wki2", [128, 8, 128], BF16)
    widx = sb("widx", [128, 4, 8])
    Dh = [sb(f"Dh{i}", [128, 8, 128], BF16) for i in range(2)]
    Dh_i = [0]
    relu_t = [sb(f"relu{i}", [128, 512], BF16) for i in range(3)]
    relu_i = [0]
    sc = [sb(f"sc{i}", [128, T]) for i in range(2)]
    sc_i = [0]
    bst = sb("bst", [128, 2, 8])
    wtab = sb("wtab", [128, 2, 32])
    wtab2 = sb("wtab2", [128, 2, 32])
    mask_tm = [sb(f"masktm{i}", [128, T], BF16) for i in range(2)]
    mask_i = [0]
    qidxT = sb("qidxT", [128, 4, GT], BF16)
    dsa_end = a_ptr[0]
    a_ptr[0] = layer_mark
    ksT2 = sb("ksT2", [128, 2, T], BF16)
    kwT2 = sb("kwT2", [128, 2, T], BF16)
    V1s = sb("V1s", [128, 16, 2, 128], BF16)
    V1w = sb("V1w", [128, 16, 2, 128], BF16)
    ph_mark = a_ptr[0]
    kcT = sb("kcT", [128, T], BF16)
    vcT = sb("vcT", [128, T], BF16)
    ph_end = a_ptr[0]
    a_ptr[0] = ph_mark
    cmpo = sb("cmpo", [128, 6, GT], BF16)
    oacc = sb("oacc", [128, GT])
    a_ptr[0] = max(a_ptr[0], ph_end)
    cb_b = sb("cb_b", [128, 2])
    kcmpT2 = sb("kcmpT2", [128, 2, 128], BF16)
    V1c = sb("V1c", [128, 2, 128], BF16)
    gel = [sb(f"gel{i}", [128, 128]) for i in range(4)]
    gT = sb("gT", [128, 128], BF16)
    qrawT = sb("qrawT", [128, 6, GT], BF16)
    sigT = sb("sigT", [36, GT])
    sigH = sb("sigH", [36, GT], BF16)
    sigL = sb("sigL", [36, GT], BF16)
    imp = sb("imp", [128, 4, 2, 32])
    imp2 = sb("imp2", [128, 4, 2, 32])
    imp3 = sb("imp3", [128, 4, 2, 32])
    m8 = sb("m8", [128, 16])
    selm = sb("selm", [128, 4, 64], BF16)
    selT = sb("selT", [64, GT], BF16)
    rec = sb("rec", [128, 8])
    otmp = [sb(f"otmp{i}", [128, GT]) for i in range(1)]
    otmp_i = [0]
    rg = [sb(f"rg{i}", [64, GT]) for i in range(2)]
    rg_i = [0]
    nsa_end = a_ptr[0]
    print("SBUF plan: dsa_end", dsa_end, "nsa_end", nsa_end, "limit", ARENA_BYTES)
    a_ptr[0] = max(dsa_end, nsa_end)

    def rot(lst, ctr):
        i = ctr[0] % len(lst)
        ctr[0] += 1
        return i

    def load_w(name, col0, ncols, nk=8):
        i = rot(wbuf, wbuf_i)
        src = wb_d[name].rearrange("(c p) n -> p c n", p=128)[:, 0:nk, col0:col0 + ncols]
        P.dma(wbuf[i][:, 0:nk, 0:ncols], src, R=[("wb", name)], W=[("wbuf", i)])
        return wbuf[i], ("wbuf", i)

    def sumsq_mm(b, src_ap, src_key, ncols, first, last):
        i = rot(sq, sq_i)
        P.op(act, lambda: nc.scalar.activation(out=sq[i][:, 0:ncols], in_=src_ap, func=AF.Square), R=[src_key], W=[("sq", i)])
        j = rot(sqh, sqh_i)
        P.op(dve, lambda: nc.vector.tensor_copy(out=sqh[j][:, 0, 0:ncols], in_=sq[i][:, 0:ncols]), R=[("sq", i)], W=[("sqh", j)])
        P.op(pool, lambda: nc.gpsimd.tensor_tensor(out=sqh[j][:, 1, 0:ncols], in0=sq[i][:, 0:ncols], in1=sqh[j][:, 0, 0:ncols], op=ALU.subtract),
             R=[("sq", i), ("sqh", j)], W=[("sqh", j)])
        P.op(pe, lambda: nc.tensor.matmul(psf[:, b, 0:ncols], lhsT=ones_b, rhs=sqh[j][:, 0, 0:ncols], start=first, stop=False),
             R=[("sqh", j), "misc_b"], W=[("ps", b)])
        P.op(pe, lambda: nc.tensor.matmul(psf[:, b, 0:ncols], lhsT=ones_b, rhs=sqh[j][:, 1, 0:ncols], start=False, stop=last),
             R=[("sqh", j), "misc_b"], W=[("ps", b)])

    def norm_group(xt, xkey, gidx, ncols=GT, dst=None, dkey="hT"):
        dst = hT if dst is None else dst
        b = bank()
        for c in range(8):
            sumsq_mm(b, xt[:, c, 0:ncols], xkey, ncols, c == 0, c == 7)
        P.op(act, lambda: nc.scalar.activation(out=rstd[:, 0:ncols], in_=psf[:, b, 0:ncols], func=AF.Sqrt,
                                               scale=1.0 / D, bias=epsT[:, 0:1]),
             R=[("ps", b)], W=["rstd"])
        P.op(dve, lambda: nc.vector.reciprocal(out=rstd[:, 0:ncols], in_=rstd[:, 0:ncols]), R=["rstd"], W=["rstd"])
        for c in range(8):
            P.op(dve, lambda c=c: nc.vector.scalar_tensor_tensor(
                out=dst[:, c, 0:ncols], in0=xt[:, c, 0:ncols], scalar=gam[:, gidx, c:c + 1],
                in1=rstd[:, 0:ncols], op0=ALU.mult, op1=ALU.mult),
                 R=[xkey, "rstd", "gam"], W=[dkey])

    def proj(lhs_fn, lhs_keys, rhs_t, rhs_key, ncols, M=128):
        b = bank()
        for kc in range(8):
            P.op(pe, lambda kc=kc: nc.tensor.matmul(psf[0:M, b, 0:ncols], lhsT=lhs_fn(kc), rhs=rhs_t[:, kc, 0:ncols],
                                                    start=(kc == 0), stop=(kc == 7)),
                 R=list(lhs_keys) + [rhs_key], W=[("ps", b)])
        return b

    def rope_from_bank(b, dst_ap, dkey, ncols=GT, raw=None):
        i = rot(pre, pre_i)
        P.op(act, lambda: nc.scalar.copy(out=pre[i][:, 0:ncols], in_=psf[:, b, 0:ncols]), R=[("ps", b)], W=[("pre", i)])
        if raw is not None:
            P.op(pool, lambda: nc.gpsimd.tensor_copy(out=raw[0], in_=pre[i][:, 0:ncols]), R=[("pre", i)], W=[raw[1]])
        b2 = bank()
        P.op(pe, lambda: nc.tensor.matmul(psf[:, b2, 0:ncols], lhsT=perm_b, rhs=pre[i][:, 0:ncols], start=True, stop=True),
             R=[("pre", i), "misc_b"], W=[("ps", b2)])
        j = rot(rtmp, rtmp_i)
        P.op(dve, lambda: nc.vector.tensor_tensor(out=rtmp[j][:, 0:ncols], in0=psf[:, b2, 0:ncols], in1=ropeSs[:, 0:ncols], op=ALU.mult),
             R=[("ps", b2), "ropeS"], W=[("rtmp", j)])
        k = rot(rtmp, rtmp_i)
        P.op(pool, lambda: nc.gpsimd.tensor_tensor(out=rtmp[k][:, 0:ncols], in0=pre[i][:, 0:ncols], in1=ropeCs[:, 0:ncols], op=ALU.mult),
             R=[("pre", i), "ropeC"], W=[("rtmp", k)])
        P.op(pool, lambda: nc.gpsimd.tensor_tensor(out=dst_ap, in0=rtmp[j][:, 0:ncols], in1=rtmp[k][:, 0:ncols], op=ALU.add),
             R=[("rtmp", j), ("rtmp", k)], W=[dkey])

    def evac(b, dst_ap, dkey, ncols=GT, M=128, eng=None):
        e = eng or act
        if e is act:
            P.op(act, lambda: nc.scalar.copy(out=dst_ap, in_=psf[0:M, b, 0:ncols]), R=[("ps", b)], W=[dkey])
        else:
            P.op(e, lambda: e.b.tensor_copy(out=dst_ap, in_=psf[0:M, b, 0:ncols]), R=[("ps", b)], W=[dkey])

    def load_rope(g):
        P.dma(ropeCs[:], c_d["ropeC"][:, g * GT:(g + 1) * GT], W=["ropeC"])
        P.dma(ropeSs[:], c_d["ropeS"][:, g * GT:(g + 1) * GT], W=["ropeS"])

    def load_x(src_d, s, g):
        i = rot(xg, xg_i)
        src = src_d[s].rearrange("(c p) t -> p c t", p=128)
        for c in range(0, 8, 2):
            P.dma(xg[i][:, c:c + 2, :], src[:, c:c + 2, g * GT:(g + 1) * GT], W=[("xg", i)])
        return xg[i], ("xg", i)

    class XChain:
        def __init__(self, src_d, s):
            self.src_d, self.s = src_d, s
            self.order = list(range(NG)) + list(range(NG))
            self.k = 0
            self.pending = load_x(src_d, s, self.order[0])

        def get(self):
            cur = self.pending
            self.k += 1
            self.pending = load_x(self.src_d, self.s, self.order[self.k]) if self.k < len(self.order) else None
            return cur

    def attn_head(q_ap, q_key, blocks, dst_fn, scale=0.125, guard=False):
        bo = bankL()
        nb = len(blocks)
        pend = []
        for idx, blk in enumerate(blocks):
            bs = bank()
            P.op(pe, lambda: nc.tensor.matmul(psf[:, bs, :], lhsT=blk["kT"], rhs=q_ap, start=True, stop=True),
                 R=[blk["kkey"], q_key], W=[("ps", bs)])
            pi = rot(PT, PT_i)
            P.op(act, lambda: nc.scalar.activation(out=PT[pi][:], in_=psf[:, bs, :], func=AF.Exp, scale=scale),
                 R=[("ps", bs)], W=[("PT", pi)])
            if blk.get("mask") is not None:
                m_ap, m_key = blk["mask"]
                P.op(dve, lambda: nc.vector.tensor_tensor(out=PT[pi][:], in0=PT[pi][:], in1=m_ap, op=ALU.mult),
                     R=[("PT", pi), m_key], W=[("PT", pi)])
            pend.append((blk, pi, idx))
            if len(pend) > 2:
                _pv(pend.pop(0), bo, nb)
        while pend:
            _pv(pend.pop(0), bo, nb)
        ri = rot(rd, rd_i)
        if guard:
            P.op(dve, lambda: nc.vector.tensor_scalar_max(out=rd[ri][0:64, :], in0=psf[0:64, bo, :], scalar1=1e-18),
                 R=[("ps", bo)], W=[("rd", ri)])
            P.op(act, lambda: nc.scalar.activation(out=rd[ri][0:64, :], in_=rd[ri][0:64, :], func=AF.Ln), R=[("rd", ri)], W=[("rd", ri)])
        else:
            P.op(act, lambda: nc.scalar.activation(out=rd[ri][0:64, :], in_=psf[0:64, bo, :], func=AF.Ln), R=[("ps", bo)], W=[("rd", ri)])
        P.op(act, lambda: nc.scalar.activation(out=rd[ri][0:64, :], in_=rd[ri][0:64, :], func=AF.Exp, scale=-1.0), R=[("rd", ri)], W=[("rd", ri)])
        dst_fn(bo, rd[ri][0:64, :], ("rd", ri))

    def _pv(item, bo, nb):
        blk, pi, idx = item
        P.op(pe, lambda: nc.tensor.matmul(psf[:, bo, :], lhsT=blk["v1"], rhs=PT[pi][:], start=(idx == 0), stop=(idx == nb - 1)),
             R=[blk["vkey"], ("PT", pi)], W=[("ps", bo)])

    def plain_dst(chunk, base):
        def f(bo, rden, rkey):
            P.op(dve, lambda: nc.vector.tensor_tensor(out=oT[base:base + 64, chunk, :], in0=psf[64:128, bo, :], in1=rden, op=ALU.mult),
                 R=[("ps", bo), rkey], W=["hT"])
        return f

    def mem_kv(s, li):
        for c in range(0, 8, 4):
            P.dma(memf[:, c:c + 4, :], memT_d[s].rearrange("(c p) t -> p c t", p=128)[:, c:c + 4, :], W=[("xg", 0)])
        norm_group(memf, ("xg", 0), 2 + li, ncols=256, dst=memnT, dkey="maskT")
        wname = f"mem_w_kv{li}"
        wt, wk = load_w(wname, 0, 512)
        for c in range(2):
            b = proj(lambda kc, c=c: wt[:, kc, c * 128:(c + 1) * 128], [wk], memnT, "maskT", 256)
            evac(b, kmemT[:, c, :], "kmemT", ncols=256)
        P.op(pool, lambda: nc.gpsimd.memset(v1m[:, :, :, 0:64], 1.0), W=["v1m"])
        for nb_ in range(2):
            b = bank()
            for kc in range(8):
                P.op(pe, lambda kc=kc: nc.tensor.matmul(psf[:, b, 0:256], lhsT=memnT[:, kc, nb_ * 128:(nb_ + 1) * 128],
                                                        rhs=wt[:, kc, 256:512], start=(kc == 0), stop=(kc == 7)),
                     R=["maskT", wk], W=[("ps", b)])
            P.op(act, lambda: nc.scalar.copy(out=v1m[:, nb_, :, 64:128], in_=psf[:, b, 0:256].rearrange("p (h d) -> p h d", h=4)),
                 R=[("ps", b)], W=["v1m"])

    def mem_attn():
        for h in range(4):
            c, base = h // 2, (h % 2) * 64
            blocks = [dict(kT=kmemT[base:base + 64, c, nb_ * 128:(nb_ + 1) * 128], kkey="kmemT",
                           v1=v1m[:, nb_, h, :], vkey="v1m", mask=None) for nb_ in range(2)]
            attn_head(qmemT[base:base + 64, c, :], "qmemT", blocks, plain_dst(6 + c, base))

    def wo_ffn(s, li, g, xt, xkey, last):
        for half in range(2):
            wt, wk = load_w(f"w_o{li}", half * 512, 512)
            for oc4 in range(4):
                oc = half * 4 + oc4
                b = proj(lambda kc, oc4=oc4, wt=wt: wt[:, kc, oc4 * 128:(oc4 + 1) * 128], [wk], oT, "hT", GT)
                P.op(dve, lambda oc=oc, b=b: nc.vector.tensor_tensor(out=xt[:, oc, :], in0=xt[:, oc, :], in1=psf[:, b, :], op=ALU.add),
                     R=[xkey, ("ps", b)], W=[xkey])
        norm_group(xt, xkey, 4 + li)
        wsrc = wb_d[f"ffn_w_in{li}"].rearrange("(c p) n -> p c n", p=128)
        dsrc = wb_d[f"ffn_w_down{li}"].rearrange("(c p) n -> p c n", p=128)
        for fh in range(2):
            for cl in range(11):
                ch = fh * 11 + cl
                i = rot(wbuf, wbuf_i)
                P.dma(wbuf[i][:, :, 0:128], wsrc[:, :, ch * 128:(ch + 1) * 128], R=[("wb", f"ffn_w_in{li}")], W=[("wbuf", i)])
                P.dma(wbuf[i][:, :, 128:256], wsrc[:, :, DFF + ch * 128:DFF + (ch + 1) * 128], R=[("wb", f"ffn_w_in{li}")], W=[("wbuf", i)])
                wgk = ("wbuf", i)
                bg = proj(lambda kc, i=i: wbuf[i][:, kc, 0:128], [wgk], hT, "hT", GT)
                bu = proj(lambda kc, i=i: wbuf[i][:, kc, 128:256], [wgk], hT, "hT", GT)
                si = rot(sil, sil_i)
                P.op(act, lambda bg=bg, si=si: nc.scalar.activation(out=sil[si][:], in_=psf[:, bg, :], func=AF.Silu),
                     R=[("ps", bg)], W=[("sil", si)])
                P.op(dve, lambda cl=cl, bu=bu, si=si: nc.vector.tensor_tensor(out=actT[:, cl, :], in0=psf[:, bu, :], in1=sil[si][:], op=ALU.mult),
                     R=[("ps", bu), ("sil", si)], W=["maskT"])
            for oc in range(8):
                i = rot(wdn, wdn_i)
                P.dma(wdn[i][:], dsrc[:, fh * 11:(fh + 1) * 11, oc * 128:(oc + 1) * 128], R=[("wb", f"ffn_w_down{li}")], W=[("wdn", i)])
                b = bankL()
                for kc in range(11):
                    P.op(pe, lambda kc=kc, i=i, b=b: nc.tensor.matmul(psf[:, b, :], lhsT=wdn[i][:, kc, :], rhs=actT[:, kc, :],
                                                                     start=(kc == 0), stop=(kc == 10)),
                         R=[("wdn", i), "maskT"], W=[("ps", b)])
                P.op(dve, lambda oc=oc, b=b: nc.vector.tensor_tensor(out=xt[:, oc, :], in0=xt[:, oc, :], in1=psf[:, b, :], op=ALU.add),
                     R=[xkey, ("ps", b)], W=[xkey])

    def final_store(s, g, xt, xkey, last):
        if last:
            b = bank()
            for c in range(8):
                sumsq_mm(b, xt[:, c, :], xkey, GT, c == 0, c == 7)
            P.op(act, lambda: nc.scalar.activation(out=rstd[:], in_=psf[:, b, :], func=AF.Sqrt, scale=1.0 / D, bias=epsT[:, 0:1]),
                 R=[("ps", b)], W=["rstd"])
            P.op(dve, lambda: nc.vector.reciprocal(out=rstd[:], in_=rstd[:]), R=["rstd"], W=["rstd"])
            for c in range(8):
                P.op(dve, lambda c=c: nc.vector.scalar_tensor_tensor(out=xt[:, c, :], in0=xt[:, c, :], scalar=gam[:, 6, c:c + 1],
                                                                      in1=rstd[:], op0=ALU.mult, op1=ALU.mult),
                     R=[xkey, "rstd", "gam"], W=[xkey])
            dst = outT_d[s].rearrange("(c p) t -> p c t", p=128)
            P.dma(dst[:, :, g * GT:(g + 1) * GT], xt[:], R=[xkey], W=["out"])
        else:
            dst = xs_d[s].rearrange("(c p) t -> p c t", p=128)
            P.dma(dst[:, :, g * GT:(g + 1) * GT], xt[:], R=[xkey], W=[("xs", s)])

    def dsa_layer(s, li, src_d, last):
        barrier()
        mem_kv(s, li)
        xc = XChain(src_d, s)
        wt, wk = load_w("dsa_w_in", 896, 16 + 0)
        P.op(pool, lambda: nc.gpsimd.memset(wkr2[:], 0.0), W=["wkr2"])
        P.op(pool, lambda: nc.gpsimd.tensor_copy(out=wkr2[:, :, 0:16], in_=wt[:, :, 0:16]), R=[wk], W=["wkr2"])
        P.op(pool, lambda: nc.gpsimd.tensor_copy(out=wkr2[:, :, 64:80], in_=wt[:, :, 0:16]), R=[wk], W=["wkr2"])
        wt2, wk2 = load_w("dsa_w_in", 1424, 64)
        P.op(pool, lambda: nc.gpsimd.tensor_copy(out=wki2[:, :, 0:64], in_=wt2[:, :, 0:64]), R=[wk2], W=["wki2"])
        P.op(pool, lambda: nc.gpsimd.tensor_copy(out=wki2[:, :, 64:128], in_=wt2[:, :, 0:64]), R=[wk2], W=["wki2"])
        P.op(pool, lambda: nc.gpsimd.memset(V1[:, :, 0:64], 1.0), W=["V1"])
        for g in range(NG):
            xt, xkey = xc.get()
            load_rope(g)
            norm_group(xt, xkey, 0 + li)
            wc, wck = load_w("dsa_w_in", 768, 128)
            b = proj(lambda kc: wc[:, kc, 0:128], [wck], hT, "hT", GT)
            evac(b, ckvf[:], "ckvf", eng=dve)
            b2 = bank()
            sumsq_mm(b2, ckvf[:], "ckvf", GT, True, True)
            P.op(act, lambda: nc.scalar.activation(out=rstd[:], in_=psf[:, b2, :], func=AF.Sqrt, scale=1.0 / 128, bias=epsT[:, 0:1]),
                 R=[("ps", b2)], W=["rstd"])
            P.op(dve, lambda: nc.vector.reciprocal(out=rstd[:], in_=rstd[:]), R=["rstd"], W=["rstd"])
            P.op(dve, lambda: nc.vector.scalar_tensor_tensor(out=ckvn[:], in0=ckvf[:], scalar=ckvg[:, 0:1], in1=rstd[:],
                                                             op0=ALU.mult, op1=ALU.mult),
                 R=["ckvf", "rstd", "ckvg"], W=["ckvn"])
            bk = bank()
            for kc in range(8):
                P.op(pe, lambda kc=kc: nc.tensor.matmul(psf[:, bk, :], lhsT=wkr2[:, kc, :], rhs=hT[:, kc, :], start=(kc == 0), stop=False),
                     R=["wkr2", "hT"], W=[("ps", bk)])
            P.op(pe, lambda: nc.tensor.matmul(psf[:, bk, :], lhsT=wuk2[:], rhs=ckvn[:], start=False, stop=True),
                 R=["wuk2", "ckvn"], W=[("ps", bk)])
            rope_from_bank(bk, KT2[:, g * GT:(g + 1) * GT], "KT2")
            bi = proj(lambda kc: wki2[:, kc, :], ["wki2"], hT, "hT", GT)
            rope_from_bank(bi, kidxT2[:, g * GT:(g + 1) * GT], "kidxT2")
            for tt in range(4):
                bv = bank()
                P.op(pe, lambda tt=tt: nc.tensor.matmul(psf[:, bv, 0:64], lhsT=ckvn[:, tt * 128:(tt + 1) * 128], rhs=wuv[:], start=True, stop=True),
                     R=["ckvn", "wuv"], W=[("ps", bv)])
                P.op(act, lambda tt=tt, bv=bv: nc.scalar.copy(out=V1[:, g * 4 + tt, 64:128], in_=psf[:, bv, 0:64]), R=[("ps", bv)], W=["V1"])
        for g in range(NG):
            xt, xkey = xc.get()
            load_rope(g)
            norm_group(xt, xkey, 0 + li)
            wq, wqk = load_w("dsa_w_in", 0, 512)
            for c in range(4):
                b = proj(lambda kc, c=c: wq[:, kc, c * 128:(c + 1) * 128], [wqk], hT, "hT", GT)
                rope_from_bank(b, qT[:, c, :], "qT")
            wq2, wq2k = load_w("dsa_w_in", 512, 256)
            for c in range(2):
                b = proj(lambda kc, c=c: wq2[:, kc, c * 128:(c + 1) * 128], [wq2k], hT, "hT", GT)
                rope_from_bank(b, qT[:, 4 + c, :], "qT")
            wi, wik = load_w("dsa_w_in", 912, 512)
            for c in range(4):
                b = proj(lambda kc, c=c: wi[:, kc, c * 128:(c + 1) * 128], [wik], hT, "hT", GT)
                rope_from_bank(b, qidxT[:, c, :], "qidxT")
            wm_, wmk = load_w("dsa_w_in", 1488, 264)
            for c in range(2):
                b = proj(lambda kc, c=c: wm_[:, kc, 8 + c * 128:8 + (c + 1) * 128], [wmk], hT, "hT", GT)
                evac(b, qmemT[:, c, :], "qmemT")
            for tt in range(4):
                b = bank()
                for kc in range(8):
                    P.op(pe, lambda kc=kc, tt=tt: nc.tensor.matmul(psf[:, b, 0:8], lhsT=hT[:, kc, tt * 128:(tt + 1) * 128], rhs=wm_[:, kc, 0:8],
                                                                   start=(kc == 0), stop=(kc == 7)),
                         R=["hT", wmk], W=[("ps", b)])
                P.op(act, lambda tt=tt, b=b: nc.scalar.mul(widx[:, tt, :], psf[:, b, 0:8], float(8 ** -0.5 * 64 ** -0.5)),
                     R=[("ps", b)], W=["widx"])
            P.op(pool, lambda: nc.gpsimd.memset(maskT[:], 0.0), W=["maskT"])
            def idx_phase(tt):
                qi = g * 4 + tt
                u = tt % 2
                Sc = 128 * (qi + 1)
                di = rot(Dh, Dh_i)
                for h in range(8):
                    P.op(dve, lambda h=h: nc.vector.tensor_scalar(out=Dh[di][:, h, :], in0=ident_b, scalar1=widx[:, tt, h:h + 1],
                                                                  scalar2=None, op0=ALU.mult),
                         R=["misc_b", "widx"], W=[("Dh", di)])
                si = rot(sc, sc_i)
                for k0 in range(0, Sc, 512):
                    kn = min(512, Sc - k0)
                    ba = bankL()
                    for h in range(8):
                        c, base = h // 2, (h % 2) * 64
                        bl = bank()
                        P.op(pe, lambda: nc.tensor.matmul(
                            psf[:, bl, 0:kn], lhsT=qidxT[base:base + 64, c, tt * 128:(tt + 1) * 128],
                            rhs=kidxT2[base:base + 64, k0:k0 + kn], start=True, stop=True),
                             R=["qidxT", "kidxT2"], W=[("ps", bl)])
                        ri = rot(relu_t, relu_i)
                        P.op(act, lambda: nc.scalar.activation(out=relu_t[ri][:, 0:kn], in_=psf[:, bl, 0:kn], func=AF.Relu),
                             R=[("ps", bl)], W=[("relu", ri)])
                        P.op(pe, lambda: nc.tensor.matmul(
                            psf[:, ba, 0:kn], lhsT=Dh[di][:, h, :], rhs=relu_t[ri][:, 0:kn], start=(h == 0), stop=(h == 7)),
                             R=[("Dh", di), ("relu", ri)], W=[("ps", ba)])
                    P.op(act, lambda: nc.scalar.copy(out=sc[si][:, k0:k0 + kn], in_=psf[:, ba, 0:kn]),
                         R=[("ps", ba)], W=[("sc", si)])
                scv = sc[si]
                skey = ("sc", si)
                B = lambda j: bst[:, u, j:j + 1]
                K = lambda j: ("bst", u, j)
                if qi >= 2:
                    P.op(dve, lambda: nc.vector.tensor_reduce(out=B(0), in_=scv[:, 0:Sc], axis=AX.X, op=ALU.min), R=[skey], W=[K(0)])
                    P.op(dve, lambda: nc.vector.tensor_reduce(out=B(1), in_=scv[:, 0:Sc], axis=AX.X, op=ALU.max), R=[skey], W=[K(1)])
                P.op(dve, lambda: nc.vector.tensor_tensor(out=scv[:, Sc - 128:Sc], in0=scv[:, Sc - 128:Sc], in1=cbias_f, op=ALU.add),
                     R=[skey, "misc_f", K(0), K(1)], W=[skey])
                mi = rot(mask_tm, mask_i)
                return dict(tt=tt, qi=qi, u=u, Sc=Sc, scv=scv, skey=skey, mi=mi)

            def bis_gen(st):
                u, Sc, scv, skey, mi = st["u"], st["Sc"], st["scv"], st["skey"], st["mi"]
                B = lambda j: bst[:, u, j:j + 1]
                K = lambda j: ("bst", u, j)
                wk, wk2 = ("wtab", u), ("wtab2", u)
                NIT = 16
                P.op(dve, lambda: nc.vector.tensor_tensor(out=B(2), in0=B(1), in1=B(0), op=ALU.subtract), R=[K(0), K(1)], W=[K(2)])
                P.op(dve, lambda: nc.vector.tensor_scalar(out=wtab[:, u, :], in0=bis[:], scalar1=B(2), scalar2=None, op0=ALU.mult),
                     R=["bis", K(2)], W=[wk])
                P.op(dve, lambda: nc.vector.tensor_scalar(out=wtab2[:, u, :], in0=bis[:], scalar1=B(2), scalar2=2.0, op0=ALU.mult, op1=ALU.mult),
                     R=["bis", K(2)], W=[wk2])
                P.op(dve, lambda: nc.vector.tensor_tensor(out=B(3), in0=B(0), in1=wtab[:, u, 0:1], op=ALU.add), R=[K(0), wk], W=[K(3)])
                yield
                for it in range(NIT):
                    P.op(dve, lambda: nc.vector.tensor_scalar(out=mask_tm[mi][:, 0:Sc], in0=scv[:, 0:Sc], scalar1=B(3), scalar2=0.0,
                                                              op0=ALU.is_ge, op1=ALU.add, accum_out=B(4)),
                         R=[skey, K(3)], W=[("masktm", mi), K(4)])
                    yield
                    if it < NIT - 1:
                        wn = wtab[:, u, it + 1:it + 2]
                        wn2 = wtab2[:, u, it + 1:it + 2]
                        P.op(dve, lambda: nc.vector.tensor_scalar(out=B(5), in0=B(4), scalar1=255.5, scalar2=wn2, op0=ALU.is_ge, op1=ALU.mult),
                             R=[K(4), wk2], W=[K(5)])
                        yield
                        P.op(dve, lambda: nc.vector.scalar_tensor_tensor(out=B(3), in0=B(5), scalar=wn, in1=B(3), op0=ALU.subtract, op1=ALU.add),
                             R=[K(5), K(3), wk], W=[K(3)])
                        yield
                    else:
                        wl = wtab[:, u, it:it + 1]
                        P.op(dve, lambda: nc.vector.tensor_scalar(out=B(5), in0=B(4), scalar1=255.5, scalar2=1.0, op0=ALU.is_ge, op1=ALU.subtract),
                             R=[K(4)], W=[K(5)])
                        yield
                        P.op(dve, lambda: nc.vector.scalar_tensor_tensor(out=B(6), in0=B(5), scalar=wl, in1=B(3), op0=ALU.mult, op1=ALU.add),
                             R=[K(5), K(3), wk], W=[K(6)])
                        yield

            def fin_phase(st):
                tt, qi, u, Sc, scv, skey, mi = st["tt"], st["qi"], st["u"], st["Sc"], st["scv"], st["skey"], st["mi"]
                if qi >= 2:
                    P.op(dve, lambda: nc.vector.tensor_scalar(out=mask_tm[mi][:, 0:Sc], in0=scv[:, 0:Sc], scalar1=bst[:, u, 6:7], scalar2=None, op0=ALU.is_ge),
                         R=[skey, ("bst", u, 6)], W=[("masktm", mi)])
                else:
                    P.op(dve, lambda: nc.vector.tensor_scalar(out=mask_tm[mi][:, 0:Sc], in0=scv[:, 0:Sc], scalar1=-1.0e29, scalar2=None, op0=ALU.is_ge),
                         R=[skey], W=[("masktm", mi)])
                nblk = qi + 1
                for j0 in range(0, nblk, 8):
                    jn = min(8, nblk - j0)
                    for j in range(j0, j0 + jn):
                        P.op(pe, lambda: nc.tensor.transpose(psb[:, (j - j0) * 128:(j - j0 + 1) * 128],
                                                             mask_tm[mi][:, j * 128:(j + 1) * 128], ident_b),
                             R=[("masktm", mi), "misc_b"], W=["psb"])
                    P.op(act, lambda: nc.scalar.copy(out=maskT[:, j0:j0 + jn, tt * 128:(tt + 1) * 128],
                                                     in_=psb[:, 0:jn * 128].rearrange("p (j t) -> p j t", j=jn)),
                         R=["psb"], W=["maskT"])

            for t0 in (0, 2):
                sts = [idx_phase(t0), idx_phase(t0 + 1)]
                gens = [bis_gen(st) for st in sts if st["qi"] >= 2]
                while gens:
                    for gg in list(gens):
                        try:
                            next(gg)
                        except StopIteration:
                            gens.remove(gg)
                for st in sts:
                    fin_phase(st)
            nkb = 4 * g + 4
            for h in range(12):
                c, base = h // 2, (h % 2) * 64
                blocks = [dict(kT=KT2[base:base + 64, j * 128:(j + 1) * 128], kkey="KT2", v1=V1[:, j, :], vkey="V1",
                               mask=(maskT[:, j, :], "maskT")) for j in range(nkb)]
                attn_head(qT[base:base + 64, c, :], "qT", blocks, plain_dst(c, base))
            mem_attn()
            wo_ffn(s, li, g, xt, xkey, last)
            final_store(s, g, xt, xkey, last)

    def nsa_layer(s, li, src_d, last):
        barrier()
        mem_kv(s, li)
        xc = XChain(src_d, s)
        for kv, nm in enumerate(["cmp_k_w1", "cmp_v_w1"]):
            i = rot(wbuf, wbuf_i)
            w1c = wbuf[i][:].rearrange("p a b -> p (a b)")[:, 0:2048].rearrange("p (c m) -> p c m", m=128)
            src2 = wb_d[nm].rearrange("(c p) m -> p c m", p=128)
            P.dma(w1c, src2, R=[("wb", nm)], W=[("wbuf", i)])
            b = bank()
            for c in range(16):
                P.op(pe, lambda c=c, kv=kv, w1c=w1c, b=b: nc.tensor.matmul(psf[:, b, 0:1], lhsT=w1c[:, c, :], rhs=posb[:, kv, c:c + 1],
                                                                          start=(c == 0), stop=(c == 15)),
                     R=[("wbuf", i), "posb"], W=[("ps", b)])
            P.op(act, lambda kv=kv, b=b: nc.scalar.copy(out=cb_b[:, kv:kv + 1], in_=psf[:, b, 0:1]), R=[("ps", b)], W=["cb_b"])
        if STOP[0] == 1:
            raise StopBuild()
        P.op(pool, lambda: nc.gpsimd.memset(V1s[:, :, :, 0:64], 1.0), W=["V1s"])
        P.op(pool, lambda: nc.gpsimd.memset(V1w[:, :, :, 0:64], 1.0), W=["V1w"])
        for g in range(NG):
            xt, xkey = xc.get()
            load_rope(g)
            norm_group(xt, xkey, 0 + li)
            wk_, wkk = load_w("nsa_w_in", 768, 512)
            wk2_, wk2k = load_w("nsa_w_in", 1280, 256)
            b = proj(lambda kc: wk_[:, kc, 0:128], [wkk], hT, "hT", GT)
            evac(b, kcT[:, g * GT:(g + 1) * GT], "kcT")
            b = proj(lambda kc: wk_[:, kc, 128:256], [wkk], hT, "hT", GT)
            evac(b, vcT[:, g * GT:(g + 1) * GT], "vcT")
            for src_t, src_k, off, dstT, dkey in ((wk_, wkk, 256, ksT2, "ksT2"), (wk2_, wk2k, 0, kwT2, "kwT2")):
                for grp in range(2):
                    bb = bank()
                    for half in range(2):
                        for kc in range(8):
                            P.op(pe, lambda kc=kc, half=half, grp=grp, src_t=src_t, off=off, bb=bb: nc.tensor.matmul(
                                psf[half * 64:half * 64 + 64, bb, :], lhsT=src_t[:, kc, off + grp * 64:off + grp * 64 + 64], rhs=hT[:, kc, :],
                                start=(kc == 0), stop=(kc == 7)),
                                 R=[src_k, "hT"], W=[("ps", bb)])
                    rope_from_bank(bb, dstT[:, grp, g * GT:(g + 1) * GT], dkey)
            for src_t, src_k, off, dstV, dkey in ((wk_, wkk, 384, V1s, "V1s"), (wk2_, wk2k, 128, V1w, "V1w")):
                for tt in range(4):
                    bv = bank()
                    for kc in range(8):
                        P.op(pe, lambda kc=kc, tt=tt, src_t=src_t, off=off, bv=bv: nc.tensor.matmul(
                            psf[:, bv, 0:128], lhsT=hT[:, kc, tt * 128:(tt + 1) * 128], rhs=src_t[:, kc, off:off + 128],
                            start=(kc == 0), stop=(kc == 7)),
                             R=["hT", src_k], W=[("ps", bv)])
                    P.op(act, lambda tt=tt, bv=bv, dstV=dstV: nc.scalar.copy(out=dstV[:, g * 4 + tt, :, 64:128],
                                                                               in_=psf[:, bv, 0:128].rearrange("p (g d) -> p g d", g=2)),
                         R=[("ps", bv)], W=[dkey])
        if STOP[0] == 2:
            raise StopBuild()
        P.op(pool, lambda: nc.gpsimd.memset(V1c[:, :, 0:64], 1.0), W=["V1c"])
        for kv, (srcT, skey) in enumerate(((kcT, "kcT"), (vcT, "vcT"))):
            nm = ["cmp_k_w1", "cmp_v_w1"][kv]
            wi_ = rot(wbuf, wbuf_i)
            w1dup = wbuf[wi_][:].rearrange("p a b -> p (a b)").rearrange("p (l m) -> p l m", m=128)
            srcw = wb_d[nm].rearrange("(l d) m -> d l m", d=64)
            P.dma(w1dup[0:64, :, :], srcw, R=[("wb", nm)], W=[("wbuf", wi_)])
            P.dma(w1dup[64:128, :, :], srcw, R=[("wb", nm)], W=[("wbuf", wi_)])
            for grp in range(2):
                base = grp * 64
                b = bank()
                for l in range(32):
                    P.op(pe, lambda l=l, kv=kv, base=base, srcT=srcT, b=b, w1dup=w1dup: nc.tensor.matmul(
                        psf[:, b, 0:127], lhsT=w1dup[base:base + 64, l, :], rhs=srcT[base:base + 64, l:l + 16 * 126 + 1:16],
                        start=(l == 0), stop=(l == 31)),
                         R=[("wbuf", wi_), skey], W=[("ps", b)])
                P.op(act, lambda kv=kv, b=b: nc.scalar.activation(out=gel[0][:, 0:127], in_=psf[:, b, 0:127], func=AF.Identity,
                                                                  bias=cb_b[:, kv:kv + 1], scale=1.0),
                     R=[("ps", b), "cb_b"], W=["gel0"])
                P.op(act, lambda: nc.scalar.activation(out=gel[1][:, 0:127], in_=gel[0][:, 0:127], func=AF.Square), R=["gel0"], W=["gel1"])
                P.op(dve, lambda: nc.vector.tensor_scalar(out=gel[1][:, 0:127], in0=gel[1][:, 0:127], scalar1=0.044715, scalar2=1.0,
                                                          op0=ALU.mult, op1=ALU.add), R=["gel1"], W=["gel1"])
                P.op(dve, lambda: nc.vector.tensor_tensor(out=gel[2][:, 0:127], in0=gel[1][:, 0:127], in1=gel[0][:, 0:127], op=ALU.mult),
                     R=["gel1", "gel0"], W=["gel2"])
                P.op(act, lambda: nc.scalar.activation(out=gel[3][:, 0:127], in_=gel[2][:, 0:127], func=AF.Sigmoid, scale=1.5957691216),
                     R=["gel2"], W=["gel3"])
                P.op(pool, lambda: nc.gpsimd.memset(gT[:], 0.0), W=["gT"])
                P.op(dve, lambda: nc.vector.tensor_tensor(out=gT[:, 0:127], in0=gel[3][:, 0:127], in1=gel[0][:, 0:127], op=ALU.mult),
                     R=["gel3", "gel0"], W=["gT"])
                b2 = bank()
                if kv == 0:
                    P.op(pe, lambda: nc.tensor.matmul(psf[:, b2, 0:128], lhsT=w2k2[:], rhs=gT[:], start=True, stop=True),
                         R=["w2k2", "gT"], W=[("ps", b2)])
                    evac(b2, kcmpT2[:, grp, :], "kcmpT2", ncols=128)
                else:
                    P.op(pe, lambda: nc.tensor.matmul(psf[:, b2, 0:64], lhsT=gT[:], rhs=w2v[:], start=True, stop=True),
                         R=["w2v", "gT"], W=[("ps", b2)])
                    evac(b2, V1c[:, grp, 64:128], "V1c", ncols=64)
        if STOP[0] == 3:
            raise StopBuild()
        barrier()
        if STOP[0] == 40:
            raise StopBuild()
        for g in range(NG):
            xt, xkey = xc.get()
            if STOP[0] == 401:
                raise StopBuild()
            load_rope(g)
            if STOP[0] == 402:
                raise StopBuild()
            norm_group(xt, xkey, 0 + li)
            if STOP[0] == 41:
                raise StopBuild()
            wq, wqk = load_w("nsa_w_in", 0, 512)
            wq2, wq2k = load_w("nsa_w_in", 512, 256)
            for c in range(6):
                w_, wk_ = (wq, wqk) if c < 4 else (wq2, wq2k)
                cc = c if c < 4 else c - 4
                b = proj(lambda kc, cc=cc, w_=w_: w_[:, kc, cc * 128:(cc + 1) * 128], [wk_], hT, "hT", GT)
                rope_from_bank(b, qT[:, c, :], "qT", raw=(qrawT[:, c, :], "qrawT"))
            if STOP[0] == 42:
                raise StopBuild()
            wm_, wmk = load_w("nsa_w_in", 1536, 292)
            for c in range(2):
                b = proj(lambda kc, c=c: wm_[:, kc, 36 + c * 128:36 + (c + 1) * 128], [wmk], hT, "hT", GT)
                evac(b, qmemT[:, c, :], "qmemT")
            if STOP[0] == 43:
                raise StopBuild()
            b = proj(lambda kc: wm_[:, kc, 0:64], [wmk], hT, "hT", GT, M=64)
            P.op(act, lambda b=b: nc.scalar.activation(out=sigT[:], in_=psf[0:36, b, :], func=AF.Sigmoid), R=[("ps", b)], W=["sigT"])
            P.op(dve, lambda: nc.vector.tensor_copy(out=sigH[:], in_=sigT[:]), R=["sigT"], W=["sigH"])
            P.op(dve, lambda: nc.vector.tensor_tensor(out=sigL[:], in0=sigT[:], in1=sigH[:], op=ALU.subtract), R=["sigT", "sigH"], W=["sigL"])
            if STOP[0] == 4:
                raise StopBuild()
            mcg = mc_b[:, g * GT:(g + 1) * GT]
            P.op(pool, lambda: nc.gpsimd.memset(imp[:], 0.0), W=["imp"])

            def gated_dst(h, br, add_ap, add_key, out_ap, out_key):
                def f(bo, rden, rkey):
                    bgate = bank()
                    kk = h * 3 + br
                    P.op(pe, lambda: nc.tensor.matmul(psf[0:64, bgate, :], lhsT=E_flat[0:36, kk * 64:(kk + 1) * 64], rhs=sigH[:], start=True, stop=False),
                         R=["E_b", "sigH"], W=[("ps", bgate)])
                    P.op(pe, lambda: nc.tensor.matmul(psf[0:64, bgate, :], lhsT=E_flat[0:36, kk * 64:(kk + 1) * 64], rhs=sigL[:], start=False, stop=True),
                         R=["E_b", "sigL"], W=[("ps", bgate)])
                    gi = rot(rg, rg_i)
                    P.op(dve, lambda: nc.vector.tensor_tensor(out=rg[gi][:], in0=psf[0:64, bgate, :], in1=rden, op=ALU.mult),
                         R=[("ps", bgate), rkey], W=[("rg", gi)])
                    base = (h % 2) * 64
                    if add_ap is None:
                        P.op(dve, lambda: nc.vector.tensor_tensor(out=out_ap, in0=psf[64:128, bo, :], in1=rg[gi][:], op=ALU.mult),
                             R=[("ps", bo), ("rg", gi)], W=[out_key])
                    else:
                        oi = rot(otmp, otmp_i)
                        P.op(dve, lambda: nc.vector.tensor_tensor(out=otmp[oi][base:base + 64, :], in0=psf[64:128, bo, :], in1=rg[gi][:], op=ALU.mult),
                             R=[("ps", bo), ("rg", gi)], W=[("otmp", oi)])
                        P.op(pool, lambda: nc.gpsimd.tensor_tensor(out=out_ap, in0=otmp[oi][base:base + 64, :], in1=add_ap, op=ALU.add),
                             R=[("otmp", oi), add_key], W=[out_key])
                return f

            for grp in range(2):
                heads = list(range(grp * 6, grp * 6 + 6))
                for h in heads:
                    c, base = h // 2, (h % 2) * 64
                    bs = bank()
                    P.op(pe, lambda c=c, base=base, bs=bs: nc.tensor.matmul(psf[:, bs, :], lhsT=kcmpT2[base:base + 64, grp, :],
                                                                           rhs=qrawT[base:base + 64, c, :], start=True, stop=True),
                         R=["kcmpT2", "qrawT"], W=[("ps", bs)])
                    pi = rot(PT, PT_i)
                    P.op(act, lambda bs=bs, pi=pi: nc.scalar.activation(out=PT[pi][:], in_=psf[:, bs, :], func=AF.Exp, scale=0.125),
                         R=[("ps", bs)], W=[("PT", pi)])
                    P.op(dve, lambda pi=pi: nc.vector.tensor_tensor(out=PT[pi][:], in0=PT[pi][:], in1=mcg, op=ALU.mult),
                         R=[("PT", pi), "mc_b"], W=[("PT", pi)])
                    for tt in range(4):
                        bi_ = bank()
                        P.op(pe, lambda tt=tt, pi=pi, bi_=bi_: nc.tensor.matmul(psf[:, bi_, 0:33], lhsT=PT[pi][:, tt * 128:(tt + 1) * 128],
                                                                               rhs=ov_b[:], start=True, stop=True),
                             R=[("PT", pi), "ov_b"], W=[("ps", bi_)])
                        P.op(dve, lambda bi_=bi_, tt=tt: nc.vector.tensor_scalar_max(out=rec[:, tt:tt + 1], in0=psf[:, bi_, 32:33], scalar1=1e-30),
                             R=[("ps", bi_)], W=[("rec", tt)])
                        P.op(dve, lambda tt=tt: nc.vector.reciprocal(out=rec[:, tt:tt + 1], in_=rec[:, tt:tt + 1]), R=[("rec", tt)], W=[("rec", tt)])
                        P.op(dve, lambda bi_=bi_, tt=tt: nc.vector.scalar_tensor_tensor(out=imp[:, tt, grp, :], in0=psf[:, bi_, 0:32],
                                                                                        scalar=rec[:, tt:tt + 1], in1=imp[:, tt, grp, :],
                                                                                        op0=ALU.mult, op1=ALU.add),
                             R=[("ps", bi_), ("rec", tt), "imp"], W=["imp"])
                    bo = bankL()
                    P.op(pe, lambda pi=pi, bo=bo: nc.tensor.matmul(psf[:, bo, :], lhsT=V1c[:, grp, :], rhs=PT[pi][:], start=True, stop=True),
                         R=["V1c", ("PT", pi)], W=[("ps", bo)])
                    ri = rot(rd, rd_i)
                    P.op(dve, lambda bo=bo, ri=ri: nc.vector.tensor_scalar_max(out=rd[ri][0:64, :], in0=psf[0:64, bo, :], scalar1=1e-18),
                         R=[("ps", bo)], W=[("rd", ri)])
                    P.op(act, lambda ri=ri: nc.scalar.activation(out=rd[ri][0:64, :], in_=rd[ri][0:64, :], func=AF.Ln), R=[("rd", ri)], W=[("rd", ri)])
                    P.op(act, lambda ri=ri: nc.scalar.activation(out=rd[ri][0:64, :], in_=rd[ri][0:64, :], func=AF.Exp, scale=-1.0), R=[("rd", ri)], W=[("rd", ri)])
                    hl = h - grp * 6
                    gated_dst(h, 0, None, None, cmpo[base:base + 64, hl, :], "cmpo")(bo, rd[ri][0:64, :], ("rd", ri))
                for tt in range(4):
                    qi = g * 4 + tt
                    P.op(dve, lambda tt=tt, qi=qi: nc.vector.tensor_tensor(out=imp2[:, tt, grp, :], in0=imp[:, tt, grp, :], in1=selF[:, 30 - 2 * qi:62 - 2 * qi], op=ALU.max),
                         R=["imp", "selF"], W=["imp2"])
                    P.op(dve, lambda tt=tt, qi=qi: nc.vector.tensor_tensor(out=imp2[:, tt, grp, :], in0=imp2[:, tt, grp, :], in1=selA[:, 30 - 2 * qi:62 - 2 * qi], op=ALU.add),
                         R=["imp2", "selA"], W=["imp2"])
                    P.op(dve, lambda tt=tt: nc.vector.memset(imp2[:, tt, grp, 0:1], 1.0e9), R=["imp2"], W=["imp2"])
                    P.op(dve, lambda tt=tt: nc.vector.max(out=m8[:, 0:8], in_=imp2[:, tt, grp, :]), R=["imp2"], W=["m8a"])
                    P.op(dve, lambda tt=tt: nc.vector.match_replace(out=imp3[:, tt, grp, :], in_to_replace=m8[:, 0:8], in_values=imp2[:, tt, grp, :],
                                                                    imm_value=-3.0e38),
                         R=["imp2", "m8a"], W=["imp3"])
                    P.op(dve, lambda tt=tt: nc.vector.max(out=m8[:, 8:16], in_=imp3[:, tt, grp, :]), R=["imp3"], W=["m8b"])
                    P.op(dve, lambda tt=tt: nc.vector.tensor_scalar(out=selm[:, tt, grp * 32:(grp + 1) * 32], in0=imp2[:, tt, grp, :],
                                                                    scalar1=m8[:, 15:16], scalar2=None, op0=ALU.is_ge),
                         R=["imp2", "m8b"], W=["selm"])
                if STOP[0] == 5:
                    raise StopBuild()
                if grp == 0:
                    P.op(pool, lambda: nc.gpsimd.memset(selm[:, :, 32:64], 0.0), W=["selm"])
                for tt in range(4):
                    P.op(pe, lambda tt=tt: nc.tensor.transpose(psb[0:64, tt * 128:(tt + 1) * 128], selm[:, tt, :], ident_b),
                         R=["selm", "misc_b"], W=["psb"])
                P.op(act, lambda: nc.scalar.copy(out=selT[:], in_=psb[0:64, 0:512]), R=["psb"], W=["selT"])
                nkb = 4 * g + 4
                for j in range(nkb):
                    bm = bank()
                    P.op(pe, lambda j=j, bm=bm: nc.tensor.matmul(psf[:, bm, :], lhsT=E_b[:, grp, j * 128:(j + 1) * 128], rhs=selT[:], start=True, stop=True),
                         R=["E_b", "selT"], W=[("ps", bm)])
                    if j >= 4 * g:
                        P.op(dve, lambda j=j, bm=bm: nc.vector.tensor_tensor(out=maskT[:, j, :], in0=psf[:, bm, :], in1=cm_b[:, j - 4 * g, :], op=ALU.mult),
                             R=[("ps", bm), "cm_b"], W=["maskT"])
                    else:
                        evac(bm, maskT[:, j, :], "maskT")
                if STOP[0] == 6:
                    raise StopBuild()
                for h in heads:
                    c, base = h // 2, (h % 2) * 64
                    hl = h - grp * 6
                    blocks = [dict(kT=ksT2[base:base + 64, grp, j * 128:(j + 1) * 128], kkey="ksT2", v1=V1s[:, j, grp, :], vkey="V1s",
                                   mask=(maskT[:, j, :], "maskT")) for j in range(nkb)]
                    attn_head(qT[base:base + 64, c, :], "qT", blocks, gated_dst(h, 1, cmpo[base:base + 64, hl, :], "cmpo", oacc[base:base + 64, :], "oacc"))
                    j0 = max(0, 4 * g - 4)
                    order = [4 * g] + [j for j in range(j0, nkb) if j != 4 * g]
                    blocks = [dict(kT=kwT2[base:base + 64, grp, j * 128:(j + 1) * 128], kkey="kwT2", v1=V1w[:, j, grp, :], vkey="V1w",
                                   mask=(wm_b[:, j - 4 * g + 4, :], "wm_b")) for j in order]
                    attn_head(qT[base:base + 64, c, :], "qT", blocks, gated_dst(h, 2, oacc[base:base + 64, :], "oacc", oT[base:base + 64, c, :], "hT"))
            mem_attn()
            wo_ffn(s, li, g, xt, xkey, last)
            final_store(s, g, xt, xkey, last)

    try:
        for s in range(n_seq):
            for li in range(n_layers):
                src_d = xT_d if li == 0 else xs_d
                last = (li == n_layers - 1)
                if li % 2 == 0:
                    dsa_layer(s, li, src_d, last)
                else:
                    nsa_layer(s, li, src_d, last)
    except StopBuild:
        pass
    barrier()
    P.drain()
    return nc, P


def host_inputs(inputs, core, n_seq=SEQ_PER_CORE):
    x = inputs["x"][core * n_seq:(core + 1) * n_seq]
    mem = inputs["mem"][core * n_seq:(core + 1) * n_seq]
    m = {}
    m["xT"] = np.ascontiguousarray(np.transpose(x, (0, 2, 1)))
    m["memT"] = np.ascontiguousarray(np.transpose(mem, (0, 2, 1)))
    g = np.concatenate([inputs["attn_norm"], inputs["mem_norm"], inputs["ffn_norm"], inputs["final_norm"][None]], axis=0)
    m["gam"] = np.ascontiguousarray(g.reshape(7, 8, 128).transpose(2, 0, 1))
    m["ckvg"] = np.ascontiguousarray(inputs["dsa_ckv_norm"][0].reshape(128, 1))
    m["posk"] = np.ascontiguousarray(inputs["nsa_cmp_pos_k"][0].reshape(16, 128).T)
    m["posv"] = np.ascontiguousarray(inputs["nsa_cmp_pos_v"][0].reshape(16, 128).T)
    m["dsa_w_in"] = inputs["dsa_w_in"][0]
    m["nsa_w_in"] = inputs["nsa_w_in"][0]
    for i in range(2):
        m[f"mem_w_kv{i}"] = inputs["mem_w_kv"][i]
        m[f"w_o{i}"] = inputs["w_o"][i]
        m[f"ffn_w_in{i}"] = inputs["ffn_w_in"][i]
        m[f"ffn_w_down{i}"] = inputs["ffn_w_down"][i]
    m["cmp_k_w1"] = inputs["nsa_cmp_k_w1"][0]
    m["cmp_v_w1"] = inputs["nsa_cmp_v_w1"][0]
    m["dsa_w_uk"] = inputs["dsa_w_uk"][0]
    m["dsa_w_uv"] = inputs["dsa_w_uv"][0]
    m["cmp_k_w2"] = inputs["nsa_cmp_k_w2"][0]
    m["cmp_v_w2"] = inputs["nsa_cmp_v_w2"][0]
    for k, v in make_consts().items():
        m["c_" + k] = v
    return {k: np.ascontiguousarray(v, dtype=np.float32) for k, v in m.items()}


def build2(n_seq, n_layers=2):
    _, P1 = build(n_seq, n_layers)
    return build(n_seq, n_layers, needed=P1.waited)


def kernel(**inputs):
    inputs = {k: np.asarray(v) for k, v in inputs.items()}
    nc, _ = build2(SEQ_PER_CORE)
    in_maps = [host_inputs(inputs, c) for c in range(NCORES)]
    res = run_bass_kernel_spmd(nc, in_maps, core_ids=list(range(NCORES)))
    outs = [np.transpose(r["outT"], (0, 2, 1)) for r in res.results]
    return np.ascontiguousarray(np.concatenate(outs, axis=0)).astype(np.float32)
```

```python
import numpy as np
import concourse.bass as bass
import concourse.mybir as mybir
from concourse.bass_utils import run_bass_kernel_spmd

F32 = mybir.dt.float32
BF16 = mybir.dt.bfloat16
AF = mybir.ActivationFunctionType
ALU = mybir.AluOpType
AX = mybir.AxisListType

T = 2048
NG = 4
GT = 512
D = 1024
DFF = 2816
NCORES = 8
SEQ_PER_CORE = 4
EPS = 1e-6
SEM_L = 8000
N_DSEM = 24
NEG = -1.0e30

DSA_W = 1752
NSA_W = 1828


class Eng:
    def __init__(self, P, name, b, pe=False):
        self.P, self.name, self.b, self.pe = P, name, b, pe
        self.cnt = 0
        self.rank = 0
        self.rankmap = {}
        self.sems = []
        self.seen = {}

    def _sem(self, r):
        e = (r - 1) // SEM_L
        while len(self.sems) <= e:
            self.sems.append(self.P.new_sem(f"{self.name}{len(self.sems)}"))
        return self.sems[e], (r - 1) % SEM_L + 1

    def sem_for(self, c):
        return self._sem(self.rankmap[c])


class DSem:
    def __init__(self, P, i):
        self.sem = P.new_sem(f"dma{i}")
        self.cnt = 0
        self.pe = False
        self.name = f"dma{i}"

    def sem_for(self, c):
        return self.sem, 16 * c


class Prog:
    def __init__(self, nc, needed=None):
        self.nc = nc
        self.nsem = 0
        self.needed = needed
        self.waited = set()
        self.pe = Eng(self, "pe", nc.tensor, pe=True)
        self.act = Eng(self, "act", nc.scalar)
        self.dve = Eng(self, "dve", nc.vector)
        self.pool = Eng(self, "pool", nc.gpsimd)
        self.sp = Eng(self, "sp", nc.sync)
        self.dsems = [DSem(self, i) for i in range(N_DSEM)]
        self.di = 0
        self.R = {}
        self.nins = 0
        self.nwait = 0
        self.ninc = 0

    def new_sem(self, name):
        self.nsem += 1
        return self.nc.semaphore(name).__enter__()

    def _wait(self, eng, obj, c):
        if eng.seen.get(obj, 0) >= c:
            return
        if isinstance(obj, Eng):
            self.waited.add((obj.name, c))
        sem, v = obj.sem_for(c)
        eng.b.wait_ge(sem, v)
        eng.seen[obj] = c
        self.nwait += 1

    def _deps(self, eng, reads, writes, nowaw=False):
        for k in reads:
            r = self.R.get(k)
            if r is None:
                continue
            for obj, c in r[0].items():
                if obj is eng and eng.pe:
                    continue
                self._wait(eng, obj, c)
        for k in writes:
            r = self.R.get(k)
            if r is None:
                continue
            if not nowaw:
                for obj, c in r[0].items():
                    if obj is eng and eng.pe:
                        continue
                    self._wait(eng, obj, c)
            for obj, c in r[1].items():
                if obj is eng and eng.pe:
                    continue
                self._wait(eng, obj, c)

    def _mark(self, obj, c, reads, writes):
        for k in reads:
            r = self.R.get(k)
            if r is None:
                r = self.R[k] = [{}, {}]
            r[1][obj] = c
        for k in writes:
            r = self.R.get(k)
            if r is None or r[1]:
                self.R[k] = [{obj: c}, {}]
            else:
                r[0][obj] = c

    def op(self, eng, fn, R=(), W=()):
        self._deps(eng, R, W)
        ins = fn()
        eng.cnt += 1
        if self.needed is None or (eng.name, eng.cnt) in self.needed:
            eng.rank += 1
            sem, _ = eng._sem(eng.rank)
            ins.then_inc(sem, 1)
            self.ninc += 1
        eng.rankmap[eng.cnt] = eng.rank
        self._mark(eng, eng.cnt, R, W)
        self.nins += 1
        return ins

    def dma(self, out, in_, R=(), W=(), q=None, nowaw=True):
        eng = q or self.sp
        d = self.dsems[self.di % N_DSEM]
        self.di += 1
        if d.cnt > 0:
            self._wait(eng, d, d.cnt)
        self._deps(eng, R, W, nowaw=nowaw)
        ins = eng.b.dma_start(out=out, in_=in_)
        d.cnt += 1
        ins.then_inc(d.sem, 16)
        self._mark(d, d.cnt, R, W)
        self.nins += 1
        return ins

    def drain(self):
        for d in self.dsems:
            if d.cnt > 0:
                self._wait(self.sp, d, d.cnt)


class Ring:
    def __init__(self, items):
        self.items = items
        self.i = 0

    def next(self):
        it = self.items[self.i % len(self.items)]
        self.i += 1
        return it


def _rope_tables():
    inv = 500000.0 ** (-np.arange(0, 16, 2, dtype=np.float64) / 16)
    ang = np.arange(T, dtype=np.float64)[:, None] * inv[None, :]
    cos, sin = np.cos(ang).astype(np.float32), np.sin(ang).astype(np.float32)
    C = np.ones((128, T), np.float32)
    S = np.zeros((128, T), np.float32)
    for r in range(128):
        d = r % 64
        if d < 8:
            C[r] = cos[:, d]
            S[r] = -sin[:, d]
        elif d < 16:
            C[r] = cos[:, d - 8]
            S[r] = sin[:, d - 8]
    return C, S


def make_consts():
    c = {}
    C, S = _rope_tables()
    c["ropeC"], c["ropeS"] = C, S
    perm = np.zeros((128, 128), np.float32)
    for m in range(128):
        d = m % 64
        base = m - d
        if d < 8:
            perm[base + d + 8, m] = 1.0
        elif d < 16:
            perm[base + d - 8, m] = 1.0
    ident = np.eye(128, dtype=np.float32)
    sl = np.arange(128)
    cb = np.where(sl[None, :] > sl[:, None], NEG, 0.0).astype(np.float32)
    c["misc128"] = np.stack([perm, ident, np.ones((128, 128), np.float32), cb], axis=1)
    tl = np.arange(512)
    cm = np.zeros((128, 4, 512), np.float32)
    for r in range(4):
        cm[:, r, :] = ((128 * r + sl)[:, None] <= tl[None, :]).astype(np.float32)
    c["cm"] = cm
    wm = np.zeros((128, 8, 512), np.float32)
    for j in range(8):
        s = 128 * (j - 4) + sl
        wm[:, j, :] = ((s[:, None] <= tl[None, :]) & (s[:, None] > tl[None, :] - 512)).astype(np.float32)
    c["wm"] = wm
    cc = np.arange(128)
    mc = ((16 * cc + 31)[:, None] <= np.arange(T)[None, :]).astype(np.float32)
    mc[127] = 0.0
    c["mcT"] = mc
    c0 = cc * 16
    s0 = np.arange(32) * 64
    ov = np.minimum(c0[:, None] + 32, s0[None, :] + 64) - np.maximum(c0[:, None], s0[None, :])
    ov = (np.clip(ov, 0, None) / 32.0).astype(np.float32)
    ov33 = np.concatenate([ov, np.ones((128, 1), np.float32)], axis=1)
    ov33[127] = 0.0
    c["ov33"] = ov33
    tt = np.arange(T)
    cur = tt // 64
    n = np.arange(32)
    forced = (n[None] == 0) | (n[None] == cur[:, None]) | (n[None] == cur[:, None] - 1)
    adm = n[None] <= cur[:, None]
    Ff = np.where(forced, 1.0e9, 0.0).astype(np.float32)
    Ab = np.where(adm, 0.0, NEG).astype(np.float32)
    pl = (np.arange(128) >= 64).astype(np.int64)
    y = np.arange(62) - 30
    c["selF"] = np.where((y[None] == pl[:, None]) | (y[None] == pl[:, None] - 1), 1.0e9, 0.0).astype(np.float32)
    c["selA"] = np.where(y[None] <= pl[:, None], 0.0, NEG).astype(np.float32)
    E = np.zeros((64, 2, T), np.float32)
    for g in range(2):
        for nn in range(32):
            E[g * 32 + nn, g, nn * 64:(nn + 1) * 64] = 1.0
    c["Eexp"] = E
    c["bis"] = np.tile((2.0 ** -(np.arange(32) + 1)).astype(np.float32)[None, :], (128, 1))
    return c


WSPEC = [
    ("dsa_w_in", 1024, DSA_W), ("nsa_w_in", 1024, NSA_W),
    ("mem_w_kv0", 1024, 512), ("mem_w_kv1", 1024, 512),
    ("w_o0", 1024, 1024), ("w_o1", 1024, 1024),
    ("ffn_w_in0", 1024, 2 * DFF), ("ffn_w_in1", 1024, 2 * DFF),
    ("ffn_w_down0", DFF, 1024), ("ffn_w_down1", DFF, 1024),
    ("cmp_k_w1", 2048, 128), ("cmp_v_w1", 2048, 128),
]
SMALLW = [("dsa_w_uk", 128, 48), ("dsa_w_uv", 128, 64), ("cmp_k_w2", 128, 64), ("cmp_v_w2", 128, 64)]
CONST_SHAPES = None


class StopBuild(Exception):
    pass


STOP = [0]


def build(n_seq, n_layers=2, needed=None):
    nc = bass.Bass("TRN2", target_bir_lowering=False)
    P = Prog(nc, needed)
    pe, act, dve, pool = P.pe, P.act, P.dve, P.pool
    consts = make_consts()

    def din(name, shape, dt=F32):
        return nc.dram_tensor(name, list(shape), dt, kind="ExternalInput").ap()

    xT_d = din("xT", [n_seq, D, T])
    memT_d = din("memT", [n_seq, D, 256])
    gam_d = din("gam", [128, 7, 8])
    ckvg_d = din("ckvg", [128, 1])
    posk_d = din("posk", [128, 16])
    posv_d = din("posv", [128, 16])
    w_d = {n: din(n, [K, N]) for n, K, N in WSPEC}
    sw_d = {n: din(n, [K, N]) for n, K, N in SMALLW}
    c_d = {k: din("c_" + k, v.shape) for k, v in consts.items()}
    outT_d = nc.dram_tensor("outT", [n_seq, D, T], F32, kind="ExternalOutput").ap()
    xs_d = nc.dram_tensor("xs", [n_seq, D, T], F32).ap()
    wb_d = {n: nc.dram_tensor("b_" + n, [K, N], BF16).ap() for n, K, N in WSPEC}

    ARENA_BYTES = 212000
    arena = nc.alloc_sbuf_tensor("arena", [128, ARENA_BYTES // 4], F32)
    a_base = nc.lookup_mloc(arena).addr
    a_ptr = [0]
    a_max = [0]
    DT_SZ = {F32: 4, BF16: 2}

    def sb(name, shape, dt=F32):
        nbytes = int(np.prod(shape[1:])) * DT_SZ[dt]
        off = (a_ptr[0] + 31) // 32 * 32
        a_ptr[0] = off + nbytes
        a_max[0] = max(a_max[0], a_ptr[0])
        assert a_ptr[0] <= ARENA_BYTES, (name, a_ptr[0])
        return nc.alloc_sbuf_tensor_at("s_" + name, list(shape), dt, offset=a_base + off)

    def barrier():
        engs = [P.pe, P.act, P.dve, P.pool, P.sp]
        for e in engs:
            for o in engs:
                if o is not e and o.cnt > 0:
                    P._wait(e, o, o.cnt)
            for d_ in P.dsems:
                if d_.cnt > 0:
                    P._wait(e, d_, d_.cnt)

    xg = [sb("xg0", [128, 8, GT])]
    wbuf = [sb(f"wbuf{i}", [128, 8, 512], BF16) for i in range(2)]
    xg_flat = xg[0][:].rearrange("p a b -> p (a b)")
    stage = [xg_flat[:, i * 2048:(i + 1) * 2048] for i in range(2)]
    stage_b = [wbuf[i][:].rearrange("p a b -> p (a b)")[:, 0:2048] for i in range(2)]
    SCH = 2048
    misc_f = sb("misc_f", [128, 4, 128])
    misc_b = sb("misc_b", [128, 3, 128], BF16)
    cm_b = sb("cm_b", [128, 4, 512], BF16)
    wm_b = sb("wm_b", [128, 8, 512], BF16)
    mc_b = sb("mc_b", [128, T], BF16)
    ov_b = sb("ov_b", [128, 33], BF16)
    selF = sb("selF", [128, 62])
    selA = sb("selA", [128, 62])
    E_b = sb("E_b", [64, 2, T], BF16)
    bis = sb("bis", [128, 32])
    gam = sb("gam", [128, 7, 8])
    ckvg = sb("ckvg", [128, 1])
    cvt_i = [0]
    epsT = sb("epsT", [128, 1])
    P.op(pool, lambda: nc.gpsimd.memset(epsT[:], EPS), W=["epsT"])

    def cvt(dst_ap, src_ap, R, W):
        engs = [dve, pool, act]
        e = engs[cvt_i[0] % 3]
        cvt_i[0] += 1
        if e is act:
            P.op(e, lambda: nc.scalar.copy(out=dst_ap, in_=src_ap), R=R, W=W)
        else:
            P.op(e, lambda: e.b.tensor_copy(out=dst_ap, in_=src_ap), R=R, W=W)

    def load_const(dst, src_d, key, bf=False, npart=128, cols=None):
        if not bf:
            P.dma(dst[:], src_d, W=[key])
            return
        shp = list(src_d.shape)
        flat = int(np.prod(shp[1:]))
        srcf = src_d if len(shp) == 2 else src_d.rearrange("p a b -> p (a b)")
        dstf = dst[:] if len(shp) == 2 else dst[:].rearrange("p a b -> p (a b)")
        for o in range(0, flat, SCH):
            n = min(SCH, flat - o)
            i = cvt_i[0] % 2
            P.dma(stage[i][0:shp[0], 0:n], srcf[:, o:o + n], W=[("stage", i)], nowaw=False)
            cvt(dstf[:, o:o + n], stage[i][0:shp[0], 0:n], R=[("stage", i)], W=[key])

    load_const(misc_f, c_d["misc128"], "misc_f")
    P.op(dve, lambda: nc.vector.tensor_copy(out=misc_b[:], in_=misc_f[:, 0:3, :]), R=["misc_f"], W=["misc_b"])
    load_const(cm_b, c_d["cm"], "cm_b", bf=True)
    load_const(wm_b, c_d["wm"], "wm_b", bf=True)
    load_const(mc_b, c_d["mcT"], "mc_b", bf=True)
    load_const(ov_b, c_d["ov33"], "ov_b", bf=True)
    load_const(selF, c_d["selF"], "selF")
    load_const(selA, c_d["selA"], "selA")
    load_const(E_b, c_d["Eexp"], "E_b", bf=True)
    load_const(bis, c_d["bis"], "bis")
    load_const(gam, gam_d, "gam")
    load_const(ckvg, ckvg_d, "ckvg")
    perm_b, ident_b, ones_b = misc_b[:, 0, :], misc_b[:, 1, :], misc_b[:, 2, :]
    E_flat = E_b[:].rearrange("p g s -> p (g s)")
    ones_f = misc_f[:, 2, :]
    cbias_f = misc_f[:, 3, :]

    for name, K, N in WSPEC:
        for kc in range(K // 128):
            for o in range(0, N, SCH):
                n = min(SCH, N - o)
                i = cvt_i[0] % 2
                P.dma(stage[i][:, 0:n], w_d[name][kc * 128:(kc + 1) * 128, o:o + n], W=[("stage", i)], nowaw=False)
                j = i
                cvt(stage_b[j][:, 0:n], stage[i][:, 0:n], R=[("stage", i)], W=[("stageb", j)])
                P.dma(wb_d[name][kc * 128:(kc + 1) * 128, o:o + n], stage_b[j][:, 0:n],
                      R=[("stageb", j)], W=[("wb", name)])
    wuk2 = sb("wuk2", [128, 128], BF16)
    wuv = sb("wuv", [128, 64], BF16)
    w2k2 = sb("w2k2", [128, 128], BF16)
    w2v = sb("w2v", [128, 64], BF16)
    smst = sb("smst", [128, 4, 64])
    P.op(pool, lambda: nc.gpsimd.memset(wuk2[:], 0.0), W=["wuk2"])
    P.dma(smst[:, 0, 0:48], sw_d["dsa_w_uk"], W=["smst"])
    P.dma(smst[:, 1, :], sw_d["dsa_w_uv"], W=["smst"])
    P.dma(smst[:, 2, :], sw_d["cmp_k_w2"], W=["smst"])
    P.dma(smst[:, 3, :], sw_d["cmp_v_w2"], W=["smst"])
    P.op(dve, lambda: nc.vector.tensor_copy(out=wuk2[:, 16:64], in_=smst[:, 0, 0:48]), R=["smst"], W=["wuk2"])
    P.op(dve, lambda: nc.vector.tensor_copy(out=wuk2[:, 80:128], in_=smst[:, 0, 0:48]), R=["smst"], W=["wuk2"])
    P.op(dve, lambda: nc.vector.tensor_copy(out=wuv[:], in_=smst[:, 1, :]), R=["smst"], W=["wuv"])
    P.op(dve, lambda: nc.vector.tensor_copy(out=w2k2[:, 0:64], in_=smst[:, 2, :]), R=["smst"], W=["w2k2"])
    P.op(dve, lambda: nc.vector.tensor_copy(out=w2k2[:, 64:128], in_=smst[:, 2, :]), R=["smst"], W=["w2k2"])
    P.op(dve, lambda: nc.vector.tensor_copy(out=w2v[:], in_=smst[:, 3, :]), R=["smst"], W=["w2v"])
    posf = sb("posf", [128, 2, 16])
    posb = sb("posb", [128, 2, 16], BF16)
    P.dma(posf[:, 0, :], posk_d, W=["posf"])
    P.dma(posf[:, 1, :], posv_d, W=["posf"])
    P.op(dve, lambda: nc.vector.tensor_copy(out=posb[:], in_=posf[:]), R=["posf"], W=["posb"])

    barrier()
    psf = nc.alloc_psum_tensor("psf", [128, 7, 512], F32)
    psb = nc.alloc_psum_tensor("psb", [128, 1024], BF16)
    bank_i = [0]

    def bank():
        b = bank_i[0] % 4
        bank_i[0] += 1
        return b

    bankL_i = [0]

    def bankL():
        b = 4 + bankL_i[0] % 3
        bankL_i[0] += 1
        return b

    def bank2():
        if not hasattr(bank2, "i"):
            bank2.i = 0
        b = (bank2.i % 2) * 2
        bank2.i += 1
        return b

    xg.append(sb("xg1", [128, 8, GT]))
    xg_i = [0]
    wbuf_i = [0]
    hT = sb("hT", [128, 8, GT], BF16)
    oT = hT
    sq = [sb(f"sq{i}", [128, GT]) for i in range(1)]
    sq_i = [0]
    sqh = [sb(f"sqh{i}", [128, 2, GT], BF16) for i in range(1)]
    sqh_i = [0]
    rstd = sb("rstd", [128, GT])
    wdn = [sb(f"wdn{i}", [128, 11, 128], BF16) for i in range(2)]
    wdn_i = [0]
    ropeCs = sb("ropeCs", [128, GT])
    ropeSs = sb("ropeSs", [128, GT])
    pre = [sb(f"pre{i}", [128, GT], BF16) for i in range(1)]
    pre_i = [0]
    rtmp = [sb(f"rtmp{i}", [128, GT]) for i in range(2)]
    rtmp_i = [0]
    qT = sb("qT", [128, 6, GT], BF16)
    qmemT = sb("qmemT", [128, 2, GT], BF16)
    mk_mark = a_ptr[0]
    maskT = sb("maskT", [128, 16, GT], BF16)
    mk_end = a_ptr[0]
    a_ptr[0] = mk_mark
    actT = sb("maskT", [128, 11, GT], BF16)
    a_ptr[0] = mk_mark
    memnT = sb("maskT", [128, 8, 256], BF16)
    a_ptr[0] = mk_end
    memf = xg[0][:, :, 0:256]
    sil = [sb(f"sil{i}", [128, GT], BF16) for i in range(1)]
    sil_i = [0]
    kmemT = sb("kmemT", [128, 2, 256], BF16)
    v1m = sb("v1m", [128, 2, 4, 128], BF16)
    PT = [sb(f"PT{i}", [128, GT], BF16) for i in range(4)]
    PT_i = [0]
    rd = [sb(f"rd{i}", [128, GT]) for i in range(2)]
    rd_i = [0]
    layer_mark = a_ptr[0]
    KT2 = sb("KT2", [128, T], BF16)
    kidxT2 = sb("kidxT2", [128, T], BF16)
    V1 = sb("V1", [128, 16, 128], BF16)
    ckvn = sb("ckvn", [128, GT], BF16)
    ckvf = sb("ckvf", [128, GT])
    wkr2 = sb("wkr2", [128, 8, 128], BF16)
    wki2 = sb("wki2", [128, 8, 128], BF16)
    widx = sb("widx", [128, 4, 8])
    Dh = [sb(f"Dh{i}", [128, 8, 128], BF16) for i in range(2)]
    Dh_i = [0]
    relu_t = [sb(f"relu{i}", [128, 512], BF16) for i in range(3)]
    relu_i = [0]
    sc = [sb(f"sc{i}", [128, T]) for i in range(2)]
    sc_i = [0]
    bst = sb("bst", [128, 2, 8])
    wtab = sb("wtab", [128, 2, 32])
    wtab2 = sb("wtab2", [128, 2, 32])
    mask_tm = [sb(f"masktm{i}", [128, T], BF16) for i in range(2)]
    mask_i = [0]
    qidxT = sb("qidxT", [128, 4, GT], BF16)
    dsa_end = a_ptr[0]
    a_ptr[0] = layer_mark
    ksT2 = sb("ksT2", [128, 2, T], BF16)
    kwT2 = sb("kwT2", [128, 2, T], BF16)
    V1s = sb("V1s", [128, 16, 2, 128], BF16)
    V1w = sb("V1w", [128, 16, 2, 128], BF16)
    ph_mark = a_ptr[0]
    kcT = sb("kcT", [128, T], BF16)
    vcT = sb("vcT", [128, T], BF16)
    ph_end = a_ptr[0]
    a_ptr[0] = ph_mark
    cmpo = sb("cmpo", [128, 6, GT], BF16)
    oacc = sb("oacc", [128, GT])
    a_ptr[0] = max(a_ptr[0], ph_end)
    cb_b = sb("cb_b", [128, 2])
    kcmpT2 = sb("kcmpT2", [128, 2, 128], BF16)
    V1c = sb("V1c", [128, 2, 128], BF16)
    gel = [sb(f"gel{i}", [128, 128]) for i in range(4)]
    gT = sb("gT", [128, 128], BF16)
    qrawT = sb("qrawT", [128, 6, GT], BF16)
    sigT = sb("sigT", [36, GT])
    sigH = sb("sigH", [36, GT], BF16)
    sigL = sb("sigL", [36, GT], BF16)
    imp = sb("imp", [128, 4, 2, 32])
    imp2 = sb("imp2", [128, 4, 2, 32])
    imp3 = sb("imp3", [128, 4, 2, 32])
    m8 = sb("m8", [128, 16])
    selm = sb("selm", [128, 4, 64], BF16)
    selT = sb("selT", [64, GT], BF16)
    rec = sb("rec", [128, 8])
    otmp = [sb(f"otmp{i}", [128, GT]) for i in range(1)]
    otmp_i = [0]
    rg = [sb(f"rg{i}", [64, GT]) for i in range(2)]
    rg_i = [0]
    nsa_end = a_ptr[0]
    print("SBUF plan: dsa_end", dsa_end, "nsa_end", nsa_end, "limit", ARENA_BYTES)
    a_ptr[0] = max(dsa_end, nsa_end)

    def rot(lst, ctr):
        i = ctr[0] % len(lst)
        ctr[0] += 1
        return i

    def load_w(name, col0, ncols, nk=8):
        i = rot(wbuf, wbuf_i)
        src = wb_d[name].rearrange("(c p) n -> p c n", p=128)[:, 0:nk, col0:col0 + ncols]
        P.dma(wbuf[i][:, 0:nk, 0:ncols], src, R=[("wb", name)], W=[("wbuf", i)])
        return wbuf[i], ("wbuf", i)

    def sumsq_mm(b, src_ap, src_key, ncols, first, last):
        i = rot(sq, sq_i)
        P.op(act, lambda: nc.scalar.activation(out=sq[i][:, 0:ncols], in_=src_ap, func=AF.Square), R=[src_key], W=[("sq", i)])
        j = rot(sqh, sqh_i)
        P.op(dve, lambda: nc.vector.tensor_copy(out=sqh[j][:, 0, 0:ncols], in_=sq[i][:, 0:ncols]), R=[("sq", i)], W=[("sqh", j)])
        P.op(pool, lambda: nc.gpsimd.tensor_tensor(out=sqh[j][:, 1, 0:ncols], in0=sq[i][:, 0:ncols], in1=sqh[j][:, 0, 0:ncols], op=ALU.subtract),
             R=[("sq", i), ("sqh", j)], W=[("sqh", j)])
        P.op(pe, lambda: nc.tensor.matmul(psf[:, b, 0:ncols], lhsT=ones_b, rhs=sqh[j][:, 0, 0:ncols], start=first, stop=False),
             R=[("sqh", j), "misc_b"], W=[("ps", b)])
        P.op(pe, lambda: nc.tensor.matmul(psf[:, b, 0:ncols], lhsT=ones_b, rhs=sqh[j][:, 1, 0:ncols], start=False, stop=last),
             R=[("sqh", j), "misc_b"], W=[("ps", b)])

    def norm_group(xt, xkey, gidx, ncols=GT, dst=None, dkey="hT"):
        dst = hT if dst is None else dst
        b = bank()
        for c in range(8):
            sumsq_mm(b, xt[:, c, 0:ncols], xkey, ncols, c == 0, c == 7)
        P.op(act, lambda: nc.scalar.activation(out=rstd[:, 0:ncols], in_=psf[:, b, 0:ncols], func=AF.Sqrt,
                                               scale=1.0 / D, bias=epsT[:, 0:1]),
             R=[("ps", b)], W=["rstd"])
        P.op(dve, lambda: nc.vector.reciprocal(out=rstd[:, 0:ncols], in_=rstd[:, 0:ncols]), R=["rstd"], W=["rstd"])
        for c in range(8):
            P.op(dve, lambda c=c: nc.vector.scalar_tensor_tensor(
                out=dst[:, c, 0:ncols], in0=xt[:, c, 0:ncols], scalar=gam[:, gidx, c:c + 1],
                in1=rstd[:, 0:ncols], op0=ALU.mult, op1=ALU.mult),
                 R=[xkey, "rstd", "gam"], W=[dkey])

    def proj(lhs_fn, lhs_keys, rhs_t, rhs_key, ncols, M=128):
        b = bank()
        for kc in range(8):
            P.op(pe, lambda kc=kc: nc.tensor.matmul(psf[0:M, b, 0:ncols], lhsT=lhs_fn(kc), rhs=rhs_t[:, kc, 0:ncols],
                                                    start=(kc == 0), stop=(kc == 7)),
                 R=list(lhs_keys) + [rhs_key], W=[("ps", b)])
        return b

    def rope_from_bank(b, dst_ap, dkey, ncols=GT, raw=None):
        i = rot(pre, pre_i)
        P.op(act, lambda: nc.scalar.copy(out=pre[i][:, 0:ncols], in_=psf[:, b, 0:ncols]), R=[("ps", b)], W=[("pre", i)])
        if raw is not None:
            P.op(pool, lambda: nc.gpsimd.tensor_copy(out=raw[0], in_=pre[i][:, 0:ncols]), R=[("pre", i)], W=[raw[1]])
        b2 = bank()
        P.op(pe, lambda: nc.tensor.matmul(psf[:, b2, 0:ncols], lhsT=perm_b, rhs=pre[i][:, 0:ncols], start=True, stop=True),
             R=[("pre", i), "misc_b"], W=[("ps", b2)])
        j = rot(rtmp, rtmp_i)
        P.op(dve, lambda: nc.vector.tensor_tensor(out=rtmp[j][:, 0:ncols], in0=psf[:, b2, 0:ncols], in1=ropeSs[:, 0:ncols], op=ALU.mult),
             R=[("ps", b2), "ropeS"], W=[("rtmp", j)])
        k = rot(rtmp, rtmp_i)
        P.op(pool, lambda: nc.gpsimd.tensor_tensor(out=rtmp[k][:, 0:ncols], in0=pre[i][:, 0:ncols], in1=ropeCs[:, 0:ncols], op=ALU.mult),
             R=[("pre", i), "ropeC"], W=[("rtmp", k)])
        P.op(pool, lambda: nc.gpsimd.tensor_tensor(out=dst_ap, in0=rtmp[j][:, 0:ncols], in1=rtmp[k][:, 0:ncols], op=ALU.add),
             R=[("rtmp", j), ("rtmp", k)], W=[dkey])

    def evac(b, dst_ap, dkey, ncols=GT, M=128, eng=None):
        e = eng or act
        if e is act:
            P.op(act, lambda: nc.scalar.copy(out=dst_ap, in_=psf[0:M, b, 0:ncols]), R=[("ps", b)], W=[dkey])
        else:
            P.op(e, lambda: e.b.tensor_copy(out=dst_ap, in_=psf[0:M, b, 0:ncols]), R=[("ps", b)], W=[dkey])

    def load_rope(g):
        P.dma(ropeCs[:], c_d["ropeC"][:, g * GT:(g + 1) * GT], W=["ropeC"])
        P.dma(ropeSs[:], c_d["ropeS"][:, g * GT:(g + 1) * GT], W=["ropeS"])

    def load_x(src_d, s, g):
        i = rot(xg, xg_i)
        src = src_d[s].rearrange("(c p) t -> p c t", p=128)
        for c in range(0, 8, 2):
            P.dma(xg[i][:, c:c + 2, :], src[:, c:c + 2, g * GT:(g + 1) * GT], W=[("xg", i)])
        return xg[i], ("xg", i)

    class XChain:
        def __init__(self, src_d, s):
            self.src_d, self.s = src_d, s
            self.order = list(range(NG)) + list(range(NG))
            self.k = 0
            self.pending = load_x(src_d, s, self.order[0])

        def get(self):
            cur = self.pending
            self.k += 1
            self.pending = load_x(self.src_d, self.s, self.order[self.k]) if self.k < len(self.order) else None
            return cur

    def attn_head(q_ap, q_key, blocks, dst_fn, scale=0.125, guard=False):
        bo = bankL()
        nb = len(blocks)
        pend = []
        for idx, blk in enumerate(blocks):
            bs = bank()
            c0, c1 = blk.get("c0", 0), blk.get("c1", GT)
            assert idx > 0 or (c0 == 0 and c1 == GT)
            P.op(pe, lambda: nc.tensor.matmul(psf[:, bs, c0:c1], lhsT=blk["kT"], rhs=q_ap[:, c0:c1], start=True, stop=True),
                 R=[blk["kkey"], q_key], W=[("ps", bs)])
            pi = rot(PT, PT_i)
            P.op(act, lambda: nc.scalar.activation(out=PT[pi][:, c0:c1], in_=psf[:, bs, c0:c1], func=AF.Exp, scale=scale),
                 R=[("ps", bs)], W=[("PT", pi)])
            if blk.get("mask") is not None:
                m_ap, m_key = blk["mask"]
                P.op(dve, lambda: nc.vector.tensor_tensor(out=PT[pi][:, c0:c1], in0=PT[pi][:, c0:c1], in1=m_ap[:, c0:c1], op=ALU.mult),
                     R=[("PT", pi), m_key], W=[("PT", pi)])
            pend.append((blk, pi, idx))
            if len(pend) > 2:
                _pv(pend.pop(0), bo, nb)
        while pend:
            _pv(pend.pop(0), bo, nb)
        ri = rot(rd, rd_i)
        if guard:
            P.op(dve, lambda: nc.vector.tensor_scalar_max(out=rd[ri][0:64, :], in0=psf[0:64, bo, :], scalar1=1e-18),
                 R=[("ps", bo)], W=[("rd", ri)])
            P.op(act, lambda: nc.scalar.activation(out=rd[ri][0:64, :], in_=rd[ri][0:64, :], func=AF.Ln), R=[("rd", ri)], W=[("rd", ri)])
        else:
            P.op(act, lambda: nc.scalar.activation(out=rd[ri][0:64, :], in_=psf[0:64, bo, :], func=AF.Ln), R=[("ps", bo)], W=[("rd", ri)])
        P.op(act, lambda: nc.scalar.activation(out=rd[ri][0:64, :], in_=rd[ri][0:64, :], func=AF.Exp, scale=-1.0), R=[("rd", ri)], W=[("rd", ri)])
        dst_fn(bo, rd[ri][0:64, :], ("rd", ri))

    def _pv(item, bo, nb):
        blk, pi, idx = item
        c0, c1 = blk.get("c0", 0), blk.get("c1", GT)
        P.op(pe, lambda: nc.tensor.matmul(psf[:, bo, c0:c1], lhsT=blk["v1"], rhs=PT[pi][:, c0:c1], start=(idx == 0), stop=(idx == nb - 1)),
             R=[blk["vkey"], ("PT", pi)], W=[("ps", bo)])

    def plain_dst(chunk, base):
        def f(bo, rden, rkey):
            P.op(dve, lambda: nc.vector.tensor_tensor(out=oT[base:base + 64, chunk, :], in0=psf[64:128, bo, :], in1=rden, op=ALU.mult),
                 R=[("ps", bo), rkey], W=["hT"])
        return f

    def mem_kv(s, li):
        for c in range(0, 8, 4):
            P.dma(memf[:, c:c + 4, :], memT_d[s].rearrange("(c p) t -> p c t", p=128)[:, c:c + 4, :], W=[("xg", 0)])
        norm_group(memf, ("xg", 0), 2 + li, ncols=256, dst=memnT, dkey="maskT")
        wname = f"mem_w_kv{li}"
        wt, wk = load_w(wname, 0, 512)
        for c in range(2):
            b = proj(lambda kc, c=c: wt[:, kc, c * 128:(c + 1) * 128], [wk], memnT, "maskT", 256)
            evac(b, kmemT[:, c, :], "kmemT", ncols=256)
        P.op(pool, lambda: nc.gpsimd.memset(v1m[:, :, :, 0:64], 1.0), W=["v1m"])
        for nb_ in range(2):
            b = bank()
            for kc in range(8):
                P.op(pe, lambda kc=kc: nc.tensor.matmul(psf[:, b, 0:256], lhsT=memnT[:, kc, nb_ * 128:(nb_ + 1) * 128],
                                                        rhs=wt[:, kc, 256:512], start=(kc == 0), stop=(kc == 7)),
                     R=["maskT", wk], W=[("ps", b)])
            P.op(act, lambda: nc.scalar.copy(out=v1m[:, nb_, :, 64:128], in_=psf[:, b, 0:256].rearrange("p (h d) -> p h d", h=4)),
                 R=[("ps", b)], W=["v1m"])

    def mem_attn():
        for h in range(4):
            c, base = h // 2, (h % 2) * 64
            blocks = [dict(kT=kmemT[base:base + 64, c, nb_ * 128:(nb_ + 1) * 128], kkey="kmemT",
                           v1=v1m[:, nb_, h, :], vkey="v1m", mask=None) for nb_ in range(2)]
            attn_head(qmemT[base:base + 64, c, :], "qmemT", blocks, plain_dst(6 + c, base))

    def wo_ffn(s, li, g, xt, xkey, last):
        for half in range(2):
            wt, wk = load_w(f"w_o{li}", half * 512, 512)
            for oc4 in range(4):
                oc = half * 4 + oc4
                b = proj(lambda kc, oc4=oc4, wt=wt: wt[:, kc, oc4 * 128:(oc4 + 1) * 128], [wk], oT, "hT", GT)
                P.op(dve, lambda oc=oc, b=b: nc.vector.tensor_tensor(out=xt[:, oc, :], in0=xt[:, oc, :], in1=psf[:, b, :], op=ALU.add),
                     R=[xkey, ("ps", b)], W=[xkey])
        norm_group(xt, xkey, 4 + li)
        wsrc = wb_d[f"ffn_w_in{li}"].rearrange("(c p) n -> p c n", p=128)
        dsrc = wb_d[f"ffn_w_down{li}"].rearrange("(c p) n -> p c n", p=128)
        for fh in range(2):
            for cl in range(11):
                ch = fh * 11 + cl
                i = rot(wbuf, wbuf_i)
                P.dma(wbuf[i][:, :, 0:128], wsrc[:, :, ch * 128:(ch + 1) * 128], R=[("wb", f"ffn_w_in{li}")], W=[("wbuf", i)])
                P.dma(wbuf[i][:, :, 128:256], wsrc[:, :, DFF + ch * 128:DFF + (ch + 1) * 128], R=[("wb", f"ffn_w_in{li}")], W=[("wbuf", i)])
                wgk = ("wbuf", i)
                bg = proj(lambda kc, i=i: wbuf[i][:, kc, 0:128], [wgk], hT, "hT", GT)
                bu = proj(lambda kc, i=i: wbuf[i][:, kc, 128:256], [wgk], hT, "hT", GT)
                si = rot(sil, sil_i)
                P.op(act, lambda bg=bg, si=si: nc.scalar.activation(out=sil[si][:], in_=psf[:, bg, :], func=AF.Silu),
                     R=[("ps", bg)], W=[("sil", si)])
                P.op(dve, lambda cl=cl, bu=bu, si=si: nc.vector.tensor_tensor(out=actT[:, cl, :], in0=psf[:, bu, :], in1=sil[si][:], op=ALU.mult),
                     R=[("ps", bu), ("sil", si)], W=["maskT"])
            for oc in range(8):
                i = rot(wdn, wdn_i)
                P.dma(wdn[i][:], dsrc[:, fh * 11:(fh + 1) * 11, oc * 128:(oc + 1) * 128], R=[("wb", f"ffn_w_down{li}")], W=[("wdn", i)])
                b = bankL()
                for kc in range(11):
                    P.op(pe, lambda kc=kc, i=i, b=b: nc.tensor.matmul(psf[:, b, :], lhsT=wdn[i][:, kc, :], rhs=actT[:, kc, :],
                                                                     start=(kc == 0), stop=(kc == 10)),
                         R=[("wdn", i), "maskT"], W=[("ps", b)])
                P.op(dve, lambda oc=oc, b=b: nc.vector.tensor_tensor(out=xt[:, oc, :], in0=xt[:, oc, :], in1=psf[:, b, :], op=ALU.add),
                     R=[xkey, ("ps", b)], W=[xkey])

    def final_store(s, g, xt, xkey, last):
        if last:
            b = bank()
            for c in range(8):
                sumsq_mm(b, xt[:, c, :], xkey, GT, c == 0, c == 7)
            P.op(act, lambda: nc.scalar.activation(out=rstd[:], in_=psf[:, b, :], func=AF.Sqrt, scale=1.0 / D, bias=epsT[:, 0:1]),
                 R=[("ps", b)], W=["rstd"])
            P.op(dve, lambda: nc.vector.reciprocal(out=rstd[:], in_=rstd[:]), R=["rstd"], W=["rstd"])
            for c in range(8):
                P.op(dve, lambda c=c: nc.vector.scalar_tensor_tensor(out=xt[:, c, :], in0=xt[:, c, :], scalar=gam[:, 6, c:c + 1],
                                                                      in1=rstd[:], op0=ALU.mult, op1=ALU.mult),
                     R=[xkey, "rstd", "gam"], W=[xkey])
            dst = outT_d[s].rearrange("(c p) t -> p c t", p=128)
            P.dma(dst[:, :, g * GT:(g + 1) * GT], xt[:], R=[xkey], W=["out"])
        else:
            dst = xs_d[s].rearrange("(c p) t -> p c t", p=128)
            P.dma(dst[:, :, g * GT:(g + 1) * GT], xt[:], R=[xkey], W=[("xs", s)])

    def dsa_layer(s, li, src_d, last):
        barrier()
        mem_kv(s, li)
        xc = XChain(src_d, s)
        wt, wk = load_w("dsa_w_in", 896, 16 + 0)
        P.op(pool, lambda: nc.gpsimd.memset(wkr2[:], 0.0), W=["wkr2"])
        P.op(pool, lambda: nc.gpsimd.tensor_copy(out=wkr2[:, :, 0:16], in_=wt[:, :, 0:16]), R=[wk], W=["wkr2"])
        P.op(pool, lambda: nc.gpsimd.tensor_copy(out=wkr2[:, :, 64:80], in_=wt[:, :, 0:16]), R=[wk], W=["wkr2"])
        wt2, wk2 = load_w("dsa_w_in", 1424, 64)
        P.op(pool, lambda: nc.gpsimd.tensor_copy(out=wki2[:, :, 0:64], in_=wt2[:, :, 0:64]), R=[wk2], W=["wki2"])
        P.op(pool, lambda: nc.gpsimd.tensor_copy(out=wki2[:, :, 64:128], in_=wt2[:, :, 0:64]), R=[wk2], W=["wki2"])
        P.op(pool, lambda: nc.gpsimd.memset(V1[:, :, 0:64], 1.0), W=["V1"])
        for g in range(NG):
            xt, xkey = xc.get()
            load_rope(g)
            norm_group(xt, xkey, 0 + li)
            wc, wck = load_w("dsa_w_in", 768, 128)
            b = proj(lambda kc: wc[:, kc, 0:128], [wck], hT, "hT", GT)
            evac(b, ckvf[:], "ckvf", eng=dve)
            b2 = bank()
            sumsq_mm(b2, ckvf[:], "ckvf", GT, True, True)
            P.op(act, lambda: nc.scalar.activation(out=rstd[:], in_=psf[:, b2, :], func=AF.Sqrt, scale=1.0 / 128, bias=epsT[:, 0:1]),
                 R=[("ps", b2)], W=["rstd"])
            P.op(dve, lambda: nc.vector.reciprocal(out=rstd[:], in_=rstd[:]), R=["rstd"], W=["rstd"])
            P.op(dve, lambda: nc.vector.scalar_tensor_tensor(out=ckvn[:], in0=ckvf[:], scalar=ckvg[:, 0:1], in1=rstd[:],
                                                             op0=ALU.mult, op1=ALU.mult),
                 R=["ckvf", "rstd", "ckvg"], W=["ckvn"])
            bk = bank()
            for kc in range(8):
                P.op(pe, lambda kc=kc: nc.tensor.matmul(psf[:, bk, :], lhsT=wkr2[:, kc, :], rhs=hT[:, kc, :], start=(kc == 0), stop=False),
                     R=["wkr2", "hT"], W=[("ps", bk)])
            P.op(pe, lambda: nc.tensor.matmul(psf[:, bk, :], lhsT=wuk2[:], rhs=ckvn[:], start=False, stop=True),
                 R=["wuk2", "ckvn"], W=[("ps", bk)])
            rope_from_bank(bk, KT2[:, g * GT:(g + 1) * GT], "KT2")
            bi = proj(lambda kc: wki2[:, kc, :], ["wki2"], hT, "hT", GT)
            rope_from_bank(bi, kidxT2[:, g * GT:(g + 1) * GT], "kidxT2")
            for tt in range(4):
                bv = bank()
                P.op(pe, lambda tt=tt: nc.tensor.matmul(psf[:, bv, 0:64], lhsT=ckvn[:, tt * 128:(tt + 1) * 128], rhs=wuv[:], start=True, stop=True),
                     R=["ckvn", "wuv"], W=[("ps", bv)])
                P.op(act, lambda tt=tt, bv=bv: nc.scalar.copy(out=V1[:, g * 4 + tt, 64:128], in_=psf[:, bv, 0:64]), R=[("ps", bv)], W=["V1"])
        for g in range(NG):
            xt, xkey = xc.get()
            load_rope(g)
            norm_group(xt, xkey, 0 + li)
            wq, wqk = load_w("dsa_w_in", 0, 512)
            for c in range(4):
                b = proj(lambda kc, c=c: wq[:, kc, c * 128:(c + 1) * 128], [wqk], hT, "hT", GT)
                rope_from_bank(b, qT[:, c, :], "qT")
            wq2, wq2k = load_w("dsa_w_in", 512, 256)
            for c in range(2):
                b = proj(lambda kc, c=c: wq2[:, kc, c * 128:(c + 1) * 128], [wq2k], hT, "hT", GT)
                rope_from_bank(b, qT[:, 4 + c, :], "qT")
            wi, wik = load_w("dsa_w_in", 912, 512)
            for c in range(4):
                b = proj(lambda kc, c=c: wi[:, kc, c * 128:(c + 1) * 128], [wik], hT, "hT", GT)
                rope_from_bank(b, qidxT[:, c, :], "qidxT")
            wm_, wmk = load_w("dsa_w_in", 1488, 264)
            for c in range(2):
                b = proj(lambda kc, c=c: wm_[:, kc, 8 + c * 128:8 + (c + 1) * 128], [wmk], hT, "hT", GT)
                evac(b, qmemT[:, c, :], "qmemT")
            for tt in range(4):
                b = bank()
                for kc in range(8):
                    P.op(pe, lambda kc=kc, tt=tt: nc.tensor.matmul(psf[:, b, 0:8], lhsT=hT[:, kc, tt * 128:(tt + 1) * 128], rhs=wm_[:, kc, 0:8],
                                                                   start=(kc == 0), stop=(kc == 7)),
                         R=["hT", wmk], W=[("ps", b)])
                P.op(act, lambda tt=tt, b=b: nc.scalar.mul(widx[:, tt, :], psf[:, b, 0:8], float(8 ** -0.5 * 64 ** -0.5)),
                     R=[("ps", b)], W=["widx"])
            P.op(pool, lambda: nc.gpsimd.memset(maskT[:], 0.0), W=["maskT"])
            def idx_phase(tt):
                qi = g * 4 + tt
                u = tt % 2
                Sc = 128 * (qi + 1)
                di = rot(Dh, Dh_i)
                for h in range(8):
                    P.op(dve, lambda h=h: nc.vector.tensor_scalar(out=Dh[di][:, h, :], in0=ident_b, scalar1=widx[:, tt, h:h + 1],
                                                                  scalar2=None, op0=ALU.mult),
                         R=["misc_b", "widx"], W=[("Dh", di)])
                si = rot(sc, sc_i)
                for k0 in range(0, Sc, 512):
                    kn = min(512, Sc - k0)
                    ba = bankL()
                    for h in range(8):
                        c, base = h // 2, (h % 2) * 64
                        bl = bank()
                        P.op(pe, lambda: nc.tensor.matmul(
                            psf[:, bl, 0:kn], lhsT=qidxT[base:base + 64, c, tt * 128:(tt + 1) * 128],
                            rhs=kidxT2[base:base + 64, k0:k0 + kn], start=True, stop=True),
                             R=["qidxT", "kidxT2"], W=[("ps", bl)])
                        ri = rot(relu_t, relu_i)
                        P.op(act, lambda: nc.scalar.activation(out=relu_t[ri][:, 0:kn], in_=psf[:, bl, 0:kn], func=AF.Relu),
                             R=[("ps", bl)], W=[("relu", ri)])
                        P.op(pe, lambda: nc.tensor.matmul(
                            psf[:, ba, 0:kn], lhsT=Dh[di][:, h, :], rhs=relu_t[ri][:, 0:kn], start=(h == 0), stop=(h == 7)),
                             R=[("Dh", di), ("relu", ri)], W=[("ps", ba)])
                    P.op(act, lambda: nc.scalar.copy(out=sc[si][:, k0:k0 + kn], in_=psf[:, ba, 0:kn]),
                         R=[("ps", ba)], W=[("sc", si)])
                scv = sc[si]
                skey = ("sc", si)
                B = lambda j: bst[:, u, j:j + 1]
                K = lambda j: ("bst", u, j)
                if qi >= 2:
                    P.op(dve, lambda: nc.vector.tensor_reduce(out=B(0), in_=scv[:, 0:Sc], axis=AX.X, op=ALU.min), R=[skey], W=[K(0)])
                    P.op(dve, lambda: nc.vector.tensor_reduce(out=B(1), in_=scv[:, 0:Sc], axis=AX.X, op=ALU.max), R=[skey], W=[K(1)])
                P.op(dve, lambda: nc.vector.tensor_tensor(out=scv[:, Sc - 128:Sc], in0=scv[:, Sc - 128:Sc], in1=cbias_f, op=ALU.add),
                     R=[skey, "misc_f", K(0), K(1)], W=[skey])
                mi = rot(mask_tm, mask_i)
                return dict(tt=tt, qi=qi, u=u, Sc=Sc, scv=scv, skey=skey, mi=mi)

            def bis_gen(st):
                u, Sc, scv, skey, mi = st["u"], st["Sc"], st["scv"], st["skey"], st["mi"]
                B = lambda j: bst[:, u, j:j + 1]
                K = lambda j: ("bst", u, j)
                wk, wk2 = ("wtab", u), ("wtab2", u)
                NIT = 16
                P.op(dve, lambda: nc.vector.tensor_tensor(out=B(2), in0=B(1), in1=B(0), op=ALU.subtract), R=[K(0), K(1)], W=[K(2)])
                P.op(dve, lambda: nc.vector.tensor_scalar(out=wtab[:, u, :], in0=bis[:], scalar1=B(2), scalar2=None, op0=ALU.mult),
                     R=["bis", K(2)], W=[wk])
                P.op(dve, lambda: nc.vector.tensor_scalar(out=wtab2[:, u, :], in0=bis[:], scalar1=B(2), scalar2=2.0, op0=ALU.mult, op1=ALU.mult),
                     R=["bis", K(2)], W=[wk2])
                P.op(dve, lambda: nc.vector.tensor_tensor(out=B(3), in0=B(0), in1=wtab[:, u, 0:1], op=ALU.add), R=[K(0), wk], W=[K(3)])
                yield
                for it in range(NIT):
                    P.op(dve, lambda: nc.vector.tensor_scalar(out=mask_tm[mi][:, 0:Sc], in0=scv[:, 0:Sc], scalar1=B(3), scalar2=0.0,
                                                              op0=ALU.is_ge, op1=ALU.add, accum_out=B(4)),
                         R=[skey, K(3)], W=[("masktm", mi), K(4)])
                    yield
                    if it < NIT - 1:
                        wn = wtab[:, u, it + 1:it + 2]
                        wn2 = wtab2[:, u, it + 1:it + 2]
                        P.op(dve, lambda: nc.vector.tensor_scalar(out=B(5), in0=B(4), scalar1=255.5, scalar2=wn2, op0=ALU.is_ge, op1=ALU.mult),
                             R=[K(4), wk2], W=[K(5)])
                        yield
                        P.op(dve, lambda: nc.vector.scalar_tensor_tensor(out=B(3), in0=B(5), scalar=wn, in1=B(3), op0=ALU.subtract, op1=ALU.add),
                             R=[K(5), K(3), wk], W=[K(3)])
                        yield
                    else:
                        wl = wtab[:, u, it:it + 1]
                        P.op(dve, lambda: nc.vector.tensor_scalar(out=B(5), in0=B(4), scalar1=255.5, scalar2=1.0, op0=ALU.is_ge, op1=ALU.subtract),
                             R=[K(4)], W=[K(5)])
                        yield
                        P.op(dve, lambda: nc.vector.scalar_tensor_tensor(out=B(6), in0=B(5), scalar=wl, in1=B(3), op0=ALU.mult, op1=ALU.add),
                             R=[K(5), K(3), wk], W=[K(6)])
                        yield

            def fin_phase(st):
                tt, qi, u, Sc, scv, skey, mi = st["tt"], st["qi"], st["u"], st["Sc"], st["scv"], st["skey"], st["mi"]
                if qi >= 2:
                    P.op(dve, lambda: nc.vector.tensor_scalar(out=mask_tm[mi][:, 0:Sc], in0=scv[:, 0:Sc], scalar1=bst[:, u, 6:7], scalar2=None, op0=ALU.is_ge),
                         R=[skey, ("bst", u, 6)], W=[("masktm", mi)])
                else:
                    P.op(dve, lambda: nc.vector.tensor_scalar(out=mask_tm[mi][:, 0:Sc], in0=scv[:, 0:Sc], scalar1=-1.0e29, scalar2=None, op0=ALU.is_ge),
                         R=[skey], W=[("masktm", mi)])
                nblk = qi + 1
                for j0 in range(0, nblk, 8):
                    jn = min(8, nblk - j0)
                    for j in range(j0, j0 + jn):
                        P.op(pe, lambda: nc.tensor.transpose(psb[:, (j - j0) * 128:(j - j0 + 1) * 128],
                                                             mask_tm[mi][:, j * 128:(j + 1) * 128], ident_b),
                             R=[("masktm", mi), "misc_b"], W=["psb"])
                    P.op(act, lambda: nc.scalar.copy(out=maskT[:, j0:j0 + jn, tt * 128:(tt + 1) * 128],
                                                     in_=psb[:, 0:jn * 128].rearrange("p (j t) -> p j t", j=jn)),
                         R=["psb"], W=["maskT"])

            for t0 in (0, 2):
                sts = [idx_phase(t0), idx_phase(t0 + 1)]
                gens = [bis_gen(st) for st in sts if st["qi"] >= 2]
                while gens:
                    for gg in list(gens):
                        try:
                            next(gg)
                        except StopIteration:
                            gens.remove(gg)
                for st in sts:
                    fin_phase(st)
            nkb = 4 * g + 4
            for h in range(12):
                c, base = h // 2, (h % 2) * 64
                blocks = [dict(kT=KT2[base:base + 64, j * 128:(j + 1) * 128], kkey="KT2", v1=V1[:, j, :], vkey="V1",
                               mask=(maskT[:, j, :], "maskT"), c0=128 * max(0, j - 4 * g)) for j in range(nkb)]
                attn_head(qT[base:base + 64, c, :], "qT", blocks, plain_dst(c, base))
            mem_attn()
            wo_ffn(s, li, g, xt, xkey, last)
            final_store(s, g, xt, xkey, last)

    def nsa_layer(s, li, src_d, last):
        barrier()
        mem_kv(s, li)
        xc = XChain(src_d, s)
        for kv, nm in enumerate(["cmp_k_w1", "cmp_v_w1"]):
            i = rot(wbuf, wbuf_i)
            w1c = wbuf[i][:].rearrange("p a b -> p (a b)")[:, 0:2048].rearrange("p (c m) -> p c m", m=128)
            src2 = wb_d[nm].rearrange("(c p) m -> p c m", p=128)
            P.dma(w1c, src2, R=[("wb", nm)], W=[("wbuf", i)])
            b = bank()
            for c in range(16):
                P.op(pe, lambda c=c, kv=kv, w1c=w1c, b=b: nc.tensor.matmul(psf[:, b, 0:1], lhsT=w1c[:, c, :], rhs=posb[:, kv, c:c + 1],
                                                                          start=(c == 0), stop=(c == 15)),
                     R=[("wbuf", i), "posb"], W=[("ps", b)])
            P.op(act, lambda kv=kv, b=b: nc.scalar.copy(out=cb_b[:, kv:kv + 1], in_=psf[:, b, 0:1]), R=[("ps", b)], W=["cb_b"])
        if STOP[0] == 1:
            raise StopBuild()
        P.op(pool, lambda: nc.gpsimd.memset(V1s[:, :, :, 0:64], 1.0), W=["V1s"])
        P.op(pool, lambda: nc.gpsimd.memset(V1w[:, :, :, 0:64], 1.0), W=["V1w"])
        for g in range(NG):
            xt, xkey = xc.get()
            load_rope(g)
            norm_group(xt, xkey, 0 + li)
            wk_, wkk = load_w("nsa_w_in", 768, 512)
            wk2_, wk2k = load_w("nsa_w_in", 1280, 256)
            b = proj(lambda kc: wk_[:, kc, 0:128], [wkk], hT, "hT", GT)
            evac(b, kcT[:, g * GT:(g + 1) * GT], "kcT")
            b = proj(lambda kc: wk_[:, kc, 128:256], [wkk], hT, "hT", GT)
            evac(b, vcT[:, g * GT:(g + 1) * GT], "vcT")
            for src_t, src_k, off, dstT, dkey in ((wk_, wkk, 256, ksT2, "ksT2"), (wk2_, wk2k, 0, kwT2, "kwT2")):
                for grp in range(2):
                    bb = bank()
                    for half in range(2):
                        for kc in range(8):
                            P.op(pe, lambda kc=kc, half=half, grp=grp, src_t=src_t, off=off, bb=bb: nc.tensor.matmul(
                                psf[half * 64:half * 64 + 64, bb, :], lhsT=src_t[:, kc, off + grp * 64:off + grp * 64 + 64], rhs=hT[:, kc, :],
                                start=(kc == 0), stop=(kc == 7)),
                                 R=[src_k, "hT"], W=[("ps", bb)])
                    rope_from_bank(bb, dstT[:, grp, g * GT:(g + 1) * GT], dkey)
            for src_t, src_k, off, dstV, dkey in ((wk_, wkk, 384, V1s, "V1s"), (wk2_, wk2k, 128, V1w, "V1w")):
                for tt in range(4):
                    bv = bank()
                    for kc in range(8):
                        P.op(pe, lambda kc=kc, tt=tt, src_t=src_t, off=off, bv=bv: nc.tensor.matmul(
                            psf[:, bv, 0:128], lhsT=hT[:, kc, tt * 128:(tt + 1) * 128], rhs=src_t[:, kc, off:off + 128],
                            start=(kc == 0), stop=(kc == 7)),
                             R=["hT", src_k], W=[("ps", bv)])
                    P.op(act, lambda tt=tt, bv=bv, dstV=dstV: nc.scalar.copy(out=dstV[:, g * 4 + tt, :, 64:128],
                                                                               in_=psf[:, bv, 0:128].rearrange("p (g d) -> p g d", g=2)),
                         R=[("ps", bv)], W=[dkey])
        if STOP[0] == 2:
            raise StopBuild()
        P.op(pool, lambda: nc.gpsimd.memset(V1c[:, :, 0:64], 1.0), W=["V1c"])
        for kv, (srcT, skey) in enumerate(((kcT, "kcT"), (vcT, "vcT"))):
            nm = ["cmp_k_w1", "cmp_v_w1"][kv]
            wi_ = rot(wbuf, wbuf_i)
            w1dup = wbuf[wi_][:].rearrange("p a b -> p (a b)").rearrange("p (l m) -> p l m", m=128)
            srcw = wb_d[nm].rearrange("(l d) m -> d l m", d=64)
            P.dma(w1dup[0:64, :, :], srcw, R=[("wb", nm)], W=[("wbuf", wi_)])
            P.dma(w1dup[64:128, :, :], srcw, R=[("wb", nm)], W=[("wbuf", wi_)])
            for grp in range(2):
                base = grp * 64
                b = bank()
                for l in range(32):
                    P.op(pe, lambda l=l, kv=kv, base=base, srcT=srcT, b=b, w1dup=w1dup: nc.tensor.matmul(
                        psf[:, b, 0:127], lhsT=w1dup[base:base + 64, l, :], rhs=srcT[base:base + 64, l:l + 16 * 126 + 1:16],
                        start=(l == 0), stop=(l == 31)),
                         R=[("wbuf", wi_), skey], W=[("ps", b)])
                P.op(act, lambda kv=kv, b=b: nc.scalar.activation(out=gel[0][:, 0:127], in_=psf[:, b, 0:127], func=AF.Identity,
                                                                  bias=cb_b[:, kv:kv + 1], scale=1.0),
                     R=[("ps", b), "cb_b"], W=["gel0"])
                P.op(act, lambda: nc.scalar.activation(out=gel[1][:, 0:127], in_=gel[0][:, 0:127], func=AF.Square), R=["gel0"], W=["gel1"])
                P.op(dve, lambda: nc.vector.tensor_scalar(out=gel[1][:, 0:127], in0=gel[1][:, 0:127], scalar1=0.044715, scalar2=1.0,
                                                          op0=ALU.mult, op1=ALU.add), R=["gel1"], W=["gel1"])
                P.op(dve, lambda: nc.vector.tensor_tensor(out=gel[2][:, 0:127], in0=gel[1][:, 0:127], in1=gel[0][:, 0:127], op=ALU.mult),
                     R=["gel1", "gel0"], W=["gel2"])
                P.op(act, lambda: nc.scalar.activation(out=gel[3][:, 0:127], in_=gel[2][:, 0:127], func=AF.Sigmoid, scale=1.5957691216),
                     R=["gel2"], W=["gel3"])
                P.op(pool, lambda: nc.gpsimd.memset(gT[:], 0.0), W=["gT"])
                P.op(dve, lambda: nc.vector.tensor_tensor(out=gT[:, 0:127], in0=gel[3][:, 0:127], in1=gel[0][:, 0:127], op=ALU.mult),
                     R=["gel3", "gel0"], W=["gT"])
                b2 = bank()
                if kv == 0:
                    P.op(pe, lambda: nc.tensor.matmul(psf[:, b2, 0:128], lhsT=w2k2[:], rhs=gT[:], start=True, stop=True),
                         R=["w2k2", "gT"], W=[("ps", b2)])
                    evac(b2, kcmpT2[:, grp, :], "kcmpT2", ncols=128)
                else:
                    P.op(pe, lambda: nc.tensor.matmul(psf[:, b2, 0:64], lhsT=gT[:], rhs=w2v[:], start=True, stop=True),
                         R=["w2v", "gT"], W=[("ps", b2)])
                    evac(b2, V1c[:, grp, 64:128], "V1c", ncols=64)
        if STOP[0] == 3:
            raise StopBuild()
        barrier()
        if STOP[0] == 40:
            raise StopBuild()
        for g in range(NG):
            xt, xkey = xc.get()
            if STOP[0] == 401:
                raise StopBuild()
            load_rope(g)
            if STOP[0] == 402:
                raise StopBuild()
            norm_group(xt, xkey, 0 + li)
            if STOP[0] == 41:
                raise StopBuild()
            wq, wqk = load_w("nsa_w_in", 0, 512)
            wq2, wq2k = load_w("nsa_w_in", 512, 256)
            for c in range(6):
                w_, wk_ = (wq, wqk) if c < 4 else (wq2, wq2k)
                cc = c if c < 4 else c - 4
                b = proj(lambda kc, cc=cc, w_=w_: w_[:, kc, cc * 128:(cc + 1) * 128], [wk_], hT, "hT", GT)
                rope_from_bank(b, qT[:, c, :], "qT", raw=(qrawT[:, c, :], "qrawT"))
            if STOP[0] == 42:
                raise StopBuild()
            wm_, wmk = load_w("nsa_w_in", 1536, 292)
            for c in range(2):
                b = proj(lambda kc, c=c: wm_[:, kc, 36 + c * 128:36 + (c + 1) * 128], [wmk], hT, "hT", GT)
                evac(b, qmemT[:, c, :], "qmemT")
            if STOP[0] == 43:
                raise StopBuild()
            b = proj(lambda kc: wm_[:, kc, 0:64], [wmk], hT, "hT", GT, M=64)
            P.op(act, lambda b=b: nc.scalar.activation(out=sigT[:], in_=psf[0:36, b, :], func=AF.Sigmoid), R=[("ps", b)], W=["sigT"])
            P.op(dve, lambda: nc.vector.tensor_copy(out=sigH[:], in_=sigT[:]), R=["sigT"], W=["sigH"])
            P.op(dve, lambda: nc.vector.tensor_tensor(out=sigL[:], in0=sigT[:], in1=sigH[:], op=ALU.subtract), R=["sigT", "sigH"], W=["sigL"])
            if STOP[0] == 4:
                raise StopBuild()
            mcg = mc_b[:, g * GT:(g + 1) * GT]
            P.op(pool, lambda: nc.gpsimd.memset(imp[:], 0.0), W=["imp"])

            def gated_dst(h, br, add_ap, add_key, out_ap, out_key):
                def f(bo, rden, rkey):
                    bgate = bank()
                    kk = h * 3 + br
                    P.op(pe, lambda: nc.tensor.matmul(psf[0:64, bgate, :], lhsT=E_flat[0:36, kk * 64:(kk + 1) * 64], rhs=sigH[:], start=True, stop=False),
                         R=["E_b", "sigH"], W=[("ps", bgate)])
                    P.op(pe, lambda: nc.tensor.matmul(psf[0:64, bgate, :], lhsT=E_flat[0:36, kk * 64:(kk + 1) * 64], rhs=sigL[:], start=False, stop=True),
                         R=["E_b", "sigL"], W=[("ps", bgate)])
                    gi = rot(rg, rg_i)
                    P.op(dve, lambda: nc.vector.tensor_tensor(out=rg[gi][:], in0=psf[0:64, bgate, :], in1=rden, op=ALU.mult),
                         R=[("ps", bgate), rkey], W=[("rg", gi)])
                    base = (h % 2) * 64
                    if add_ap is None:
                        P.op(dve, lambda: nc.vector.tensor_tensor(out=out_ap, in0=psf[64:128, bo, :], in1=rg[gi][:], op=ALU.mult),
                             R=[("ps", bo), ("rg", gi)], W=[out_key])
                    else:
                        oi = rot(otmp, otmp_i)
                        P.op(dve, lambda: nc.vector.tensor_tensor(out=otmp[oi][base:base + 64, :], in0=psf[64:128, bo, :], in1=rg[gi][:], op=ALU.mult),
                             R=[("ps", bo), ("rg", gi)], W=[("otmp", oi)])
                        P.op(pool, lambda: nc.gpsimd.tensor_tensor(out=out_ap, in0=otmp[oi][base:base + 64, :], in1=add_ap, op=ALU.add),
                             R=[("otmp", oi), add_key], W=[out_key])
                return f

            for grp in range(2):
                heads = list(range(grp * 6, grp * 6 + 6))
                for h in heads:
                    c, base = h // 2, (h % 2) * 64
                    bs = bank()
                    P.op(pe, lambda c=c, base=base, bs=bs: nc.tensor.matmul(psf[:, bs, :], lhsT=kcmpT2[base:base + 64, grp, :],
                                                                           rhs=qrawT[base:base + 64, c, :], start=True, stop=True),
                         R=["kcmpT2", "qrawT"], W=[("ps", bs)])
                    pi = rot(PT, PT_i)
                    P.op(act, lambda bs=bs, pi=pi: nc.scalar.activation(out=PT[pi][:], in_=psf[:, bs, :], func=AF.Exp, scale=0.125),
                         R=[("ps", bs)], W=[("PT", pi)])
                    P.op(dve, lambda pi=pi: nc.vector.tensor_tensor(out=PT[pi][:], in0=PT[pi][:], in1=mcg, op=ALU.mult),
                         R=[("PT", pi), "mc_b"], W=[("PT", pi)])
                    for tt in range(4):
                        bi_ = bank()
                        P.op(pe, lambda tt=tt, pi=pi, bi_=bi_: nc.tensor.matmul(psf[:, bi_, 0:33], lhsT=PT[pi][:, tt * 128:(tt + 1) * 128],
                                                                               rhs=ov_b[:], start=True, stop=True),
                             R=[("PT", pi), "ov_b"], W=[("ps", bi_)])
                        P.op(dve, lambda bi_=bi_, tt=tt: nc.vector.tensor_scalar_max(out=rec[:, tt:tt + 1], in0=psf[:, bi_, 32:33], scalar1=1e-30),
                             R=[("ps", bi_)], W=[("rec", tt)])
                        P.op(dve, lambda tt=tt: nc.vector.reciprocal(out=rec[:, tt:tt + 1], in_=rec[:, tt:tt + 1]), R=[("rec", tt)], W=[("rec", tt)])
                        P.op(dve, lambda bi_=bi_, tt=tt: nc.vector.scalar_tensor_tensor(out=imp[:, tt, grp, :], in0=psf[:, bi_, 0:32],
                                                                                        scalar=rec[:, tt:tt + 1], in1=imp[:, tt, grp, :],
                                                                                        op0=ALU.mult, op1=ALU.add),
                             R=[("ps", bi_), ("rec", tt), "imp"], W=["imp"])
                    bo = bankL()
                    P.op(pe, lambda pi=pi, bo=bo: nc.tensor.matmul(psf[:, bo, :], lhsT=V1c[:, grp, :], rhs=PT[pi][:], start=True, stop=True),
                         R=["V1c", ("PT", pi)], W=[("ps", bo)])
                    ri = rot(rd, rd_i)
                    P.op(dve, lambda bo=bo, ri=ri: nc.vector.tensor_scalar_max(out=rd[ri][0:64, :], in0=psf[0:64, bo, :], scalar1=1e-18),
                         R=[("ps", bo)], W=[("rd", ri)])
                    P.op(act, lambda ri=ri: nc.scalar.activation(out=rd[ri][0:64, :], in_=rd[ri][0:64, :], func=AF.Ln), R=[("rd", ri)], W=[("rd", ri)])
                    P.op(act, lambda ri=ri: nc.scalar.activation(out=rd[ri][0:64, :], in_=rd[ri][0:64, :], func=AF.Exp, scale=-1.0), R=[("rd", ri)], W=[("rd", ri)])
                    hl = h - grp * 6
                    gated_dst(h, 0, None, None, cmpo[base:base + 64, hl, :], "cmpo")(bo, rd[ri][0:64, :], ("rd", ri))
                for tt in range(4):
                    qi = g * 4 + tt
                    P.op(dve, lambda tt=tt, qi=qi: nc.vector.tensor_tensor(out=imp2[:, tt, grp, :], in0=imp[:, tt, grp, :], in1=selF[:, 30 - 2 * qi:62 - 2 * qi], op=ALU.max),
                         R=["imp", "selF"], W=["imp2"])
                    P.op(dve, lambda tt=tt, qi=qi: nc.vector.tensor_tensor(out=imp2[:, tt, grp, :], in0=imp2[:, tt, grp, :], in1=selA[:, 30 - 2 * qi:62 - 2 * qi], op=ALU.add),
                         R=["imp2", "selA"], W=["imp2"])
                    P.op(dve, lambda tt=tt: nc.vector.memset(imp2[:, tt, grp, 0:1], 1.0e9), R=["imp2"], W=["imp2"])
                    P.op(dve, lambda tt=tt: nc.vector.max(out=m8[:, 0:8], in_=imp2[:, tt, grp, :]), R=["imp2"], W=["m8a"])
                    P.op(dve, lambda tt=tt: nc.vector.match_replace(out=imp3[:, tt, grp, :], in_to_replace=m8[:, 0:8], in_values=imp2[:, tt, grp, :],
                                                                    imm_value=-3.0e38),
                         R=["imp2", "m8a"], W=["imp3"])
                    P.op(dve, lambda tt=tt: nc.vector.max(out=m8[:, 8:16], in_=imp3[:, tt, grp, :]), R=["imp3"], W=["m8b"])
                    P.op(dve, lambda tt=tt: nc.vector.tensor_scalar(out=selm[:, tt, grp * 32:(grp + 1) * 32], in0=imp2[:, tt, grp, :],
                                                                    scalar1=m8[:, 15:16], scalar2=None, op0=ALU.is_ge),
                         R=["imp2", "m8b"], W=["selm"])
                if STOP[0] == 5:
                    raise StopBuild()
                if grp == 0:
                    P.op(pool, lambda: nc.gpsimd.memset(selm[:, :, 32:64], 0.0), W=["selm"])
                for tt in range(4):
                    P.op(pe, lambda tt=tt: nc.tensor.transpose(psb[0:64, tt * 128:(tt + 1) * 128], selm[:, tt, :], ident_b),
                         R=["selm", "misc_b"], W=["psb"])
                P.op(act, lambda: nc.scalar.copy(out=selT[:], in_=psb[0:64, 0:512]), R=["psb"], W=["selT"])
                nkb = 4 * g + 4
                for j in range(nkb):
                    bm = bank()
                    P.op(pe, lambda j=j, bm=bm: nc.tensor.matmul(psf[:, bm, :], lhsT=E_b[:, grp, j * 128:(j + 1) * 128], rhs=selT[:], start=True, stop=True),
                         R=["E_b", "selT"], W=[("ps", bm)])
                    if j >= 4 * g:
                        P.op(dve, lambda j=j, bm=bm: nc.vector.tensor_tensor(out=maskT[:, j, :], in0=psf[:, bm, :], in1=cm_b[:, j - 4 * g, :], op=ALU.mult),
                             R=[("ps", bm), "cm_b"], W=["maskT"])
                    else:
                        evac(bm, maskT[:, j, :], "maskT")
                if STOP[0] == 6:
                    raise StopBuild()
                for h in heads:
                    c, base = h // 2, (h % 2) * 64
                    hl = h - grp * 6
                    blocks = [dict(kT=ksT2[base:base + 64, grp, j * 128:(j + 1) * 128], kkey="ksT2", v1=V1s[:, j, grp, :], vkey="V1s",
                                   mask=(maskT[:, j, :], "maskT"), c0=128 * max(0, j - 4 * g)) for j in range(nkb)]
                    attn_head(qT[base:base + 64, c, :], "qT", blocks, gated_dst(h, 1, cmpo[base:base + 64, hl, :], "cmpo", oacc[base:base + 64, :], "oacc"))
                    j0 = max(0, 4 * g - 4)
                    order = [4 * g] + [j for j in range(j0, nkb) if j != 4 * g]
                    blocks = [dict(kT=kwT2[base:base + 64, grp, j * 128:(j + 1) * 128], kkey="kwT2", v1=V1w[:, j, grp, :], vkey="V1w",
                                   mask=(wm_b[:, j - 4 * g + 4, :], "wm_b"),
                                   c0=128 * max(0, j - 4 * g), c1=(GT if j >= 4 * g else 128 * (j - 4 * g + 5))) for j in order]
                    attn_head(qT[base:base + 64, c, :], "qT", blocks, gated_dst(h, 2, oacc[base:base + 64, :], "oacc", oT[base:base + 64, c, :], "hT"))
            mem_attn()
            wo_ffn(s, li, g, xt, xkey, last)
            final_store(s, g, xt, xkey, last)

    try:
        for s in range(n_seq):
            for li in range(n_layers):
                src_d = xT_d if li == 0 else xs_d
                last = (li == n_layers - 1)
                if li % 2 == 0:
                    dsa_layer(s, li, src_d, last)
                else:
                    nsa_layer(s, li, src_d, last)
    except StopBuild:
        pass
    barrier()
    P.drain()
    return nc, P


def host_inputs(inputs, core, n_seq=SEQ_PER_CORE):
    x = inputs["x"][core * n_seq:(core + 1) * n_seq]
    mem = inputs["mem"][core * n_seq:(core + 1) * n_seq]
    m = {}
    m["xT"] = np.ascontiguousarray(np.transpose(x, (0, 2, 1)))
    m["memT"] = np.ascontiguousarray(np.transpose(mem, (0, 2, 1)))
    g = np.concatenate([inputs["attn_norm"], inputs["mem_norm"], inputs["ffn_norm"], inputs["final_norm"][None]], axis=0)
    m["gam"] = np.ascontiguousarray(g.reshape(7, 8, 128).transpose(2, 0, 1))
    m["ckvg"] = np.ascontiguousarray(inputs["dsa_ckv_norm"][0].reshape(128, 1))
    m["posk"] = np.ascontiguousarray(inputs["nsa_cmp_pos_k"][0].reshape(16, 128).T)
    m["posv"] = np.ascontiguousarray(inputs["nsa_cmp_pos_v"][0].reshape(16, 128).T)
    m["dsa_w_in"] = inputs["dsa_w_in"][0]
    m["nsa_w_in"] = inputs["nsa_w_in"][0]
    for i in range(2):
        m[f"mem_w_kv{i}"] = inputs["mem_w_kv"][i]
        m[f"w_o{i}"] = inputs["w_o"][i]
        m[f"ffn_w_in{i}"] = inputs["ffn_w_in"][i]
        m[f"ffn_w_down{i}"] = inputs["ffn_w_down"][i]
    m["cmp_k_w1"] = inputs["nsa_cmp_k_w1"][0]
    m["cmp_v_w1"] = inputs["nsa_cmp_v_w1"][0]
    m["dsa_w_uk"] = inputs["dsa_w_uk"][0]
    m["dsa_w_uv"] = inputs["dsa_w_uv"][0]
    m["cmp_k_w2"] = inputs["nsa_cmp_k_w2"][0]
    m["cmp_v_w2"] = inputs["nsa_cmp_v_w2"][0]
    for k, v in make_consts().items():
        m["c_" + k] = v
    return {k: np.ascontiguousarray(v, dtype=np.float32) for k, v in m.items()}


def build2(n_seq, n_layers=2):
    _, P1 = build(n_seq, n_layers)
    return build(n_seq, n_layers, needed=P1.waited)


def kernel(**inputs):
    inputs = {k: np.asarray(v) for k, v in inputs.items()}
    nc, _ = build2(SEQ_PER_CORE)
    in_maps = [host_inputs(inputs, c) for c in range(NCORES)]
    res = run_bass_kernel_spmd(nc, in_maps, core_ids=list(range(NCORES)))
    outs = [np.transpose(r["outT"], (0, 2, 1)) for r in res.results]
    return np.ascontiguousarray(np.concatenate(outs, axis=0)).astype(np.float32)
```

```python
import numpy as np
import concourse.bass as bass
import concourse.mybir as mybir
from concourse.bass_utils import run_bass_kernel_spmd

F32 = mybir.dt.float32
BF16 = mybir.dt.bfloat16
AF = mybir.ActivationFunctionType
ALU = mybir.AluOpType
AX = mybir.AxisListType

T = 2048
NG = 4
GT = 512
D = 1024
DFF = 2816
NCORES = 8
SEQ_PER_CORE = 4
EPS = 1e-6
SEM_L = 8000
N_DSEM = 24
NEG = -1.0e30

DSA_W = 1752
NSA_W = 1828


class Eng:
    def __init__(self, P, name, b, pe=False):
        self.P, self.name, self.b, self.pe = P, name, b, pe
        self.cnt = 0
        self.rank = 0
        self.rankmap = {}
        self.sems = []
        self.seen = {}

    def _sem(self, r):
        e = (r - 1) // SEM_L
        while len(self.sems) <= e:
            self.sems.append(self.P.new_sem(f"{self.name}{len(self.sems)}"))
        return self.sems[e], (r - 1) % SEM_L + 1

    def sem_for(self, c):
        return self._sem(self.rankmap[c])


class DSem:
    def __init__(self, P, i):
        self.sem = P.new_sem(f"dma{i}")
        self.cnt = 0
        self.pe = False
        self.name = f"dma{i}"

    def sem_for(self, c):
        return self.sem, 16 * c


class Prog:
    def __init__(self, nc, needed=None):
        self.nc = nc
        self.nsem = 0
        self.needed = needed
        self.waited = set()
        self.pe = Eng(self, "pe", nc.tensor, pe=True)
        self.act = Eng(self, "act", nc.scalar)
        self.dve = Eng(self, "dve", nc.vector)
        self.pool = Eng(self, "pool", nc.gpsimd)
        self.sp = Eng(self, "sp", nc.sync)
        self.dsems = [DSem(self, i) for i in range(N_DSEM)]
        self.di = 0
        self.R = {}
        self.nins = 0
        self.nwait = 0
        self.ninc = 0

    def new_sem(self, name):
        self.nsem += 1
        return self.nc.semaphore(name).__enter__()

    def _wait(self, eng, obj, c):
        if eng.seen.get(obj, 0) >= c:
            return
        if isinstance(obj, Eng):
            self.waited.add((obj.name, c))
        sem, v = obj.sem_for(c)
        eng.b.wait_ge(sem, v)
        eng.seen[obj] = c
        self.nwait += 1

    def _deps(self, eng, reads, writes, nowaw=False):
        for k in reads:
            r = self.R.get(k)
            if r is None:
                continue
            for obj, c in r[0].items():
                if obj is eng and eng.pe:
                    continue
                self._wait(eng, obj, c)
        for k in writes:
            r = self.R.get(k)
            if r is None:
                continue
            if not nowaw:
                for obj, c in r[0].items():
                    if obj is eng and eng.pe:
                        continue
                    self._wait(eng, obj, c)
            for obj, c in r[1].items():
                if obj is eng and eng.pe:
                    continue
                self._wait(eng, obj, c)

    def _mark(self, obj, c, reads, writes):
        for k in reads:
            r = self.R.get(k)
            if r is None:
                r = self.R[k] = [{}, {}]
            r[1][obj] = c
        for k in writes:
            r = self.R.get(k)
            if r is None or r[1]:
                self.R[k] = [{obj: c}, {}]
            else:
                r[0][obj] = c

    def op(self, eng, fn, R=(), W=()):
        self._deps(eng, R, W)
        ins = fn()
        eng.cnt += 1
        if self.needed is None or (eng.name, eng.cnt) in self.needed:
            eng.rank += 1
            sem, _ = eng._sem(eng.rank)
            ins.then_inc(sem, 1)
            self.ninc += 1
        eng.rankmap[eng.cnt] = eng.rank
        self._mark(eng, eng.cnt, R, W)
        self.nins += 1
        return ins

    def dma(self, out, in_, R=(), W=(), q=None, nowaw=True):
        eng = q or self.sp
        d = self.dsems[self.di % N_DSEM]
        self.di += 1
        if d.cnt > 0:
            self._wait(eng, d, d.cnt)
        self._deps(eng, R, W, nowaw=nowaw)
        ins = eng.b.dma_start(out=out, in_=in_)
        d.cnt += 1
        ins.then_inc(d.sem, 16)
        self._mark(d, d.cnt, R, W)
        self.nins += 1
        return ins

    def drain(self):
        for d in self.dsems:
            if d.cnt > 0:
                self._wait(self.sp, d, d.cnt)


class Ring:
    def __init__(self, items):
        self.items = items
        self.i = 0

    def next(self):
        it = self.items[self.i % len(self.items)]
        self.i += 1
        return it


def _rope_tables():
    inv = 500000.0 ** (-np.arange(0, 16, 2, dtype=np.float64) / 16)
    ang = np.arange(T, dtype=np.float64)[:, None] * inv[None, :]
    cos, sin = np.cos(ang).astype(np.float32), np.sin(ang).astype(np.float32)
    C = np.ones((128, T), np.float32)
    S = np.zeros((128, T), np.float32)
    for r in range(128):
        d = r % 64
        if d < 8:
            C[r] = cos[:, d]
            S[r] = -sin[:, d]
        elif d < 16:
            C[r] = cos[:, d - 8]
            S[r] = sin[:, d - 8]
    return C, S


def make_consts():
    c = {}
    C, S = _rope_tables()
    c["ropeC"], c["ropeS"] = C, S
    perm = np.zeros((128, 128), np.float32)
    for m in range(128):
        d = m % 64
        base = m - d
        if d < 8:
            perm[base + d + 8, m] = 1.0
        elif d < 16:
            perm[base + d - 8, m] = 1.0
    ident = np.eye(128, dtype=np.float32)
    sl = np.arange(128)
    cb = np.where(sl[None, :] > sl[:, None], NEG, 0.0).astype(np.float32)
    c["misc128"] = np.stack([perm, ident, np.ones((128, 128), np.float32), cb], axis=1)
    tl = np.arange(512)
    cm = np.zeros((128, 4, 512), np.float32)
    for r in range(4):
        cm[:, r, :] = ((128 * r + sl)[:, None] <= tl[None, :]).astype(np.float32)
    c["cm"] = cm
    wm = np.zeros((128, 8, 512), np.float32)
    for j in range(8):
        s = 128 * (j - 4) + sl
        wm[:, j, :] = ((s[:, None] <= tl[None, :]) & (s[:, None] > tl[None, :] - 512)).astype(np.float32)
    c["wm"] = wm
    cc = np.arange(128)
    mc = ((16 * cc + 31)[:, None] <= np.arange(T)[None, :]).astype(np.float32)
    mc[127] = 0.0
    c["mcT"] = mc
    c0 = cc * 16
    s0 = np.arange(32) * 64
    ov = np.minimum(c0[:, None] + 32, s0[None, :] + 64) - np.maximum(c0[:, None], s0[None, :])
    ov = (np.clip(ov, 0, None) / 32.0).astype(np.float32)
    ov33 = np.concatenate([ov, np.ones((128, 1), np.float32)], axis=1)
    ov33[127] = 0.0
    c["ov33"] = ov33
    tt = np.arange(T)
    cur = tt // 64
    n = np.arange(32)
    forced = (n[None] == 0) | (n[None] == cur[:, None]) | (n[None] == cur[:, None] - 1)
    adm = n[None] <= cur[:, None]
    Ff = np.where(forced, 1.0e9, 0.0).astype(np.float32)
    Ab = np.where(adm, 0.0, NEG).astype(np.float32)
    pl = (np.arange(128) >= 64).astype(np.int64)
    y = np.arange(62) - 30
    c["selF"] = np.where((y[None] == pl[:, None]) | (y[None] == pl[:, None] - 1), 1.0e9, 0.0).astype(np.float32)
    c["selA"] = np.where(y[None] <= pl[:, None], 0.0, NEG).astype(np.float32)
    E = np.zeros((64, 2, T), np.float32)
    for g in range(2):
        for nn in range(32):
            E[g * 32 + nn, g, nn * 64:(nn + 1) * 64] = 1.0
    c["Eexp"] = E
    c["bis"] = np.tile((2.0 ** -(np.arange(32) + 1)).astype(np.float32)[None, :], (128, 1))
    return c


WSPEC = [
    ("dsa_w_in", 1024, DSA_W), ("nsa_w_in", 1024, NSA_W),
    ("mem_w_kv0", 1024, 512), ("mem_w_kv1", 1024, 512),
    ("w_o0", 1024, 1024), ("w_o1", 1024, 1024),
    ("ffn_w_in0", 1024, 2 * DFF), ("ffn_w_in1", 1024, 2 * DFF),
    ("ffn_w_down0", DFF, 1024), ("ffn_w_down1", DFF, 1024),
    ("cmp_k_w1", 2048, 128), ("cmp_v_w1", 2048, 128),
]
SMALLW = [("dsa_w_uk", 128, 48), ("dsa_w_uv", 128, 64), ("cmp_k_w2", 128, 64), ("cmp_v_w2", 128, 64)]
CONST_SHAPES = None


class StopBuild(Exception):
    pass


STOP = [0]


def build(n_seq, n_layers=2, needed=None):
    nc = bass.Bass("TRN2", target_bir_lowering=False)
    P = Prog(nc, needed)
    pe, act, dve, pool = P.pe, P.act, P.dve, P.pool
    consts = make_consts()

    def din(name, shape, dt=F32):
        return nc.dram_tensor(name, list(shape), dt, kind="ExternalInput").ap()

    xT_d = din("xT", [n_seq, D, T])
    memT_d = din("memT", [n_seq, D, 256])
    gam_d = din("gam", [128, 7, 8])
    ckvg_d = din("ckvg", [128, 1])
    posk_d = din("posk", [128, 16])
    posv_d = din("posv", [128, 16])
    w_d = {n: din(n, [K, N]) for n, K, N in WSPEC}
    sw_d = {n: din(n, [K, N]) for n, K, N in SMALLW}
    c_d = {k: din("c_" + k, v.shape) for k, v in consts.items()}
    outT_d = nc.dram_tensor("outT", [n_seq, D, T], F32, kind="ExternalOutput").ap()
    xs_d = nc.dram_tensor("xs", [n_seq, D, T], F32).ap()
    wb_d = {n: nc.dram_tensor("b_" + n, [K, N], BF16).ap() for n, K, N in WSPEC}

    ARENA_BYTES = 212000
    arena = nc.alloc_sbuf_tensor("arena", [128, ARENA_BYTES // 4], F32)
    a_base = nc.lookup_mloc(arena).addr
    a_ptr = [0]
    a_max = [0]
    DT_SZ = {F32: 4, BF16: 2}

    def sb(name, shape, dt=F32):
        nbytes = int(np.prod(shape[1:])) * DT_SZ[dt]
        off = (a_ptr[0] + 31) // 32 * 32
        a_ptr[0] = off + nbytes
        a_max[0] = max(a_max[0], a_ptr[0])
        assert a_ptr[0] <= ARENA_BYTES, (name, a_ptr[0])
        return nc.alloc_sbuf_tensor_at("s_" + name, list(shape), dt, offset=a_base + off)

    def barrier():
        engs = [P.pe, P.act, P.dve, P.pool, P.sp]
        for e in engs:
            for o in engs:
                if o is not e and o.cnt > 0:
                    P._wait(e, o, o.cnt)
            for d_ in P.dsems:
                if d_.cnt > 0:
                    P._wait(e, d_, d_.cnt)

    xg = [sb("xg0", [128, 8, GT]), sb("xg1", [128, 8, GT])]
    wbuf = [sb(f"wbuf{i}", [128, 8, 512], BF16) for i in range(2)]
    NSTG = 4
    stage = [xg[i // 2][:].rearrange("p a b -> p (a b)")[:, (i % 2) * 2048:(i % 2 + 1) * 2048] for i in range(NSTG)]
    stage_b = [wbuf[i // 2][:].rearrange("p a b -> p (a b)")[:, (i % 2) * 2048:(i % 2 + 1) * 2048] for i in range(NSTG)]
    SCH = 2048
    misc_f = sb("misc_f", [128, 4, 128])
    misc_b = sb("misc_b", [128, 3, 128], BF16)
    cm_b = sb("cm_b", [128, 4, 512], BF16)
    wm_b = sb("wm_b", [128, 8, 512], BF16)
    mc_b = sb("mc_b", [128, T], BF16)
    ov_b = sb("ov_b", [128, 33], BF16)
    selF = sb("selF", [128, 62])
    selA = sb("selA", [128, 62])
    E_b = sb("E_b", [64, 2, T], BF16)
    bis = sb("bis", [128, 32])
    gam = sb("gam", [128, 7, 8])
    ckvg = sb("ckvg", [128, 1])
    cvt_i = [0]
    epsT = sb("epsT", [128, 1])
    P.op(pool, lambda: nc.gpsimd.memset(epsT[:], EPS), W=["epsT"])

    def cvt(dst_ap, src_ap, R, W):
        engs = [dve, pool, act]
        e = engs[cvt_i[0] % 3]
        cvt_i[0] += 1
        if e is act:
            P.op(e, lambda: nc.scalar.copy(out=dst_ap, in_=src_ap), R=R, W=W)
        else:
            P.op(e, lambda: e.b.tensor_copy(out=dst_ap, in_=src_ap), R=R, W=W)

    def load_const(dst, src_d, key, bf=False, npart=128, cols=None):
        if not bf:
            P.dma(dst[:], src_d, W=[key])
            return
        shp = list(src_d.shape)
        flat = int(np.prod(shp[1:]))
        srcf = src_d if len(shp) == 2 else src_d.rearrange("p a b -> p (a b)")
        dstf = dst[:] if len(shp) == 2 else dst[:].rearrange("p a b -> p (a b)")
        for o in range(0, flat, SCH):
            n = min(SCH, flat - o)
            i = cvt_i[0] % NSTG
            P.dma(stage[i][0:shp[0], 0:n], srcf[:, o:o + n], W=[("stage", i)], nowaw=False)
            cvt(dstf[:, o:o + n], stage[i][0:shp[0], 0:n], R=[("stage", i)], W=[key])

    load_const(misc_f, c_d["misc128"], "misc_f")
    P.op(dve, lambda: nc.vector.tensor_copy(out=misc_b[:], in_=misc_f[:, 0:3, :]), R=["misc_f"], W=["misc_b"])
    load_const(cm_b, c_d["cm"], "cm_b", bf=True)
    load_const(wm_b, c_d["wm"], "wm_b", bf=True)
    load_const(mc_b, c_d["mcT"], "mc_b", bf=True)
    load_const(ov_b, c_d["ov33"], "ov_b", bf=True)
    load_const(selF, c_d["selF"], "selF")
    load_const(selA, c_d["selA"], "selA")
    load_const(E_b, c_d["Eexp"], "E_b", bf=True)
    load_const(bis, c_d["bis"], "bis")
    load_const(gam, gam_d, "gam")
    load_const(ckvg, ckvg_d, "ckvg")
    perm_b, ident_b, ones_b = misc_b[:, 0, :], misc_b[:, 1, :], misc_b[:, 2, :]
    E_flat = E_b[:].rearrange("p g s -> p (g s)")
    ones_f = misc_f[:, 2, :]
    cbias_f = misc_f[:, 3, :]

    for name, K, N in WSPEC:
        for kc in range(K // 128):
            for o in range(0, N, SCH):
                n = min(SCH, N - o)
                i = cvt_i[0] % NSTG
                P.dma(stage[i][:, 0:n], w_d[name][kc * 128:(kc + 1) * 128, o:o + n], W=[("stage", i)], nowaw=False)
                j = i
                cvt(stage_b[j][:, 0:n], stage[i][:, 0:n], R=[("stage", i)], W=[("stageb", j)])
                P.dma(wb_d[name][kc * 128:(kc + 1) * 128, o:o + n], stage_b[j][:, 0:n],
                      R=[("stageb", j)], W=[("wb", name)])
    wuk2 = sb("wuk2", [128, 128], BF16)
    wuv = sb("wuv", [128, 64], BF16)
    w2k2 = sb("w2k2", [128, 128], BF16)
    w2v = sb("w2v", [128, 64], BF16)
    smst = sb("smst", [128, 4, 64])
    P.op(pool, lambda: nc.gpsimd.memset(wuk2[:], 0.0), W=["wuk2"])
    P.dma(smst[:, 0, 0:48], sw_d["dsa_w_uk"], W=["smst"])
    P.dma(smst[:, 1, :], sw_d["dsa_w_uv"], W=["smst"])
    P.dma(smst[:, 2, :], sw_d["cmp_k_w2"], W=["smst"])
    P.dma(smst[:, 3, :], sw_d["cmp_v_w2"], W=["smst"])
    P.op(dve, lambda: nc.vector.tensor_copy(out=wuk2[:, 16:64], in_=smst[:, 0, 0:48]), R=["smst"], W=["wuk2"])
    P.op(dve, lambda: nc.vector.tensor_copy(out=wuk2[:, 80:128], in_=smst[:, 0, 0:48]), R=["smst"], W=["wuk2"])
    P.op(dve, lambda: nc.vector.tensor_copy(out=wuv[:], in_=smst[:, 1, :]), R=["smst"], W=["wuv"])
    P.op(dve, lambda: nc.vector.tensor_copy(out=w2k2[:, 0:64], in_=smst[:, 2, :]), R=["smst"], W=["w2k2"])
    P.op(dve, lambda: nc.vector.tensor_copy(out=w2k2[:, 64:128], in_=smst[:, 2, :]), R=["smst"], W=["w2k2"])
    P.op(dve, lambda: nc.vector.tensor_copy(out=w2v[:], in_=smst[:, 3, :]), R=["smst"], W=["w2v"])
    posf = sb("posf", [128, 2, 16])
    posb = sb("posb", [128, 2, 16], BF16)
    P.dma(posf[:, 0, :], posk_d, W=["posf"])
    P.dma(posf[:, 1, :], posv_d, W=["posf"])
    P.op(dve, lambda: nc.vector.tensor_copy(out=posb[:], in_=posf[:]), R=["posf"], W=["posb"])

    barrier()
    psf = nc.alloc_psum_tensor("psf", [128, 7, 512], F32)
    psb = nc.alloc_psum_tensor("psb", [128, 1024], BF16)
    bank_i = [0]

    def bank():
        b = bank_i[0] % 4
        bank_i[0] += 1
        return b

    bankL_i = [0]

    def bankL():
        b = 4 + bankL_i[0] % 3
        bankL_i[0] += 1
        return b

    def bank2():
        if not hasattr(bank2, "i"):
            bank2.i = 0
        b = (bank2.i % 2) * 2
        bank2.i += 1
        return b

    xg_i = [0]
    wbuf_i = [0]
    hT = sb("hT", [128, 8, GT], BF16)
    oT = hT
    sq = [sb(f"sq{i}", [128, GT]) for i in range(1)]
    sq_i = [0]
    sqh = [sb(f"sqh{i}", [128, 2, GT], BF16) for i in range(1)]
    sqh_i = [0]
    rstd = sb("rstd", [128, GT])
    wdn = [sb(f"wdn{i}", [128, 11, 128], BF16) for i in range(2)]
    wdn_i = [0]
    ropeCs = sb("ropeCs", [128, GT])
    ropeSs = sb("ropeSs", [128, GT])
    pre = [sb(f"pre{i}", [128, GT], BF16) for i in range(1)]
    pre_i = [0]
    rtmp = [sb(f"rtmp{i}", [128, GT]) for i in range(2)]
    rtmp_i = [0]
    qT = sb("qT", [128, 6, GT], BF16)
    qmemT = sb("qmemT", [128, 2, GT], BF16)
    mk_mark = a_ptr[0]
    maskT = sb("maskT", [128, 16, GT], BF16)
    mk_end = a_ptr[0]
    a_ptr[0] = mk_mark
    actT = sb("maskT", [128, 11, GT], BF16)
    a_ptr[0] = mk_mark
    memnT = sb("maskT", [128, 8, 256], BF16)
    a_ptr[0] = mk_end
    memf = xg[0][:, :, 0:256]
    sil = [sb(f"sil{i}", [128, GT], BF16) for i in range(1)]
    sil_i = [0]
    kmemT = sb("kmemT", [128, 2, 256], BF16)
    v1m = sb("v1m", [128, 2, 4, 128], BF16)
    PT = [sb(f"PT{i}", [128, GT], BF16) for i in range(4)]
    PT_i = [0]
    rd = [sb(f"rd{i}", [128, GT]) for i in range(2)]
    rd_i = [0]
    layer_mark = a_ptr[0]
    KT2 = sb("KT2", [128, T], BF16)
    kidxT2 = sb("kidxT2", [128, T], BF16)
    V1 = sb("V1", [128, 16, 128], BF16)
    ckvn = sb("ckvn", [128, GT], BF16)
    ckvf = sb("ckvf", [128, GT])
    wkr2 = sb("wkr2", [128, 8, 128], BF16)
    wki2 = sb("wki2", [128, 8, 128], BF16)
    widx = sb("widx", [128, 4, 8])
    Dh = [sb(f"Dh{i}", [128, 8, 128], BF16) for i in range(2)]
    Dh_i = [0]
    relu_t = [sb(f"relu{i}", [128, 512], BF16) for i in range(3)]
    relu_i = [0]
    sc = [sb(f"sc{i}", [128, T]) for i in range(2)]
    sc_i = [0]
    bst = sb("bst", [128, 2, 8])
    wtab = sb("wtab", [128, 2, 32])
    wtab2 = sb("wtab2", [128, 2, 32])
    mask_tm = [sb(f"masktm{i}", [128, T], BF16) for i in range(2)]
    mask_i = [0]
    qidxT = sb("qidxT", [128, 4, GT], BF16)
    dsa_end = a_ptr[0]
    a_ptr[0] = layer_mark
    ksT2 = sb("ksT2", [128, 2, T], BF16)
    kwT2 = sb("kwT2", [128, 2, T], BF16)
    V1s = sb("V1s", [128, 16, 2, 128], BF16)
    V1w = sb("V1w", [128, 16, 2, 128], BF16)
    ph_mark = a_ptr[0]
    kcT = sb("kcT", [128, T], BF16)
    vcT = sb("vcT", [128, T], BF16)
    ph_end = a_ptr[0]
    a_ptr[0] = ph_mark
    cmpo = sb("cmpo", [128, 6, GT], BF16)
    oacc = sb("oacc", [128, GT])
    a_ptr[0] = max(a_ptr[0], ph_end)
    cb_b = sb("cb_b", [128, 2])
    kcmpT2 = sb("kcmpT2", [128, 2, 128], BF16)
    V1c = sb("V1c", [128, 2, 128], BF16)
    gel = [sb(f"gel{i}", [128, 128]) for i in range(4)]
    gT = sb("gT", [128, 128], BF16)
    qrawT = sb("qrawT", [128, 6, GT], BF16)
    sigT = sb("sigT", [36, GT])
    sigH = sb("sigH", [36, GT], BF16)
    sigL = sb("sigL", [36, GT], BF16)
    imp = sb("imp", [128, 4, 2, 32])
    imp2 = sb("imp2", [128, 4, 2, 32])
    imp3 = sb("imp3", [128, 4, 2, 32])
    m8 = sb("m8", [128, 16])
    selm = sb("selm", [128, 4, 64], BF16)
    selT = sb("selT", [64, GT], BF16)
    rec = sb("rec", [128, 8])
    otmp = [sb(f"otmp{i}", [128, GT]) for i in range(1)]
    otmp_i = [0]
    rg = [sb(f"rg{i}", [64, GT]) for i in range(2)]
    rg_i = [0]
    nsa_end = a_ptr[0]
    print("SBUF plan: dsa_end", dsa_end, "nsa_end", nsa_end, "limit", ARENA_BYTES)
    a_ptr[0] = max(dsa_end, nsa_end)

    def rot(lst, ctr):
        i = ctr[0] % len(lst)
        ctr[0] += 1
        return i

    def load_w(name, col0, ncols, nk=8):
        i = rot(wbuf, wbuf_i)
        src = wb_d[name].rearrange("(c p) n -> p c n", p=128)[:, 0:nk, col0:col0 + ncols]
        P.dma(wbuf[i][:, 0:nk, 0:ncols], src, R=[("wb", name)], W=[("wbuf", i)])
        return wbuf[i], ("wbuf", i)

    def sumsq_mm(b, src_ap, src_key, ncols, first, last):
        i = rot(sq, sq_i)
        P.op(act, lambda: nc.scalar.activation(out=sq[i][:, 0:ncols], in_=src_ap, func=AF.Square), R=[src_key], W=[("sq", i)])
        j = rot(sqh, sqh_i)
        P.op(dve, lambda: nc.vector.tensor_copy(out=sqh[j][:, 0, 0:ncols], in_=sq[i][:, 0:ncols]), R=[("sq", i)], W=[("sqh", j)])
        P.op(pool, lambda: nc.gpsimd.tensor_tensor(out=sqh[j][:, 1, 0:ncols], in0=sq[i][:, 0:ncols], in1=sqh[j][:, 0, 0:ncols], op=ALU.subtract),
             R=[("sq", i), ("sqh", j)], W=[("sqh", j)])
        P.op(pe, lambda: nc.tensor.matmul(psf[:, b, 0:ncols], lhsT=ones_b, rhs=sqh[j][:, 0, 0:ncols], start=first, stop=False),
             R=[("sqh", j), "misc_b"], W=[("ps", b)])
        P.op(pe, lambda: nc.tensor.matmul(psf[:, b, 0:ncols], lhsT=ones_b, rhs=sqh[j][:, 1, 0:ncols], start=False, stop=last),
             R=[("sqh", j), "misc_b"], W=[("ps", b)])

    def norm_group(xt, xkey, gidx, ncols=GT, dst=None, dkey="hT"):
        dst = hT if dst is None else dst
        b = bank()
        for c in range(8):
            sumsq_mm(b, xt[:, c, 0:ncols], xkey, ncols, c == 0, c == 7)
        P.op(act, lambda: nc.scalar.activation(out=rstd[:, 0:ncols], in_=psf[:, b, 0:ncols], func=AF.Sqrt,
                                               scale=1.0 / D, bias=epsT[:, 0:1]),
             R=[("ps", b)], W=["rstd"])
        P.op(dve, lambda: nc.vector.reciprocal(out=rstd[:, 0:ncols], in_=rstd[:, 0:ncols]), R=["rstd"], W=["rstd"])
        for c in range(8):
            P.op(dve, lambda c=c: nc.vector.scalar_tensor_tensor(
                out=dst[:, c, 0:ncols], in0=xt[:, c, 0:ncols], scalar=gam[:, gidx, c:c + 1],
                in1=rstd[:, 0:ncols], op0=ALU.mult, op1=ALU.mult),
                 R=[xkey, "rstd", "gam"], W=[dkey])

    def proj(lhs_fn, lhs_keys, rhs_t, rhs_key, ncols, M=128):
        b = bank()
        for kc in range(8):
            P.op(pe, lambda kc=kc: nc.tensor.matmul(psf[0:M, b, 0:ncols], lhsT=lhs_fn(kc), rhs=rhs_t[:, kc, 0:ncols],
                                                    start=(kc == 0), stop=(kc == 7)),
                 R=list(lhs_keys) + [rhs_key], W=[("ps", b)])
        return b

    def rope_from_bank(b, dst_ap, dkey, ncols=GT, raw=None):
        i = rot(pre, pre_i)
        P.op(act, lambda: nc.scalar.copy(out=pre[i][:, 0:ncols], in_=psf[:, b, 0:ncols]), R=[("ps", b)], W=[("pre", i)])
        if raw is not None:
            P.op(pool, lambda: nc.gpsimd.tensor_copy(out=raw[0], in_=pre[i][:, 0:ncols]), R=[("pre", i)], W=[raw[1]])
        b2 = bank()
        P.op(pe, lambda: nc.tensor.matmul(psf[:, b2, 0:ncols], lhsT=perm_b, rhs=pre[i][:, 0:ncols], start=True, stop=True),
             R=[("pre", i), "misc_b"], W=[("ps", b2)])
        j = rot(rtmp, rtmp_i)
        P.op(dve, lambda: nc.vector.tensor_tensor(out=rtmp[j][:, 0:ncols], in0=psf[:, b2, 0:ncols], in1=ropeSs[:, 0:ncols], op=ALU.mult),
             R=[("ps", b2), "ropeS"], W=[("rtmp", j)])
        k = rot(rtmp, rtmp_i)
        P.op(pool, lambda: nc.gpsimd.tensor_tensor(out=rtmp[k][:, 0:ncols], in0=pre[i][:, 0:ncols], in1=ropeCs[:, 0:ncols], op=ALU.mult),
             R=[("pre", i), "ropeC"], W=[("rtmp", k)])
        P.op(pool, lambda: nc.gpsimd.tensor_tensor(out=dst_ap, in0=rtmp[j][:, 0:ncols], in1=rtmp[k][:, 0:ncols], op=ALU.add),
             R=[("rtmp", j), ("rtmp", k)], W=[dkey])

    def evac(b, dst_ap, dkey, ncols=GT, M=128, eng=None):
        e = eng or act
        if e is act:
            P.op(act, lambda: nc.scalar.copy(out=dst_ap, in_=psf[0:M, b, 0:ncols]), R=[("ps", b)], W=[dkey])
        else:
            P.op(e, lambda: e.b.tensor_copy(out=dst_ap, in_=psf[0:M, b, 0:ncols]), R=[("ps", b)], W=[dkey])

    def load_rope(g):
        P.dma(ropeCs[:], c_d["ropeC"][:, g * GT:(g + 1) * GT], W=["ropeC"])
        P.dma(ropeSs[:], c_d["ropeS"][:, g * GT:(g + 1) * GT], W=["ropeS"])

    def load_x(src_d, s, g):
        i = rot(xg, xg_i)
        src = src_d[s].rearrange("(c p) t -> p c t", p=128)
        for c in range(0, 8, 2):
            P.dma(xg[i][:, c:c + 2, :], src[:, c:c + 2, g * GT:(g + 1) * GT], W=[("xg", i)])
        return xg[i], ("xg", i)

    class XChain:
        def __init__(self, src_d, s):
            self.src_d, self.s = src_d, s
            self.order = list(range(NG)) + list(range(NG))
            self.k = 0
            self.pending = load_x(src_d, s, self.order[0])

        def get(self):
            cur = self.pending
            self.k += 1
            self.pending = load_x(self.src_d, self.s, self.order[self.k]) if self.k < len(self.order) else None
            return cur

    def attn_head(q_ap, q_key, blocks, dst_fn, scale=0.125, guard=False):
        bo = bankL()
        nb = len(blocks)
        pend = []
        for idx, blk in enumerate(blocks):
            bs = bank()
            c0, c1 = blk.get("c0", 0), blk.get("c1", GT)
            assert idx > 0 or (c0 == 0 and c1 == GT)
            P.op(pe, lambda: nc.tensor.matmul(psf[:, bs, c0:c1], lhsT=blk["kT"], rhs=q_ap[:, c0:c1], start=True, stop=True),
                 R=[blk["kkey"], q_key], W=[("ps", bs)])
            pi = rot(PT, PT_i)
            P.op(act, lambda: nc.scalar.activation(out=PT[pi][:, c0:c1], in_=psf[:, bs, c0:c1], func=AF.Exp, scale=scale),
                 R=[("ps", bs)], W=[("PT", pi)])
            if blk.get("mask") is not None:
                m_ap, m_key = blk["mask"]
                P.op(dve, lambda: nc.vector.tensor_tensor(out=PT[pi][:, c0:c1], in0=PT[pi][:, c0:c1], in1=m_ap[:, c0:c1], op=ALU.mult),
                     R=[("PT", pi), m_key], W=[("PT", pi)])
            pend.append((blk, pi, idx))
            if len(pend) > 2:
                _pv(pend.pop(0), bo, nb)
        while pend:
            _pv(pend.pop(0), bo, nb)
        ri = rot(rd, rd_i)
        if guard:
            P.op(dve, lambda: nc.vector.tensor_scalar_max(out=rd[ri][0:64, :], in0=psf[0:64, bo, :], scalar1=1e-18),
                 R=[("ps", bo)], W=[("rd", ri)])
            P.op(act, lambda: nc.scalar.activation(out=rd[ri][0:64, :], in_=rd[ri][0:64, :], func=AF.Ln), R=[("rd", ri)], W=[("rd", ri)])
        else:
            P.op(act, lambda: nc.scalar.activation(out=rd[ri][0:64, :], in_=psf[0:64, bo, :], func=AF.Ln), R=[("ps", bo)], W=[("rd", ri)])
        P.op(act, lambda: nc.scalar.activation(out=rd[ri][0:64, :], in_=rd[ri][0:64, :], func=AF.Exp, scale=-1.0), R=[("rd", ri)], W=[("rd", ri)])
        dst_fn(bo, rd[ri][0:64, :], ("rd", ri))

    def _pv(item, bo, nb):
        blk, pi, idx = item
        c0, c1 = blk.get("c0", 0), blk.get("c1", GT)
        P.op(pe, lambda: nc.tensor.matmul(psf[:, bo, c0:c1], lhsT=blk["v1"], rhs=PT[pi][:, c0:c1], start=(idx == 0), stop=(idx == nb - 1)),
             R=[blk["vkey"], ("PT", pi)], W=[("ps", bo)])

    def plain_dst(chunk, base):
        def f(bo, rden, rkey):
            P.op(dve, lambda: nc.vector.tensor_tensor(out=oT[base:base + 64, chunk, :], in0=psf[64:128, bo, :], in1=rden, op=ALU.mult),
                 R=[("ps", bo), rkey], W=["hT"])
        return f

    def mem_kv(s, li):
        for c in range(0, 8, 4):
            P.dma(memf[:, c:c + 4, :], memT_d[s].rearrange("(c p) t -> p c t", p=128)[:, c:c + 4, :], W=[("xg", 0)])
        norm_group(memf, ("xg", 0), 2 + li, ncols=256, dst=memnT, dkey="maskT")
        wname = f"mem_w_kv{li}"
        wt, wk = load_w(wname, 0, 512)
        for c in range(2):
            b = proj(lambda kc, c=c: wt[:, kc, c * 128:(c + 1) * 128], [wk], memnT, "maskT", 256)
            evac(b, kmemT[:, c, :], "kmemT", ncols=256)
        P.op(pool, lambda: nc.gpsimd.memset(v1m[:, :, :, 0:64], 1.0), W=["v1m"])
        for nb_ in range(2):
            b = bank()
            for kc in range(8):
                P.op(pe, lambda kc=kc: nc.tensor.matmul(psf[:, b, 0:256], lhsT=memnT[:, kc, nb_ * 128:(nb_ + 1) * 128],
                                                        rhs=wt[:, kc, 256:512], start=(kc == 0), stop=(kc == 7)),
                     R=["maskT", wk], W=[("ps", b)])
            P.op(act, lambda: nc.scalar.copy(out=v1m[:, nb_, :, 64:128], in_=psf[:, b, 0:256].rearrange("p (h d) -> p h d", h=4)),
                 R=[("ps", b)], W=["v1m"])

    def mem_attn():
        for h in range(4):
            c, base = h // 2, (h % 2) * 64
            blocks = [dict(kT=kmemT[base:base + 64, c, nb_ * 128:(nb_ + 1) * 128], kkey="kmemT",
                           v1=v1m[:, nb_, h, :], vkey="v1m", mask=None) for nb_ in range(2)]
            attn_head(qmemT[base:base + 64, c, :], "qmemT", blocks, plain_dst(6 + c, base))

    def wo_ffn(s, li, g, xt, xkey, last):
        for half in range(2):
            wt, wk = load_w(f"w_o{li}", half * 512, 512)
            for oc4 in range(4):
                oc = half * 4 + oc4
                b = proj(lambda kc, oc4=oc4, wt=wt: wt[:, kc, oc4 * 128:(oc4 + 1) * 128], [wk], oT, "hT", GT)
                P.op(dve, lambda oc=oc, b=b: nc.vector.tensor_tensor(out=xt[:, oc, :], in0=xt[:, oc, :], in1=psf[:, b, :], op=ALU.add),
                     R=[xkey, ("ps", b)], W=[xkey])
        norm_group(xt, xkey, 4 + li)
        wsrc = wb_d[f"ffn_w_in{li}"].rearrange("(c p) n -> p c n", p=128)
        dsrc = wb_d[f"ffn_w_down{li}"].rearrange("(c p) n -> p c n", p=128)
        for fh in range(2):
            for cl in range(11):
                ch = fh * 11 + cl
                i = rot(wbuf, wbuf_i)
                P.dma(wbuf[i][:, :, 0:128], wsrc[:, :, ch * 128:(ch + 1) * 128], R=[("wb", f"ffn_w_in{li}")], W=[("wbuf", i)])
                P.dma(wbuf[i][:, :, 128:256], wsrc[:, :, DFF + ch * 128:DFF + (ch + 1) * 128], R=[("wb", f"ffn_w_in{li}")], W=[("wbuf", i)])
                wgk = ("wbuf", i)
                bg = proj(lambda kc, i=i: wbuf[i][:, kc, 0:128], [wgk], hT, "hT", GT)
                bu = proj(lambda kc, i=i: wbuf[i][:, kc, 128:256], [wgk], hT, "hT", GT)
                si = rot(sil, sil_i)
                P.op(act, lambda bg=bg, si=si: nc.scalar.activation(out=sil[si][:], in_=psf[:, bg, :], func=AF.Silu),
                     R=[("ps", bg)], W=[("sil", si)])
                P.op(dve, lambda cl=cl, bu=bu, si=si: nc.vector.tensor_tensor(out=actT[:, cl, :], in0=psf[:, bu, :], in1=sil[si][:], op=ALU.mult),
                     R=[("ps", bu), ("sil", si)], W=["maskT"])
            for oc in range(8):
                i = rot(wdn, wdn_i)
                P.dma(wdn[i][:], dsrc[:, fh * 11:(fh + 1) * 11, oc * 128:(oc + 1) * 128], R=[("wb", f"ffn_w_down{li}")], W=[("wdn", i)])
                b = bankL()
                for kc in range(11):
                    P.op(pe, lambda kc=kc, i=i, b=b: nc.tensor.matmul(psf[:, b, :], lhsT=wdn[i][:, kc, :], rhs=actT[:, kc, :],
                                                                     start=(kc == 0), stop=(kc == 10)),
                         R=[("wdn", i), "maskT"], W=[("ps", b)])
                P.op(dve, lambda oc=oc, b=b: nc.vector.tensor_tensor(out=xt[:, oc, :], in0=xt[:, oc, :], in1=psf[:, b, :], op=ALU.add),
                     R=[xkey, ("ps", b)], W=[xkey])

    def final_store(s, g, xt, xkey, last):
        if last:
            b = bank()
            for c in range(8):
                sumsq_mm(b, xt[:, c, :], xkey, GT, c == 0, c == 7)
            P.op(act, lambda: nc.scalar.activation(out=rstd[:], in_=psf[:, b, :], func=AF.Sqrt, scale=1.0 / D, bias=epsT[:, 0:1]),
                 R=[("ps", b)], W=["rstd"])
            P.op(dve, lambda: nc.vector.reciprocal(out=rstd[:], in_=rstd[:]), R=["rstd"], W=["rstd"])
            for c in range(8):
                P.op(dve, lambda c=c: nc.vector.scalar_tensor_tensor(out=xt[:, c, :], in0=xt[:, c, :], scalar=gam[:, 6, c:c + 1],
                                                                      in1=rstd[:], op0=ALU.mult, op1=ALU.mult),
                     R=[xkey, "rstd", "gam"], W=[xkey])
            dst = outT_d[s].rearrange("(c p) t -> p c t", p=128)
            P.dma(dst[:, :, g * GT:(g + 1) * GT], xt[:], R=[xkey], W=["out"])
        else:
            dst = xs_d[s].rearrange("(c p) t -> p c t", p=128)
            P.dma(dst[:, :, g * GT:(g + 1) * GT], xt[:], R=[xkey], W=[("xs", s)])

    def dsa_layer(s, li, src_d, last):
        barrier()
        mem_kv(s, li)
        xc = XChain(src_d, s)
        wt, wk = load_w("dsa_w_in", 896, 16 + 0)
        P.op(pool, lambda: nc.gpsimd.memset(wkr2[:], 0.0), W=["wkr2"])
        P.op(pool, lambda: nc.gpsimd.tensor_copy(out=wkr2[:, :, 0:16], in_=wt[:, :, 0:16]), R=[wk], W=["wkr2"])
        P.op(pool, lambda: nc.gpsimd.tensor_copy(out=wkr2[:, :, 64:80], in_=wt[:, :, 0:16]), R=[wk], W=["wkr2"])
        wt2, wk2 = load_w("dsa_w_in", 1424, 64)
        P.op(pool, lambda: nc.gpsimd.tensor_copy(out=wki2[:, :, 0:64], in_=wt2[:, :, 0:64]), R=[wk2], W=["wki2"])
        P.op(pool, lambda: nc.gpsimd.tensor_copy(out=wki2[:, :, 64:128], in_=wt2[:, :, 0:64]), R=[wk2], W=["wki2"])
        P.op(pool, lambda: nc.gpsimd.memset(V1[:, :, 0:64], 1.0), W=["V1"])
        for g in range(NG):
            xt, xkey = xc.get()
            load_rope(g)
            norm_group(xt, xkey, 0 + li)
            wc, wck = load_w("dsa_w_in", 768, 128)
            b = proj(lambda kc: wc[:, kc, 0:128], [wck], hT, "hT", GT)
            evac(b, ckvf[:], "ckvf", eng=dve)
            b2 = bank()
            sumsq_mm(b2, ckvf[:], "ckvf", GT, True, True)
            P.op(act, lambda: nc.scalar.activation(out=rstd[:], in_=psf[:, b2, :], func=AF.Sqrt, scale=1.0 / 128, bias=epsT[:, 0:1]),
                 R=[("ps", b2)], W=["rstd"])
            P.op(dve, lambda: nc.vector.reciprocal(out=rstd[:], in_=rstd[:]), R=["rstd"], W=["rstd"])
            P.op(dve, lambda: nc.vector.scalar_tensor_tensor(out=ckvn[:], in0=ckvf[:], scalar=ckvg[:, 0:1], in1=rstd[:],
                                                             op0=ALU.mult, op1=ALU.mult),
                 R=["ckvf", "rstd", "ckvg"], W=["ckvn"])
            bk = bank()
            for kc in range(8):
                P.op(pe, lambda kc=kc: nc.tensor.matmul(psf[:, bk, :], lhsT=wkr2[:, kc, :], rhs=hT[:, kc, :], start=(kc == 0), stop=False),
                     R=["wkr2", "hT"], W=[("ps", bk)])
            P.op(pe, lambda: nc.tensor.matmul(psf[:, bk, :], lhsT=wuk2[:], rhs=ckvn[:], start=False, stop=True),
                 R=["wuk2", "ckvn"], W=[("ps", bk)])
            rope_from_bank(bk, KT2[:, g * GT:(g + 1) * GT], "KT2")
            bi = proj(lambda kc: wki2[:, kc, :], ["wki2"], hT, "hT", GT)
            rope_from_bank(bi, kidxT2[:, g * GT:(g + 1) * GT], "kidxT2")
            for tt in range(4):
                bv = bank()
                P.op(pe, lambda tt=tt: nc.tensor.matmul(psf[:, bv, 0:64], lhsT=ckvn[:, tt * 128:(tt + 1) * 128], rhs=wuv[:], start=True, stop=True),
                     R=["ckvn", "wuv"], W=[("ps", bv)])
                P.op(act, lambda tt=tt, bv=bv: nc.scalar.copy(out=V1[:, g * 4 + tt, 64:128], in_=psf[:, bv, 0:64]), R=[("ps", bv)], W=["V1"])
        for g in range(NG):
            xt, xkey = xc.get()
            load_rope(g)
            norm_group(xt, xkey, 0 + li)
            wq, wqk = load_w("dsa_w_in", 0, 512)
            for c in range(4):
                b = proj(lambda kc, c=c: wq[:, kc, c * 128:(c + 1) * 128], [wqk], hT, "hT", GT)
                rope_from_bank(b, qT[:, c, :], "qT")
            wq2, wq2k = load_w("dsa_w_in", 512, 256)
            for c in range(2):
                b = proj(lambda kc, c=c: wq2[:, kc, c * 128:(c + 1) * 128], [wq2k], hT, "hT", GT)
                rope_from_bank(b, qT[:, 4 + c, :], "qT")
            wi, wik = load_w("dsa_w_in", 912, 512)
            for c in range(4):
                b = proj(lambda kc, c=c: wi[:, kc, c * 128:(c + 1) * 128], [wik], hT, "hT", GT)
                rope_from_bank(b, qidxT[:, c, :], "qidxT")
            wm_, wmk = load_w("dsa_w_in", 1488, 264)
            for c in range(2):
                b = proj(lambda kc, c=c: wm_[:, kc, 8 + c * 128:8 + (c + 1) * 128], [wmk], hT, "hT", GT)
                evac(b, qmemT[:, c, :], "qmemT")
            for tt in range(4):
                b = bank()
                for kc in range(8):
                    P.op(pe, lambda kc=kc, tt=tt: nc.tensor.matmul(psf[:, b, 0:8], lhsT=hT[:, kc, tt * 128:(tt + 1) * 128], rhs=wm_[:, kc, 0:8],
                                                                   start=(kc == 0), stop=(kc == 7)),
                         R=["hT", wmk], W=[("ps", b)])
                P.op(act, lambda tt=tt, b=b: nc.scalar.mul(widx[:, tt, :], psf[:, b, 0:8], float(8 ** -0.5 * 64 ** -0.5)),
                     R=[("ps", b)], W=["widx"])
            P.op(pool, lambda: nc.gpsimd.memset(maskT[:], 0.0), W=["maskT"])
            def idx_phase(tt):
                qi = g * 4 + tt
                u = tt % 2
                Sc = 128 * (qi + 1)
                di = rot(Dh, Dh_i)
                for h in range(8):
                    P.op(dve, lambda h=h: nc.vector.tensor_scalar(out=Dh[di][:, h, :], in0=ident_b, scalar1=widx[:, tt, h:h + 1],
                                                                  scalar2=None, op0=ALU.mult),
                         R=["misc_b", "widx"], W=[("Dh", di)])
                si = rot(sc, sc_i)
                for k0 in range(0, Sc, 512):
                    kn = min(512, Sc - k0)
                    ba = bankL()
                    for h in range(8):
                        c, base = h // 2, (h % 2) * 64
                        bl = bank()
                        P.op(pe, lambda: nc.tensor.matmul(
                            psf[:, bl, 0:kn], lhsT=qidxT[base:base + 64, c, tt * 128:(tt + 1) * 128],
                            rhs=kidxT2[base:base + 64, k0:k0 + kn], start=True, stop=True),
                             R=["qidxT", "kidxT2"], W=[("ps", bl)])
                        ri = rot(relu_t, relu_i)
                        P.op(act, lambda: nc.scalar.activation(out=relu_t[ri][:, 0:kn], in_=psf[:, bl, 0:kn], func=AF.Relu),
                             R=[("ps", bl)], W=[("relu", ri)])
                        P.op(pe, lambda: nc.tensor.matmul(
                            psf[:, ba, 0:kn], lhsT=Dh[di][:, h, :], rhs=relu_t[ri][:, 0:kn], start=(h == 0), stop=(h == 7)),
                             R=[("Dh", di), ("relu", ri)], W=[("ps", ba)])
                    P.op(act, lambda: nc.scalar.copy(out=sc[si][:, k0:k0 + kn], in_=psf[:, ba, 0:kn]),
                         R=[("ps", ba)], W=[("sc", si)])
                scv = sc[si]
                skey = ("sc", si)
                B = lambda j: bst[:, u, j:j + 1]
                K = lambda j: ("bst", u, j)
                if qi >= 2:
                    P.op(dve, lambda: nc.vector.tensor_reduce(out=B(0), in_=scv[:, 0:Sc], axis=AX.X, op=ALU.min), R=[skey], W=[K(0)])
                    P.op(dve, lambda: nc.vector.tensor_reduce(out=B(1), in_=scv[:, 0:Sc], axis=AX.X, op=ALU.max), R=[skey], W=[K(1)])
                P.op(dve, lambda: nc.vector.tensor_tensor(out=scv[:, Sc - 128:Sc], in0=scv[:, Sc - 128:Sc], in1=cbias_f, op=ALU.add),
                     R=[skey, "misc_f", K(0), K(1)], W=[skey])
                mi = rot(mask_tm, mask_i)
                return dict(tt=tt, qi=qi, u=u, Sc=Sc, scv=scv, skey=skey, mi=mi)

            def bis_gen(st):
                u, Sc, scv, skey, mi = st["u"], st["Sc"], st["scv"], st["skey"], st["mi"]
                B = lambda j: bst[:, u, j:j + 1]
                K = lambda j: ("bst", u, j)
                wk, wk2 = ("wtab", u), ("wtab2", u)
                NIT = 16
                P.op(dve, lambda: nc.vector.tensor_tensor(out=B(2), in0=B(1), in1=B(0), op=ALU.subtract), R=[K(0), K(1)], W=[K(2)])
                P.op(dve, lambda: nc.vector.tensor_scalar(out=wtab[:, u, :], in0=bis[:], scalar1=B(2), scalar2=None, op0=ALU.mult),
                     R=["bis", K(2)], W=[wk])
                P.op(dve, lambda: nc.vector.tensor_scalar(out=wtab2[:, u, :], in0=bis[:], scalar1=B(2), scalar2=2.0, op0=ALU.mult, op1=ALU.mult),
                     R=["bis", K(2)], W=[wk2])
                P.op(dve, lambda: nc.vector.tensor_tensor(out=B(3), in0=B(0), in1=wtab[:, u, 0:1], op=ALU.add), R=[K(0), wk], W=[K(3)])
                yield
                for it in range(NIT):
                    P.op(dve, lambda: nc.vector.tensor_scalar(out=mask_tm[mi][:, 0:Sc], in0=scv[:, 0:Sc], scalar1=B(3), scalar2=0.0,
                                                              op0=ALU.is_ge, op1=ALU.add, accum_out=B(4)),
                         R=[skey, K(3)], W=[("masktm", mi), K(4)])
                    yield
                    if it < NIT - 1:
                        wn = wtab[:, u, it + 1:it + 2]
                        wn2 = wtab2[:, u, it + 1:it + 2]
                        P.op(dve, lambda: nc.vector.tensor_scalar(out=B(5), in0=B(4), scalar1=255.5, scalar2=wn2, op0=ALU.is_ge, op1=ALU.mult),
                             R=[K(4), wk2], W=[K(5)])
                        yield
                        P.op(dve, lambda: nc.vector.scalar_tensor_tensor(out=B(3), in0=B(5), scalar=wn, in1=B(3), op0=ALU.subtract, op1=ALU.add),
                             R=[K(5), K(3), wk], W=[K(3)])
                        yield
                    else:
                        wl = wtab[:, u, it:it + 1]
                        P.op(dve, lambda: nc.vector.tensor_scalar(out=B(5), in0=B(4), scalar1=255.5, scalar2=1.0, op0=ALU.is_ge, op1=ALU.subtract),
                             R=[K(4)], W=[K(5)])
                        yield
                        P.op(dve, lambda: nc.vector.scalar_tensor_tensor(out=B(6), in0=B(5), scalar=wl, in1=B(3), op0=ALU.mult, op1=ALU.add),
                             R=[K(5), K(3), wk], W=[K(6)])
                        yield

            def fin_phase(st):
                tt, qi, u, Sc, scv, skey, mi = st["tt"], st["qi"], st["u"], st["Sc"], st["scv"], st["skey"], st["mi"]
                if qi >= 2:
                    P.op(dve, lambda: nc.vector.tensor_scalar(out=mask_tm[mi][:, 0:Sc], in0=scv[:, 0:Sc], scalar1=bst[:, u, 6:7], scalar2=None, op0=ALU.is_ge),
                         R=[skey, ("bst", u, 6)], W=[("masktm", mi)])
                else:
                    P.op(dve, lambda: nc.vector.tensor_scalar(out=mask_tm[mi][:, 0:Sc], in0=scv[:, 0:Sc], scalar1=-1.0e29, scalar2=None, op0=ALU.is_ge),
                         R=[skey], W=[("masktm", mi)])
                nblk = qi + 1
                for j0 in range(0, nblk, 8):
                    jn = min(8, nblk - j0)
                    for j in range(j0, j0 + jn):
                        P.op(pe, lambda: nc.tensor.transpose(psb[:, (j - j0) * 128:(j - j0 + 1) * 128],
                                                             mask_tm[mi][:, j * 128:(j + 1) * 128], ident_b),
                             R=[("masktm", mi), "misc_b"], W=["psb"])
                    P.op(act, lambda: nc.scalar.copy(out=maskT[:, j0:j0 + jn, tt * 128:(tt + 1) * 128],
                                                     in_=psb[:, 0:jn * 128].rearrange("p (j t) -> p j t", j=jn)),
                         R=["psb"], W=["maskT"])

            for t0 in (0, 2):
                sts = [idx_phase(t0), idx_phase(t0 + 1)]
                gens = [bis_gen(st) for st in sts if st["qi"] >= 2]
                while gens:
                    for gg in list(gens):
                        try:
                            next(gg)
                        except StopIteration:
                            gens.remove(gg)
                for st in sts:
                    fin_phase(st)
            nkb = 4 * g + 4
            for h in range(12):
                c, base = h // 2, (h % 2) * 64
                blocks = [dict(kT=KT2[base:base + 64, j * 128:(j + 1) * 128], kkey="KT2", v1=V1[:, j, :], vkey="V1",
                               mask=(maskT[:, j, :], "maskT"), c0=128 * max(0, j - 4 * g)) for j in range(nkb)]
                attn_head(qT[base:base + 64, c, :], "qT", blocks, plain_dst(c, base))
            mem_attn()
            wo_ffn(s, li, g, xt, xkey, last)
            final_store(s, g, xt, xkey, last)

    def nsa_layer(s, li, src_d, last):
        barrier()
        mem_kv(s, li)
        xc = XChain(src_d, s)
        for kv, nm in enumerate(["cmp_k_w1", "cmp_v_w1"]):
            i = rot(wbuf, wbuf_i)
            w1c = wbuf[i][:].rearrange("p a b -> p (a b)")[:, 0:2048].rearrange("p (c m) -> p c m", m=128)
            src2 = wb_d[nm].rearrange("(c p) m -> p c m", p=128)
            P.dma(w1c, src2, R=[("wb", nm)], W=[("wbuf", i)])
            b = bank()
            for c in range(16):
                P.op(pe, lambda c=c, kv=kv, w1c=w1c, b=b: nc.tensor.matmul(psf[:, b, 0:1], lhsT=w1c[:, c, :], rhs=posb[:, kv, c:c + 1],
                                                                          start=(c == 0), stop=(c == 15)),
                     R=[("wbuf", i), "posb"], W=[("ps", b)])
            P.op(act, lambda kv=kv, b=b: nc.scalar.copy(out=cb_b[:, kv:kv + 1], in_=psf[:, b, 0:1]), R=[("ps", b)], W=["cb_b"])
        if STOP[0] == 1:
            raise StopBuild()
        P.op(pool, lambda: nc.gpsimd.memset(V1s[:, :, :, 0:64], 1.0), W=["V1s"])
        P.op(pool, lambda: nc.gpsimd.memset(V1w[:, :, :, 0:64], 1.0), W=["V1w"])
        for g in range(NG):
            xt, xkey = xc.get()
            load_rope(g)
            norm_group(xt, xkey, 0 + li)
            wk_, wkk = load_w("nsa_w_in", 768, 512)
            wk2_, wk2k = load_w("nsa_w_in", 1280, 256)
            b = proj(lambda kc: wk_[:, kc, 0:128], [wkk], hT, "hT", GT)
            evac(b, kcT[:, g * GT:(g + 1) * GT], "kcT")
            b = proj(lambda kc: wk_[:, kc, 128:256], [wkk], hT, "hT", GT)
            evac(b, vcT[:, g * GT:(g + 1) * GT], "vcT")
            for src_t, src_k, off, dstT, dkey in ((wk_, wkk, 256, ksT2, "ksT2"), (wk2_, wk2k, 0, kwT2, "kwT2")):
                for grp in range(2):
                    bb = bank()
                    for half in range(2):
                        for kc in range(8):
                            P.op(pe, lambda kc=kc, half=half, grp=grp, src_t=src_t, off=off, bb=bb: nc.tensor.matmul(
                                psf[half * 64:half * 64 + 64, bb, :], lhsT=src_t[:, kc, off + grp * 64:off + grp * 64 + 64], rhs=hT[:, kc, :],
                                start=(kc == 0), stop=(kc == 7)),
                                 R=[src_k, "hT"], W=[("ps", bb)])
                    rope_from_bank(bb, dstT[:, grp, g * GT:(g + 1) * GT], dkey)
            for src_t, src_k, off, dstV, dkey in ((wk_, wkk, 384, V1s, "V1s"), (wk2_, wk2k, 128, V1w, "V1w")):
                for tt in range(4):
                    bv = bank()
                    for kc in range(8):
                        P.op(pe, lambda kc=kc, tt=tt, src_t=src_t, off=off, bv=bv: nc.tensor.matmul(
                            psf[:, bv, 0:128], lhsT=hT[:, kc, tt * 128:(tt + 1) * 128], rhs=src_t[:, kc, off:off + 128],
                            start=(kc == 0), stop=(kc == 7)),
                             R=["hT", src_k], W=[("ps", bv)])
                    P.op(act, lambda tt=tt, bv=bv, dstV=dstV: nc.scalar.copy(out=dstV[:, g * 4 + tt, :, 64:128],
                                                                               in_=psf[:, bv, 0:128].rearrange("p (g d) -> p g d", g=2)),
                         R=[("ps", bv)], W=[dkey])
        if STOP[0] == 2:
            raise StopBuild()
        P.op(pool, lambda: nc.gpsimd.memset(V1c[:, :, 0:64], 1.0), W=["V1c"])
        for kv, (srcT, skey) in enumerate(((kcT, "kcT"), (vcT, "vcT"))):
            nm = ["cmp_k_w1", "cmp_v_w1"][kv]
            wi_ = rot(wbuf, wbuf_i)
            w1dup = wbuf[wi_][:].rearrange("p a b -> p (a b)").rearrange("p (l m) -> p l m", m=128)
            srcw = wb_d[nm].rearrange("(l d) m -> d l m", d=64)
            P.dma(w1dup[0:64, :, :], srcw, R=[("wb", nm)], W=[("wbuf", wi_)])
            P.dma(w1dup[64:128, :, :], srcw, R=[("wb", nm)], W=[("wbuf", wi_)])
            for grp in range(2):
                base = grp * 64
                b = bank()
                for l in range(32):
                    P.op(pe, lambda l=l, kv=kv, base=base, srcT=srcT, b=b, w1dup=w1dup: nc.tensor.matmul(
                        psf[:, b, 0:127], lhsT=w1dup[base:base + 64, l, :], rhs=srcT[base:base + 64, l:l + 16 * 126 + 1:16],
                        start=(l == 0), stop=(l == 31)),
                         R=[("wbuf", wi_), skey], W=[("ps", b)])
                P.op(act, lambda kv=kv, b=b: nc.scalar.activation(out=gel[0][:, 0:127], in_=psf[:, b, 0:127], func=AF.Identity,
                                                                  bias=cb_b[:, kv:kv + 1], scale=1.0),
                     R=[("ps", b), "cb_b"], W=["gel0"])
                P.op(act, lambda: nc.scalar.activation(out=gel[1][:, 0:127], in_=gel[0][:, 0:127], func=AF.Square), R=["gel0"], W=["gel1"])
                P.op(dve, lambda: nc.vector.tensor_scalar(out=gel[1][:, 0:127], in0=gel[1][:, 0:127], scalar1=0.044715, scalar2=1.0,
                                                          op0=ALU.mult, op1=ALU.add), R=["gel1"], W=["gel1"])
                P.op(dve, lambda: nc.vector.tensor_tensor(out=gel[2][:, 0:127], in0=gel[1][:, 0:127], in1=gel[0][:, 0:127], op=ALU.mult),
                     R=["gel1", "gel0"], W=["gel2"])
                P.op(act, lambda: nc.scalar.activation(out=gel[3][:, 0:127], in_=gel[2][:, 0:127], func=AF.Sigmoid, scale=1.5957691216),
                     R=["gel2"], W=["gel3"])
                P.op(pool, lambda: nc.gpsimd.memset(gT[:], 0.0), W=["gT"])
                P.op(dve, lambda: nc.vector.tensor_tensor(out=gT[:, 0:127], in0=gel[3][:, 0:127], in1=gel[0][:, 0:127], op=ALU.mult),
                     R=["gel3", "gel0"], W=["gT"])
                b2 = bank()
                if kv == 0:
                    P.op(pe, lambda: nc.tensor.matmul(psf[:, b2, 0:128], lhsT=w2k2[:], rhs=gT[:], start=True, stop=True),
                         R=["w2k2", "gT"], W=[("ps", b2)])
                    evac(b2, kcmpT2[:, grp, :], "kcmpT2", ncols=128)
                else:
                    P.op(pe, lambda: nc.tensor.matmul(psf[:, b2, 0:64], lhsT=gT[:], rhs=w2v[:], start=True, stop=True),
                         R=["w2v", "gT"], W=[("ps", b2)])
                    evac(b2, V1c[:, grp, 64:128], "V1c", ncols=64)
        if STOP[0] == 3:
            raise StopBuild()
        barrier()
        if STOP[0] == 40:
            raise StopBuild()
        for g in range(NG):
            xt, xkey = xc.get()
            if STOP[0] == 401:
                raise StopBuild()
            load_rope(g)
            if STOP[0] == 402:
                raise StopBuild()
            norm_group(xt, xkey, 0 + li)
            if STOP[0] == 41:
                raise StopBuild()
            wq, wqk = load_w("nsa_w_in", 0, 512)
            wq2, wq2k = load_w("nsa_w_in", 512, 256)
            for c in range(6):
                w_, wk_ = (wq, wqk) if c < 4 else (wq2, wq2k)
                cc = c if c < 4 else c - 4
                b = proj(lambda kc, cc=cc, w_=w_: w_[:, kc, cc * 128:(cc + 1) * 128], [wk_], hT, "hT", GT)
                rope_from_bank(b, qT[:, c, :], "qT", raw=(qrawT[:, c, :], "qrawT"))
            if STOP[0] == 42:
                raise StopBuild()
            wm_, wmk = load_w("nsa_w_in", 1536, 292)
            for c in range(2):
                b = proj(lambda kc, c=c: wm_[:, kc, 36 + c * 128:36 + (c + 1) * 128], [wmk], hT, "hT", GT)
                evac(b, qmemT[:, c, :], "qmemT")
            if STOP[0] == 43:
                raise StopBuild()
            b = proj(lambda kc: wm_[:, kc, 0:64], [wmk], hT, "hT", GT, M=64)
            P.op(act, lambda b=b: nc.scalar.activation(out=sigT[:], in_=psf[0:36, b, :], func=AF.Sigmoid), R=[("ps", b)], W=["sigT"])
            P.op(dve, lambda: nc.vector.tensor_copy(out=sigH[:], in_=sigT[:]), R=["sigT"], W=["sigH"])
            P.op(dve, lambda: nc.vector.tensor_tensor(out=sigL[:], in0=sigT[:], in1=sigH[:], op=ALU.subtract), R=["sigT", "sigH"], W=["sigL"])
            if STOP[0] == 4:
                raise StopBuild()
            mcg = mc_b[:, g * GT:(g + 1) * GT]
            P.op(pool, lambda: nc.gpsimd.memset(imp[:], 0.0), W=["imp"])

            def gated_dst(h, br, add_ap, add_key, out_ap, out_key):
                def f(bo, rden, rkey):
                    bgate = bank()
                    kk = h * 3 + br
                    P.op(pe, lambda: nc.tensor.matmul(psf[0:64, bgate, :], lhsT=E_flat[0:36, kk * 64:(kk + 1) * 64], rhs=sigH[:], start=True, stop=False),
                         R=["E_b", "sigH"], W=[("ps", bgate)])
                    P.op(pe, lambda: nc.tensor.matmul(psf[0:64, bgate, :], lhsT=E_flat[0:36, kk * 64:(kk + 1) * 64], rhs=sigL[:], start=False, stop=True),
                         R=["E_b", "sigL"], W=[("ps", bgate)])
                    gi = rot(rg, rg_i)
                    P.op(dve, lambda: nc.vector.tensor_tensor(out=rg[gi][:], in0=psf[0:64, bgate, :], in1=rden, op=ALU.mult),
                         R=[("ps", bgate), rkey], W=[("rg", gi)])
                    base = (h % 2) * 64
                    if add_ap is None:
                        P.op(dve, lambda: nc.vector.tensor_tensor(out=out_ap, in0=psf[64:128, bo, :], in1=rg[gi][:], op=ALU.mult),
                             R=[("ps", bo), ("rg", gi)], W=[out_key])
                    else:
                        oi = rot(otmp, otmp_i)
                        P.op(dve, lambda: nc.vector.tensor_tensor(out=otmp[oi][base:base + 64, :], in0=psf[64:128, bo, :], in1=rg[gi][:], op=ALU.mult),
                             R=[("ps", bo), ("rg", gi)], W=[("otmp", oi)])
                        P.op(pool, lambda: nc.gpsimd.tensor_tensor(out=out_ap, in0=otmp[oi][base:base + 64, :], in1=add_ap, op=ALU.add),
                             R=[("otmp", oi), add_key], W=[out_key])
                return f

            for grp in range(2):
                heads = list(range(grp * 6, grp * 6 + 6))
                for h in heads:
                    c, base = h // 2, (h % 2) * 64
                    bs = bank()
                    P.op(pe, lambda c=c, base=base, bs=bs: nc.tensor.matmul(psf[:, bs, :], lhsT=kcmpT2[base:base + 64, grp, :],
                                                                           rhs=qrawT[base:base + 64, c, :], start=True, stop=True),
                         R=["kcmpT2", "qrawT"], W=[("ps", bs)])
                    pi = rot(PT, PT_i)
                    P.op(act, lambda bs=bs, pi=pi: nc.scalar.activation(out=PT[pi][:], in_=psf[:, bs, :], func=AF.Exp, scale=0.125),
                         R=[("ps", bs)], W=[("PT", pi)])
                    P.op(dve, lambda pi=pi: nc.vector.tensor_tensor(out=PT[pi][:], in0=PT[pi][:], in1=mcg, op=ALU.mult),
                         R=[("PT", pi), "mc_b"], W=[("PT", pi)])
                    for tt in range(4):
                        bi_ = bank()
                        P.op(pe, lambda tt=tt, pi=pi, bi_=bi_: nc.tensor.matmul(psf[:, bi_, 0:33], lhsT=PT[pi][:, tt * 128:(tt + 1) * 128],
                                                                               rhs=ov_b[:], start=True, stop=True),
                             R=[("PT", pi), "ov_b"], W=[("ps", bi_)])
                        P.op(dve, lambda bi_=bi_, tt=tt: nc.vector.tensor_scalar_max(out=rec[:, tt:tt + 1], in0=psf[:, bi_, 32:33], scalar1=1e-30),
                             R=[("ps", bi_)], W=[("rec", tt)])
                        P.op(dve, lambda tt=tt: nc.vector.reciprocal(out=rec[:, tt:tt + 1], in_=rec[:, tt:tt + 1]), R=[("rec", tt)], W=[("rec", tt)])
                        P.op(dve, lambda bi_=bi_, tt=tt: nc.vector.scalar_tensor_tensor(out=imp[:, tt, grp, :], in0=psf[:, bi_, 0:32],
                                                                                        scalar=rec[:, tt:tt + 1], in1=imp[:, tt, grp, :],
                                                                                        op0=ALU.mult, op1=ALU.add),
                             R=[("ps", bi_), ("rec", tt), "imp"], W=["imp"])
                    bo = bankL()
                    P.op(pe, lambda pi=pi, bo=bo: nc.tensor.matmul(psf[:, bo, :], lhsT=V1c[:, grp, :], rhs=PT[pi][:], start=True, stop=True),
                         R=["V1c", ("PT", pi)], W=[("ps", bo)])
                    ri = rot(rd, rd_i)
                    P.op(dve, lambda bo=bo, ri=ri: nc.vector.tensor_scalar_max(out=rd[ri][0:64, :], in0=psf[0:64, bo, :], scalar1=1e-18),
                         R=[("ps", bo)], W=[("rd", ri)])
                    P.op(act, lambda ri=ri: nc.scalar.activation(out=rd[ri][0:64, :], in_=rd[ri][0:64, :], func=AF.Ln), R=[("rd", ri)], W=[("rd", ri)])
                    P.op(act, lambda ri=ri: nc.scalar.activation(out=rd[ri][0:64, :], in_=rd[ri][0:64, :], func=AF.Exp, scale=-1.0), R=[("rd", ri)], W=[("rd", ri)])
                    hl = h - grp * 6
                    gated_dst(h, 0, None, None, cmpo[base:base + 64, hl, :], "cmpo")(bo, rd[ri][0:64, :], ("rd", ri))
                for tt in range(4):
                    qi = g * 4 + tt
                    P.op(dve, lambda tt=tt, qi=qi: nc.vector.tensor_tensor(out=imp2[:, tt, grp, :], in0=imp[:, tt, grp, :], in1=selF[:, 30 - 2 * qi:62 - 2 * qi], op=ALU.max),
                         R=["imp", "selF"], W=["imp2"])
                    P.op(dve, lambda tt=tt, qi=qi: nc.vector.tensor_tensor(out=imp2[:, tt, grp, :], in0=imp2[:, tt, grp, :], in1=selA[:, 30 - 2 * qi:62 - 2 * qi], op=ALU.add),
                         R=["imp2", "selA"], W=["imp2"])
                    P.op(dve, lambda tt=tt: nc.vector.memset(imp2[:, tt, grp, 0:1], 1.0e9), R=["imp2"], W=["imp2"])
                    P.op(dve, lambda tt=tt: nc.vector.max(out=m8[:, 0:8], in_=imp2[:, tt, grp, :]), R=["imp2"], W=["m8a"])
                    P.op(dve, lambda tt=tt: nc.vector.match_replace(out=imp3[:, tt, grp, :], in_to_replace=m8[:, 0:8], in_values=imp2[:, tt, grp, :],
                                                                    imm_value=-3.0e38),
                         R=["imp2", "m8a"], W=["imp3"])
                    P.op(dve, lambda tt=tt: nc.vector.max(out=m8[:, 8:16], in_=imp3[:, tt, grp, :]), R=["imp3"], W=["m8b"])
                    P.op(dve, lambda tt=tt: nc.vector.tensor_scalar(out=selm[:, tt, grp * 32:(grp + 1) * 32], in0=imp2[:, tt, grp, :],
                                                                    scalar1=m8[:, 15:16], scalar2=None, op0=ALU.is_ge),
                         R=["imp2", "m8b"], W=["selm"])
                if STOP[0] == 5:
                    raise StopBuild()
                if grp == 0:
                    P.op(pool, lambda: nc.gpsimd.memset(selm[:, :, 32:64], 0.0), W=["selm"])
                for tt in range(4):
                    P.op(pe, lambda tt=tt: nc.tensor.transpose(psb[0:64, tt * 128:(tt + 1) * 128], selm[:, tt, :], ident_b),
                         R=["selm", "misc_b"], W=["psb"])
                P.op(act, lambda: nc.scalar.copy(out=selT[:], in_=psb[0:64, 0:512]), R=["psb"], W=["selT"])
                nkb = 4 * g + 4
                for j in range(nkb):
                    bm = bank()
                    P.op(pe, lambda j=j, bm=bm: nc.tensor.matmul(psf[:, bm, :], lhsT=E_b[:, grp, j * 128:(j + 1) * 128], rhs=selT[:], start=True, stop=True),
                         R=["E_b", "selT"], W=[("ps", bm)])
                    if j >= 4 * g:
                        P.op(dve, lambda j=j, bm=bm: nc.vector.tensor_tensor(out=maskT[:, j, :], in0=psf[:, bm, :], in1=cm_b[:, j - 4 * g, :], op=ALU.mult),
                             R=[("ps", bm), "cm_b"], W=["maskT"])
                    else:
                        evac(bm, maskT[:, j, :], "maskT")
                if STOP[0] == 6:
                    raise StopBuild()
                for h in heads:
                    c, base = h // 2, (h % 2) * 64
                    hl = h - grp * 6
                    blocks = [dict(kT=ksT2[base:base + 64, grp, j * 128:(j + 1) * 128], kkey="ksT2", v1=V1s[:, j, grp, :], vkey="V1s",
                                   mask=(maskT[:, j, :], "maskT"), c0=128 * max(0, j - 4 * g)) for j in range(nkb)]
                    attn_head(qT[base:base + 64, c, :], "qT", blocks, gated_dst(h, 1, cmpo[base:base + 64, hl, :], "cmpo", oacc[base:base + 64, :], "oacc"))
                    j0 = max(0, 4 * g - 4)
                    order = [4 * g] + [j for j in range(j0, nkb) if j != 4 * g]
                    blocks = [dict(kT=kwT2[base:base + 64, grp, j * 128:(j + 1) * 128], kkey="kwT2", v1=V1w[:, j, grp, :], vkey="V1w",
                                   mask=(wm_b[:, j - 4 * g + 4, :], "wm_b"),
                                   c0=128 * max(0, j - 4 * g), c1=(GT if j >= 4 * g else 128 * (j - 4 * g + 5))) for j in order]
                    attn_head(qT[base:base + 64, c, :], "qT", blocks, gated_dst(h, 2, oacc[base:base + 64, :], "oacc", oT[base:base + 64, c, :], "hT"))
            mem_attn()
            wo_ffn(s, li, g, xt, xkey, last)
            final_store(s, g, xt, xkey, last)

    try:
        for s in range(n_seq):
            for li in range(n_layers):
                src_d = xT_d if li == 0 else xs_d
                last = (li == n_layers - 1)
                if li % 2 == 0:
                    dsa_layer(s, li, src_d, last)
                else:
                    nsa_layer(s, li, src_d, last)
    except StopBuild:
        pass
    barrier()
    P.drain()
    return nc, P


def host_inputs(inputs, core, n_seq=SEQ_PER_CORE):
    x = inputs["x"][core * n_seq:(core + 1) * n_seq]
    mem = inputs["mem"][core * n_seq:(core + 1) * n_seq]
    m = {}
    m["xT"] = np.ascontiguousarray(np.transpose(x, (0, 2, 1)))
    m["memT"] = np.ascontiguousarray(np.transpose(mem, (0, 2, 1)))
    g = np.concatenate([inputs["attn_norm"], inputs["mem_norm"], inputs["ffn_norm"], inputs["final_norm"][None]], axis=0)
    m["gam"] = np.ascontiguousarray(g.reshape(7, 8, 128).transpose(2, 0, 1))
    m["ckvg"] = np.ascontiguousarray(inputs["dsa_ckv_norm"][0].reshape(128, 1))
    m["posk"] = np.ascontiguousarray(inputs["nsa_cmp_pos_k"][0].reshape(16, 128).T)
    m["posv"] = np.ascontiguousarray(inputs["nsa_cmp_pos_v"][0].reshape(16, 128).T)
    m["dsa_w_in"] = inputs["dsa_w_in"][0]
    m["nsa_w_in"] = inputs["nsa_w_in"][0]
    for i in range(2):
        m[f"mem_w_kv{i}"] = inputs["mem_w_kv"][i]
        m[f"w_o{i}"] = inputs["w_o"][i]
        m[f"ffn_w_in{i}"] = inputs["ffn_w_in"][i]
        m[f"ffn_w_down{i}"] = inputs["ffn_w_down"][i]
    m["cmp_k_w1"] = inputs["nsa_cmp_k_w1"][0]
    m["cmp_v_w1"] = inputs["nsa_cmp_v_w1"][0]
    m["dsa_w_uk"] = inputs["dsa_w_uk"][0]
    m["dsa_w_uv"] = inputs["dsa_w_uv"][0]
    m["cmp_k_w2"] = inputs["nsa_cmp_k_w2"][0]
    m["cmp_v_w2"] = inputs["nsa_cmp_v_w2"][0]
    for k, v in make_consts().items():
        m["c_" + k] = v
    return {k: np.ascontiguousarray(v, dtype=np.float32) for k, v in m.items()}


def build2(n_seq, n_layers=2):
    _, P1 = build(n_seq, n_layers)
    return build(n_seq, n_layers, needed=P1.waited)


def kernel(**inputs):
    inputs = {k: np.asarray(v) for k, v in inputs.items()}
    nc, _ = build2(SEQ_PER_CORE)
    in_maps = [host_inputs(inputs, c) for c in range(NCORES)]
    res = run_bass_kernel_spmd(nc, in_maps, core_ids=list(range(NCORES)))
    outs = [np.transpose(r["outT"], (0, 2, 1)) for r in res.results]
    return np.ascontiguousarray(np.concatenate(outs, axis=0)).astype(np.float32)
```
